# Optimizing a Trainium2 kernel written in Bass

```python
import math
import jax, jax.numpy as jnp
from jax import lax
import numpy as np

D_MODEL = 1024
BATCH = 8
SEQ = 4096
DEPTH = 2

HEAD_DIM = 64
A_HEADS = 6
A_KV_HEADS = 2
A_GROUP = A_HEADS // A_KV_HEADS
A_WIDTH = A_HEADS * HEAD_DIM
A_KV_DIM = A_KV_HEADS * HEAD_DIM
WINDOW = 128
BLOCK = 128
SSM_WIDTH = D_MODEL // 4
SSM_GROUP_CH = 16
SSM_GROUPS = SSM_WIDTH // SSM_GROUP_CH
SSM_STATE = 64
DT_MIN = 1e-3
DT_MAX = 1e-1
MLA_HEADS = 6
MLA_NOPE = 64
MLA_ROPE = 32
MLA_V = 64
MLA_Q_RANK = 192
MLA_KV_RANK = 128
MLA_WIDTH = MLA_HEADS * MLA_V
ROPE_BASE = 10000.0
MIX_WIDTH = A_WIDTH + SSM_WIDTH + MLA_WIDTH
IN_SIZES = [A_WIDTH, A_KV_DIM, A_KV_DIM, SSM_WIDTH, MLA_Q_RANK, MLA_KV_RANK, MLA_ROPE]
IN_COLS = sum(IN_SIZES)
N_EXPERT_GROUPS = 4
EXPERTS_PER_GROUP = 8
TOP_K = 2
D_EXPERT = D_MODEL // 4
EPS = 1e-6
NEG_INF = -1e30

kernel_name = "hymba_style_hybrid_encoder"


def rmsnorm(x, g):
    xf = x.astype(jnp.float32)
    y = xf * lax.rsqrt(jnp.mean(xf * xf, axis=-1, keepdims=True) + EPS)
    return (y * g.astype(jnp.float32)).astype(x.dtype)


def alibi_slopes(n):
    return np.array([2.0 ** (-8.0 * (h + 1) / n) for h in range(n)], dtype=np.float32)


def windowed_gqa(q, k, v, sink):
    b, s, _ = q.shape
    nblk = s // BLOCK
    qb = q.reshape(b, nblk, BLOCK, A_KV_HEADS, A_GROUP, HEAD_DIM)
    pad = ((0, 0), (BLOCK, BLOCK), (0, 0))
    kp = jnp.pad(k, pad).reshape(b, nblk + 2, BLOCK, A_KV_HEADS, HEAD_DIM)
    vp = jnp.pad(v, pad).reshape(b, nblk + 2, BLOCK, A_KV_HEADS, HEAD_DIM)
    kb = jnp.concatenate([kp[:, :-2], kp[:, 1:-1], kp[:, 2:]], axis=2)
    vb = jnp.concatenate([vp[:, :-2], vp[:, 1:-1], vp[:, 2:]], axis=2)
    scores = jnp.einsum('bnqhgd,bnkhd->bnhgqk', qb, kb).astype(jnp.float32) * (HEAD_DIM ** -0.5)
    qi = jnp.arange(BLOCK)[:, None]
    ki = jnp.arange(3 * BLOCK)[None, :]
    dist = jnp.abs(ki - BLOCK - qi)
    key_pos = jnp.arange(nblk)[:, None] * BLOCK - BLOCK + jnp.arange(3 * BLOCK)[None, :]
    valid = (dist <= WINDOW)[None] & ((key_pos >= 0) & (key_pos < s))[:, None, :]
    slopes = jnp.asarray(alibi_slopes(A_HEADS)).reshape(A_KV_HEADS, A_GROUP)
    scores = scores - slopes[:, :, None, None] * dist.astype(jnp.float32)
    scores = jnp.where(valid[None, :, None, None], scores, NEG_INF)
    sink_col = jnp.broadcast_to(sink.astype(jnp.float32).reshape(A_KV_HEADS, A_GROUP, 1, 1),
                                scores.shape[:-1] + (1,))
    p = jax.nn.softmax(jnp.concatenate([scores, sink_col], axis=-1), axis=-1)[..., :-1]
    out = jnp.einsum('bnhgqk,bnkhd->bnqhgd', p.astype(v.dtype), vb)
    return out.reshape(b, s, A_WIDTH)


def _cplx_combine(e1, e2):
    a1r, a1i, b1r, b1i = e1
    a2r, a2i, b2r, b2i = e2
    ar = a2r * a1r - a2i * a1i
    ai = a2r * a1i + a2i * a1r
    br = a2r * b1r - a2i * b1i + b2r
    bi = a2r * b1i + a2i * b1r + b2i
    return ar, ai, br, bi


def s5_bidirectional(u, lam_re, lam_im, log_dt, b_re, b_im, c_re, c_im, d_skip, w_glu, b_glu):
    bsz, s, _ = u.shape
    uf = u.astype(jnp.float32)
    ug = uf.reshape(bsz, s, SSM_GROUPS, SSM_GROUP_CH)
    y = (d_skip.astype(jnp.float32) * uf).reshape(bsz, s, SSM_GROUPS, SSM_GROUP_CH)
    for direction in range(2):
        lr = lam_re[direction].astype(jnp.float32)
        li = lam_im[direction].astype(jnp.float32)
        dt = jnp.exp(log_dt[direction].astype(jnp.float32))[:, None]
        mag = jnp.exp(lr * dt)
        ar = mag * jnp.cos(li * dt)
        ai = mag * jnp.sin(li * dt)
        nr = ar - 1.0
        den = lr * lr + li * li
        fr = (nr * lr + ai * li) / den
        fi = (ai * lr - nr * li) / den
        br = b_re[direction].astype(jnp.float32)
        bi = b_im[direction].astype(jnp.float32)
        bbr = fr[..., None] * br - fi[..., None] * bi
        bbi = fr[..., None] * bi + fi[..., None] * br
        bu_r = jnp.einsum('bsgc,gpc->sbgp', ug, bbr)
        bu_i = jnp.einsum('bsgc,gpc->sbgp', ug, bbi)
        a_r = jnp.broadcast_to(ar[None, None], (s, 1, SSM_GROUPS, SSM_STATE))
        a_i = jnp.broadcast_to(ai[None, None], (s, 1, SSM_GROUPS, SSM_STATE))
        _, _, xr, xi = lax.associative_scan(_cplx_combine, (a_r, a_i, bu_r, bu_i),
                                            reverse=(direction == 1), axis=0)
        y = y + jnp.einsum('sbgp,gcp->bsgc', xr, c_re[direction].astype(jnp.float32)) \
              - jnp.einsum('sbgp,gcp->bsgc', xi, c_im[direction].astype(jnp.float32))
    y = jax.nn.gelu(y.reshape(bsz, s, SSM_WIDTH))
    z = y @ w_glu.astype(jnp.float32) + b_glu.astype(jnp.float32)
    out = z[..., :SSM_WIDTH] * jax.nn.sigmoid(z[..., SSM_WIDTH:])
    return out.astype(u.dtype)


def rope(x, pos):
    half = MLA_ROPE // 2
    inv = ROPE_BASE ** (-jnp.arange(half, dtype=jnp.float32) / half)
    ang = pos.astype(jnp.float32)[:, None] * inv[None, :]
    cos = jnp.cos(ang)[None, :, None, :]
    sin = jnp.sin(ang)[None, :, None, :]
    x1 = x[..., :half].astype(jnp.float32)
    x2 = x[..., half:].astype(jnp.float32)
    return jnp.concatenate([x1 * cos - x2 * sin, x1 * sin + x2 * cos], axis=-1).astype(x.dtype)


def mla(cq, ckv, k_rope, q_norm_g, w_uq, kv_norm_g, w_ukv):
    b, s, _ = cq.shape
    pos = jnp.arange(s)
    q = (rmsnorm(cq, q_norm_g) @ w_uq).reshape(b, s, MLA_HEADS, MLA_NOPE + MLA_ROPE)
    kv = (rmsnorm(ckv, kv_norm_g) @ w_ukv).reshape(b, s, MLA_HEADS, MLA_NOPE + MLA_V)
    q = jnp.concatenate([q[..., :MLA_NOPE], rope(q[..., MLA_NOPE:], pos)], axis=-1)
    kr = rope(k_rope[:, :, None, :], pos)
    k = jnp.concatenate([kv[..., :MLA_NOPE],
                         jnp.broadcast_to(kr, (b, s, MLA_HEADS, MLA_ROPE))], axis=-1)
    v = kv[..., MLA_NOPE:]
    scale = (MLA_NOPE + MLA_ROPE) ** -0.5
    nblk = s // BLOCK
    qb = q.reshape(b, nblk, BLOCK, MLA_HEADS, MLA_NOPE + MLA_ROPE).transpose(1, 0, 2, 3, 4)

    def attend(qblk):
        sc = jnp.einsum('bqhd,bkhd->bhqk', qblk, k).astype(jnp.float32) * scale
        p = jax.nn.softmax(sc, axis=-1)
        return jnp.einsum('bhqk,bkhd->bqhd', p.astype(v.dtype), v)

    out = lax.map(attend, qb)
    return out.transpose(1, 0, 2, 3, 4).reshape(b, s, MLA_WIDTH)


def hier_moe(x, w_rg, b_rg, w_re, b_re, w_gate, w_up, w_down):
    b, s, d = x.shape
    xt = x.reshape(b * s, d)
    g_logits = (xt @ w_rg).astype(jnp.float32) + b_rg.astype(jnp.float32)
    g_prob = jax.nn.softmax(g_logits, axis=-1)
    g_idx = jnp.argmax(g_logits, axis=-1)
    p_group = jnp.take_along_axis(g_prob, g_idx[:, None], axis=-1)
    e_logits = ((xt @ w_re).astype(jnp.float32) + b_re.astype(jnp.float32)).reshape(
        -1, N_EXPERT_GROUPS, EXPERTS_PER_GROUP)
    e_sel = jnp.take_along_axis(e_logits, g_idx[:, None, None], axis=1)[:, 0]
    top_v, top_i = lax.top_k(e_sel, TOP_K)
    w_top = jax.nn.softmax(top_v, axis=-1) * p_group
    gate_in = jnp.einsum('nk,nke->ne', w_top,
                         jax.nn.one_hot(top_i, EXPERTS_PER_GROUP, dtype=jnp.float32))
    gate = jax.nn.one_hot(g_idx, N_EXPERT_GROUPS, dtype=jnp.float32)[:, :, None] * gate_in[:, None, :]
    y = jnp.zeros_like(xt)
    for g in range(N_EXPERT_GROUPS):
        h = jax.nn.silu(jnp.einsum('nd,edf->enf', xt, w_gate[g])) * jnp.einsum('nd,edf->enf', xt, w_up[g])
        h = h * gate[:, g].T[:, :, None].astype(h.dtype)
        y = y + jnp.einsum('enf,efd->nd', h, w_down[g])
    return y.reshape(b, s, d)


def setup_inputs(seed: int = 0) -> dict:
    key = jax.random.key(seed)
    ks = list(jax.random.split(key, 40))
    f32 = jnp.float32
    L = DEPTH

    def nrm(k, shape, scale):
        return jax.random.normal(k, shape, f32) * scale

    def gain(k, n):
        return 1.0 + 0.05 * jax.random.normal(k, (L, n), f32)

    cplx = 0.7071
    return {
        "x": nrm(ks[0], (BATCH, SEQ, D_MODEL), 1.0),
        "ln1_g": gain(ks[1], D_MODEL),
        "w_in": nrm(ks[2], (L, D_MODEL, IN_COLS), D_MODEL ** -0.5),
        "attn_sink": nrm(ks[3], (L, A_HEADS), 0.5),
        "ssm_lam_re": -0.5 + 0.01 * jax.random.normal(ks[4], (L, 2, SSM_GROUPS, SSM_STATE), f32),
        "ssm_lam_im": jnp.pi * jnp.arange(SSM_STATE, dtype=f32)
                      + 0.01 * jax.random.normal(ks[5], (L, 2, SSM_GROUPS, SSM_STATE), f32),
        "ssm_log_dt": jax.random.uniform(ks[6], (L, 2, SSM_GROUPS), f32,
                                         math.log(DT_MIN), math.log(DT_MAX)),
        "ssm_b_re": nrm(ks[7], (L, 2, SSM_GROUPS, SSM_STATE, SSM_GROUP_CH), cplx * SSM_GROUP_CH ** -0.5),
        "ssm_b_im": nrm(ks[8], (L, 2, SSM_GROUPS, SSM_STATE, SSM_GROUP_CH), cplx * SSM_GROUP_CH ** -0.5),
        "ssm_c_re": nrm(ks[9], (L, 2, SSM_GROUPS, SSM_GROUP_CH, SSM_STATE), cplx * SSM_STATE ** -0.5),
        "ssm_c_im": nrm(ks[10], (L, 2, SSM_GROUPS, SSM_GROUP_CH, SSM_STATE), cplx * SSM_STATE ** -0.5),
        "ssm_d": nrm(ks[11], (L, SSM_WIDTH), 1.0),
        "ssm_w_glu": nrm(ks[12], (L, SSM_WIDTH, 2 * SSM_WIDTH), SSM_WIDTH ** -0.5),
        "ssm_b_glu": nrm(ks[13], (L, 2 * SSM_WIDTH), 0.01),
        "mla_q_norm_g": gain(ks[14], MLA_Q_RANK),
        "mla_w_uq": nrm(ks[15], (L, MLA_Q_RANK, MLA_HEADS * (MLA_NOPE + MLA_ROPE)), MLA_Q_RANK ** -0.5),
        "mla_kv_norm_g": gain(ks[16], MLA_KV_RANK),
        "mla_w_ukv": nrm(ks[17], (L, MLA_KV_RANK, MLA_HEADS * (MLA_NOPE + MLA_V)), MLA_KV_RANK ** -0.5),
        "out_g_attn": gain(ks[18], A_WIDTH),
        "out_g_ssm": gain(ks[19], SSM_WIDTH),
        "out_g_mla": gain(ks[20], MLA_WIDTH),
        "w_out": nrm(ks[21], (L, MIX_WIDTH, D_MODEL), MIX_WIDTH ** -0.5),
        "ln2_g": gain(ks[22], D_MODEL),
        "w_router_group": nrm(ks[23], (L, D_MODEL, N_EXPERT_GROUPS), D_MODEL ** -0.5),
        "b_router_group": nrm(ks[24], (L, N_EXPERT_GROUPS), 0.01),
        "w_router_expert": nrm(ks[25], (L, D_MODEL, N_EXPERT_GROUPS * EXPERTS_PER_GROUP), D_MODEL ** -0.5),
        "b_router_expert": nrm(ks[26], (L, N_EXPERT_GROUPS * EXPERTS_PER_GROUP), 0.01),
        "w_gate": nrm(ks[27], (L, N_EXPERT_GROUPS, EXPERTS_PER_GROUP, D_MODEL, D_EXPERT), D_MODEL ** -0.5),
        "w_up": nrm(ks[28], (L, N_EXPERT_GROUPS, EXPERTS_PER_GROUP, D_MODEL, D_EXPERT), D_MODEL ** -0.5),
        "w_down": nrm(ks[29], (L, N_EXPERT_GROUPS, EXPERTS_PER_GROUP, D_EXPERT, D_MODEL), D_EXPERT ** -0.5),
        "final_g": 1.0 + 0.05 * jax.random.normal(ks[30], (D_MODEL,), f32),
    }


def reference(x, ln1_g, w_in, attn_sink, ssm_lam_re, ssm_lam_im, ssm_log_dt, ssm_b_re, ssm_b_im,
              ssm_c_re, ssm_c_im, ssm_d, ssm_w_glu, ssm_b_glu, mla_q_norm_g, mla_w_uq, mla_kv_norm_g,
              mla_w_ukv, out_g_attn, out_g_ssm, out_g_mla, w_out, ln2_g, w_router_group, b_router_group,
              w_router_expert, b_router_expert, w_gate, w_up, w_down, final_g):
    split_idx = [int(v) for v in np.cumsum(IN_SIZES)[:-1]]
    for l in range(DEPTH):
        xn = rmsnorm(x, ln1_g[l])
        proj = xn @ w_in[l]
        qa, ka, va, u, cq, ckv, kr = jnp.split(proj, split_idx, axis=-1)
        o_attn = windowed_gqa(qa, ka, va, attn_sink[l])
        o_ssm = s5_bidirectional(u, ssm_lam_re[l], ssm_lam_im[l], ssm_log_dt[l], ssm_b_re[l], ssm_b_im[l],
                                 ssm_c_re[l], ssm_c_im[l], ssm_d[l], ssm_w_glu[l], ssm_b_glu[l])
        o_mla = mla(cq, ckv, kr, mla_q_norm_g[l], mla_w_uq[l], mla_kv_norm_g[l], mla_w_ukv[l])
        heads = jnp.concatenate([rmsnorm(o_attn, out_g_attn[l]),
                                 rmsnorm(o_ssm, out_g_ssm[l]),
                                 rmsnorm(o_mla, out_g_mla[l])], axis=-1)
        x = x + heads @ w_out[l]
        x = x + hier_moe(rmsnorm(x, ln2_g[l]), w_router_group[l], b_router_group[l],
                         w_router_expert[l], b_router_expert[l], w_gate[l], w_up[l], w_down[l])
    return rmsnorm(x, final_g)
```

```python
import math
from contextlib import ExitStack

import numpy as np
import concourse.bass as bass
import concourse.mybir as mybir
from concourse.bass_utils import run_bass_kernel_spmd

F32 = mybir.dt.float32
BF16 = mybir.dt.bfloat16
I32 = mybir.dt.int32
AF = mybir.ActivationFunctionType
ALU = mybir.AluOpType
AX = mybir.AxisListType

D = 1024
HD = 64
IN_COLS = 1248
EPS = 1e-6
PI = math.pi


class Buf:
    __slots__ = ("last_w", "readers")

    def __init__(self):
        self.last_w = None
        self.readers = {}


class Trk:
    def __init__(self, nc, es):
        self.nc = nc
        self.eng = {"pe": nc.tensor, "act": nc.scalar, "dve": nc.vector, "pool": nc.gpsimd, "sync": nc.sync}
        self.sem = {}
        self.cnt = {}
        self.waited = {k: {} for k in self.eng}
        for k in ("pe", "act", "dve", "pool"):
            self.sem[k] = es.enter_context(nc.semaphore("s_" + k))
            self.cnt[k] = 0
        self.lanes = {}
        self.lane_sem = {}
        self.lane_i = {}
        for q, n in {"sync": 16, "act": 8, "pool": 8}.items():
            self.lanes[q] = []
            for i in range(n):
                nm = "%s%d" % (q, i)
                s = es.enter_context(nc.semaphore("l_" + nm))
                self.lanes[q].append([s, 0, nm])
                self.lane_sem[nm] = s
            self.lane_i[q] = 0
        self.bufs = {}

    def b(self, *key):
        v = self.bufs.get(key)
        if v is None:
            v = self.bufs[key] = Buf()
        return v

    def _wait(self, e, tok):
        kind, key, val = tok
        w = self.waited[e]
        if w.get(key, 0) >= val:
            return
        w[key] = val
        sem = self.sem[key] if kind == "e" else self.lane_sem[key]
        self.eng[e].wait_ge(sem, val)

    def _deps(self, e, reads, writes):
        deps = []
        for b in reads:
            if b.last_w is not None:
                deps.append(b.last_w)
        for b in writes:
            lw = b.last_w
            if lw is not None and not (e == "pe" and lw[0] == "e" and lw[1] == e):
                deps.append(lw)
            for t in b.readers.values():
                if e == "pe" and t[0] == "e" and t[1] == e:
                    continue
                deps.append(t)
        for d in deps:
            self._wait(e, d)

    def _record(self, tok, reads, writes):
        for b in reads:
            b.readers[tok[1]] = tok
        for b in writes:
            b.last_w = tok
            b.readers = {}

    def op(self, e, fn, reads=(), writes=(), inc=True):
        self._deps(e, reads, writes)
        ins = fn(self.eng[e])
        if inc:
            self.cnt[e] += 1
            ins.then_inc(self.sem[e], 1)
            tok = ("e", e, self.cnt[e])
        else:
            tok = ("e", e, self.cnt[e] + 1)
        self._record(tok, reads, writes)
        return ins

    def dma(self, q, out, in_, reads=(), writes=(), **kw):
        lanes = self.lanes[q]
        i = self.lane_i[q]
        self.lane_i[q] = (i + 1) % len(lanes)
        lane = lanes[i]
        if lane[1] > 0:
            self._wait(q, ("d", lane[2], lane[1]))
        self._deps(q, reads, writes)
        ins = self.eng[q].dma_start(out=out, in_=in_, **kw)
        lane[1] += 16
        ins.then_inc(lane[0], 16)
        self._record(("d", lane[2], lane[1]), reads, writes)
        return ins

    def barrier(self):
        for e in self.eng:
            for k in self.sem:
                if k != e and self.cnt[k] > 0:
                    self._wait(e, ("e", k, self.cnt[k]))
            for q, lanes in self.lanes.items():
                for lane in lanes:
                    if lane[1] > 0:
                        self._wait(e, ("d", lane[2], lane[1]))

    def finish(self, bufs, e="sync"):
        for b in bufs:
            if b.last_w is not None:
                self._wait(e, b.last_w)


INPUT_SHAPES = {
    "ln1_g": [2, 1024], "w_in": [2, 1024, 1248], "attn_sink": [2, 6],
    "ssm_lam_re": [2, 2, 16, 64], "ssm_lam_im": [2, 2, 16, 64], "ssm_log_dt": [2, 2, 16],
    "ssm_b_re": [2, 2, 16, 64, 16], "ssm_b_im": [2, 2, 16, 64, 16],
    "ssm_c_re": [2, 2, 16, 16, 64], "ssm_c_im": [2, 2, 16, 16, 64],
    "ssm_d": [2, 256], "ssm_w_glu": [2, 256, 512], "ssm_b_glu": [2, 512],
    "mla_q_norm_g": [2, 192], "mla_w_uq": [2, 192, 576], "mla_kv_norm_g": [2, 128], "mla_w_ukv": [2, 128, 768],
    "out_g_attn": [2, 384], "out_g_ssm": [2, 256], "out_g_mla": [2, 384], "w_out": [2, 1024, 1024],
    "ln2_g": [2, 1024], "w_router_group": [2, 1024, 4], "b_router_group": [2, 4],
    "w_router_expert": [2, 1024, 32], "b_router_expert": [2, 32],
    "w_gate": [2, 4, 8, 1024, 256], "w_up": [2, 4, 8, 1024, 256], "w_down": [2, 4, 8, 256, 1024],
    "final_g": [1024],
}


class MK:
    def __init__(self, T=4096, depth=2, phases=None, debug=()):
        self.T = T
        self.NT = T // 128
        self.NCH = T // 512
        self.depth = depth
        self.debug = set(debug)
        self.phases = phases
        self.nc = nc = bass.Bass("TRN2", target_bir_lowering=False)
        self.es = ExitStack()
        self.trk = Trk(nc, self.es)
        self.din = {}
        self.din["x"] = nc.dram_tensor("x", [T, D], F32, kind="ExternalInput").ap()
        for k, shp in INPUT_SHAPES.items():
            self.din[k] = nc.dram_tensor(k, shp, F32, kind="ExternalInput").ap()
        self.out = nc.dram_tensor("out", [T, D], F32, kind="ExternalOutput").ap()
        self.dbg_out = []
        self.uid = 0
        self.HT = self.dram("HT", [D, T], BF16)
        self.XN2T = self.dram("XN2T", [D, T], BF16)
        self.XR = self.dram("XR", [T, D], F32)

    def sb(self, es, name, shape, dt):
        self.uid += 1
        return es.enter_context(self.nc.sbuf_tensor("%s_%d" % (name, self.uid), list(shape), dt))

    def ps(self, es, name, shape, dt):
        self.uid += 1
        return es.enter_context(self.nc.psum_tensor("%s_%d" % (name, self.uid), list(shape), dt))

    def dram(self, name, shape, dt):
        return self.nc.dram_tensor(name, list(shape), dt, kind="Internal").ap()

    def dump(self, name, ap, shape, dt, bufs):
        if name not in self.debug:
            return
        o = self.nc.dram_tensor("dbg_" + name, list(shape), dt, kind="ExternalOutput").ap()
        ob = Buf()
        self.trk.dma("sync", o, ap, reads=bufs, writes=[ob])
        self.dbg_out.append(ob)

    def dump_dram(self, name, ap, shape, dt):
        o = self.nc.dram_tensor("dbg_" + name, list(shape), dt, kind="ExternalOutput").ap()
        with ExitStack() as es:
            rows = shape[0]
            t = self.sb(es, "dd", [128, shape[1]], dt)
            for r0 in range(0, rows, 128):
                b1, ob = Buf(), Buf()
                n = min(128, rows - r0)
                self.trk.barrier()
                self.trk.dma("sync", t[0:n, :], ap[r0:r0 + n, :], writes=[b1])
                self.trk.dma("sync", o[r0:r0 + n, :], t[0:n, :], reads=[b1], writes=[ob])
                self.dbg_out.append(ob)
            self.trk.finish(self.dbg_out)
            self.trk.barrier()

    def range_reduce(self, es, ang, shape, bufs, eng="dve", scratch=None):
        Tk = self.trk
        if scratch is None:
            it = self.sb(es, "rr_i", shape, I32)
            kt = self.sb(es, "rr_k", shape, F32)
            bi, bk = Buf(), Buf()
        else:
            it, kt, bi, bk = scratch
        sl = tuple(slice(None) for _ in shape)
        Tk.op(eng, lambda e: e.tensor_scalar(out=it[sl], in0=ang, scalar1=float(1 / (2 * PI)), scalar2=None, op0=ALU.mult), reads=bufs, writes=[bi])
        Tk.op(eng, lambda e: e.tensor_copy(out=kt[sl], in_=it[sl]), reads=[bi], writes=[bk])
        Tk.op(eng, lambda e: e.scalar_tensor_tensor(out=ang, in0=kt[sl], scalar=float(-2 * PI), in1=ang, op0=ALU.mult, op1=ALU.add), reads=[bk] + bufs, writes=bufs)
        Tk.op(eng, lambda e: e.tensor_scalar(out=kt[sl], in0=ang, scalar1=float(PI), scalar2=float(-2 * PI), op0=ALU.is_gt, op1=ALU.mult), reads=bufs, writes=[bk])
        Tk.op(eng, lambda e: e.tensor_tensor(out=ang, in0=ang, in1=kt[sl], op=ALU.add), reads=[bk] + bufs, writes=bufs)
        Tk.op(eng, lambda e: e.tensor_scalar(out=kt[sl], in0=ang, scalar1=float(-PI), scalar2=float(2 * PI), op0=ALU.is_lt, op1=ALU.mult), reads=bufs, writes=[bk])
        Tk.op(eng, lambda e: e.tensor_tensor(out=ang, in0=ang, in1=kt[sl], op=ALU.add), reads=[bk] + bufs, writes=bufs)

    def consts(self):
        nc, Tk, es, T = self.nc, self.trk, self.es, self.T
        self.ident_bf = self.sb(es, "ident_bf", [128, 128], BF16)
        self.ident_f = self.sb(es, "ident_f", [128, 128], F32)
        self.ones_bf = self.sb(es, "ones_bf", [128, 128], BF16)
        self.b_const = Buf()
        bc = self.b_const
        for t in (self.ident_bf, self.ident_f):
            Tk.op("pool", lambda e, t=t: e.memset(t[:], 1.0), writes=[bc])
            Tk.op("pool", lambda e, t=t: e.affine_select(out=t[:], in_=t[:], pattern=[[-1, 128]], compare_op=ALU.is_equal, fill=0.0, base=0, channel_multiplier=1), reads=[bc], writes=[bc])
        Tk.op("pool", lambda e: e.memset(self.ones_bf[:], 1.0), writes=[bc])
        self.ROPE = self.dram("ROPE", [2, 32, T], F32)
        self.b_rope = Buf()
        with ExitStack() as tes:
            self.rope_cos = self.sb(tes, "rope_cos", [128, T], F32)
            self.rope_sin = self.sb(tes, "rope_sin", [128, T], F32)
            pi_ = self.sb(tes, "pi", [128, 1], I32)
            pf = self.sb(tes, "pf", [128, 1], F32)
            qi = self.sb(tes, "qi", [128, 1], I32)
            qf = self.sb(tes, "qf", [128, 1], F32)
            inv = self.sb(tes, "inv", [128, 1], F32)
            ti = self.sb(tes, "ti", [128, T], I32)
            tf = self.sb(tes, "tf", [128, T], F32)
            ang = self.sb(tes, "ang", [128, T], F32)
            b1, b2, b3 = Buf(), Buf(), Buf()
            Tk.op("pool", lambda e: e.iota(pi_[:], pattern=[[0, 1]], base=0, channel_multiplier=1), writes=[b1])
            Tk.op("dve", lambda e: e.tensor_copy(out=pf[:], in_=pi_[:]), reads=[b1], writes=[b1])
            Tk.op("dve", lambda e: e.tensor_scalar(out=qi[:], in0=pf[:], scalar1=-7.5, scalar2=1.0 / 16, op0=ALU.add, op1=ALU.mult), reads=[b1], writes=[b2])
            Tk.op("dve", lambda e: e.tensor_copy(out=qf[:], in_=qi[:]), reads=[b2], writes=[b2])
            Tk.op("dve", lambda e: e.scalar_tensor_tensor(out=pf[:], in0=qf[:], scalar=-16.0, in1=pf[:], op0=ALU.mult, op1=ALU.add), reads=[b1, b2], writes=[b1])
            Tk.op("act", lambda e: e.activation(out=inv[:], in_=pf[:], func=AF.Exp, scale=float(-math.log(10000.0) / 16)), reads=[b1], writes=[b3])
            Tk.op("pool", lambda e: e.iota(ti[:], pattern=[[1, T]], base=0, channel_multiplier=0), writes=[b2])
            Tk.op("dve", lambda e: e.tensor_copy(out=tf[:], in_=ti[:]), reads=[b2], writes=[b2])
            bang = Buf()
            rrs = (self.sb(tes, "rr_i", [128, T], I32), self.sb(tes, "rr_k", [128, T], F32), Buf(), Buf())
            for tab, shift in ((self.rope_sin, 0.0), (self.rope_cos, PI / 2)):
                Tk.op("dve", lambda e: e.tensor_scalar(out=ang[:], in0=tf[:], scalar1=inv[:, 0:1], scalar2=float(shift), op0=ALU.mult, op1=ALU.add), reads=[b2, b3], writes=[bang])
                self.range_reduce(tes, ang[:], [128, T], [bang], scratch=rrs)
                Tk.op("act", lambda e, tab=tab: e.activation(out=tab[:], in_=ang[:], func=AF.Sin), reads=[bang], writes=[self.b_rope])
            bt = self.b_rope
            self.b_rope = Buf()
            Tk.dma("sync", self.ROPE[0], self.rope_cos[64:96, :], reads=[bt], writes=[self.b_rope])
            Tk.dma("sync", self.ROPE[1], self.rope_sin[64:96, :], reads=[bt], writes=[self.b_rope])
            Tk.barrier()
        self.GATE = self.sb(es, "GATE", [128, self.NT, 32], F32)

    def p1(self, l, xsrc, xbuf_key):
        nc, Tk, T, NCH = self.nc, self.trk, self.T, self.NCH
        m = self.mix
        with ExitStack() as es:
            w_in_bf = self.sb(es, "w_in_bf", [128, 8, IN_COLS], BF16)
            w_qa = self.sb(es, "w_qa", [128, 8, 384], BF16)
            w_kr = self.sb(es, "w_kr", [128, 8, 96], BF16)
            w_sw = self.sb(es, "w_sw", [128, 8, 96], BF16)
            g1bc = self.sb(es, "g1bc", [128, D], F32)
            xt = [self.sb(es, "xt", [128, D], F32) for _ in range(2)]
            junk = self.sb(es, "junk", [128, D], BF16)
            xn = [self.sb(es, "xn", [128, D], BF16) for _ in range(2)]
            xnT = [self.sb(es, "xnT", [128, 8, 512], BF16) for _ in range(2)]
            ss = [self.sb(es, "ss", [128, 1], F32) for _ in range(2)]
            rstd = [self.sb(es, "rstd", [128, 1], F32) for _ in range(2)]
            kt1 = self.sb(es, "kt1", [128, 512], F32)
            kt2 = self.sb(es, "kt2", [128, 512], F32)
            rp = [self.sb(es, "rp", [128, 2, 512], F32) for _ in range(2)]
            brp = [Buf(), Buf()]
            pT = [self.ps(es, "pT", [128, 1024], BF16) for _ in range(2)]
            pm = [self.ps(es, "pm", [128, 512], F32) for _ in range(4)]
            pva = self.ps(es, "pva", [128, 512], F32)
            bw = Buf()
            bg = Buf()
            Tk.dma("pool", w_in_bf[:], self.din["w_in"][l].rearrange("(kc p) n -> p kc n", p=128), writes=[bw])
            Tk.dma("sync", g1bc[:], self.din["ln1_g"][l:l + 1, :].partition_broadcast(128), writes=[bg])
            bw2 = Buf()
            for j, h in enumerate([0, 3, 1, 4, 2, 5]):
                Tk.op("pool", lambda e, j=j, h=h: e.tensor_copy(out=w_qa[:, :, j * 64:(j + 1) * 64], in_=w_in_bf[:, :, h * 64:(h + 1) * 64]), reads=[bw], writes=[bw2])
            Tk.op("pool", lambda e: e.memset(w_kr[:, :, 0:64], 0.0), writes=[bw2])
            Tk.op("pool", lambda e: e.memset(w_sw[:, :, 0:64], 0.0), writes=[bw2])
            Tk.op("pool", lambda e: e.tensor_copy(out=w_kr[:, :, 64:96], in_=w_in_bf[:, :, 1216:1248]), reads=[bw], writes=[bw2])
            Tk.op("act", lambda e: e.mul(out=w_sw[:, :, 64:80], in_=w_in_bf[:, :, 1232:1248], mul=-1.0), reads=[bw], writes=[bw2])
            Tk.op("act", lambda e: e.copy(out=w_sw[:, :, 80:96], in_=w_in_bf[:, :, 1216:1232]), reads=[bw], writes=[bw2])
            Tk.op("pool", lambda e: e.memset(m["VA"][:, :, :, 64:65], 1.0), writes=[Tk.b("VAones", l)])
            bxt = [Buf(), Buf()]
            bxn = [Buf(), Buf()]
            bss = [Buf(), Buf()]
            bxnT = [Buf(), Buf()]
            bpT = [Buf(), Buf()]
            bpm = [Buf() for _ in range(4)]
            bpva = Buf()
            bjunk = Buf()
            bkt = Buf()
            ev = [0]

            def evac(out, in_, reads, writes):
                e = ("act", "dve")[ev[0] % 2]
                ev[0] += 1
                if e == "act":
                    Tk.op("act", lambda en: en.copy(out=out, in_=in_), reads=reads, writes=writes)
                else:
                    Tk.op("dve", lambda en: en.tensor_copy(out=out, in_=in_), reads=reads, writes=writes)

            pmi = [0]
            for c in range(NCH):
                cb = c % 2
                cols = slice(c * 512, (c + 1) * 512)
                for tt in range(4):
                    i = 4 * c + tt
                    p = i % 2
                    Tk.dma("sync", xt[p][:], xsrc[i * 128:(i + 1) * 128, :], reads=[Tk.b(xbuf_key, i)], writes=[bxt[p]])
                    Tk.op("dve", lambda e: e.memset(ss[p][:], 0.0), writes=[bss[p]])
                    Tk.op("act", lambda e: e.activation(out=junk[:], in_=xt[p][:], func=AF.Square, accum_out=ss[p][:]), reads=[bxt[p], bss[p]], writes=[bjunk, bss[p]])
                    Tk.op("dve", lambda e: e.tensor_scalar(out=ss[p][:], in0=ss[p][:], scalar1=1.0 / D, scalar2=EPS, op0=ALU.mult, op1=ALU.add), reads=[bss[p]], writes=[bss[p]])
                    Tk.op("act", lambda e: e.activation(out=ss[p][:], in_=ss[p][:], func=AF.Sqrt), reads=[bss[p]], writes=[bss[p]])
                    Tk.op("dve", lambda e: e.reciprocal(out=rstd[p][:], in_=ss[p][:]), reads=[bss[p]], writes=[bss[p]])
                    Tk.op("dve", lambda e: e.scalar_tensor_tensor(out=xn[p][:], in0=xt[p][:], scalar=rstd[p][:, 0:1], in1=g1bc[:], op0=ALU.mult, op1=ALU.mult), reads=[bxt[p], bss[p], bg], writes=[bxn[p]])
                    for kc in range(8):
                        Tk.op("pe", lambda e, kc=kc: e.transpose(pT[p][:, kc * 128:(kc + 1) * 128], xn[p][:, kc * 128:(kc + 1) * 128], self.ident_bf[:]),
                              reads=[bxn[p], self.b_const], writes=[bpT[p]], inc=(kc == 7))
                    evac(xnT[cb][:, :, tt * 128:(tt + 1) * 128], pT[p][:].rearrange("p (k t) -> p k t", k=8), [bpT[p]], [bxnT[cb]])
                for v in range(2):
                    Tk.dma("sync", rp[cb][64:96, v, :], self.ROPE[v, :, cols], reads=[self.b_rope], writes=[brp[cb]])
                groups = [
                    (w_qa, 0, 128, ("QA", 0)), (w_qa, 128, 128, ("QA", 1)), (w_qa, 256, 128, ("QA", 2)),
                    (w_in_bf, 384, 128, ("KA", None)),
                    (w_in_bf, 640, 128, ("U", 0)), (w_in_bf, 768, 128, ("U", 1)),
                    (w_in_bf, 896, 128, ("CQ", 0)), (w_in_bf, 1024, 64, ("CQ", 1)),
                    (w_in_bf, 1088, 128, ("CKV", None)),
                    (w_kr, 0, 96, ("KR", "main")), (w_sw, 0, 96, ("KR", "swap")),
                ]
                for (wt, c0, M, (name, sub)) in groups:
                    k = pmi[0] % 4
                    pmi[0] += 1
                    for kc in range(8):
                        Tk.op("pe", lambda e, kc=kc: e.matmul(pm[k][0:M, :], lhsT=wt[:, kc, c0:c0 + M], rhs=xnT[cb][:, kc, :], start=(kc == 0), stop=(kc == 7)),
                              reads=[bw, bw2, bxnT[cb]], writes=[bpm[k]], inc=(kc == 7))
                    if name == "KR":
                        rows = slice(64, 96)
                        if sub == "main":
                            Tk.op("dve", lambda e: e.tensor_tensor(out=kt1[rows, :], in0=pm[k][rows, :], in1=rp[cb][rows, 0, :], op=ALU.mult), reads=[bpm[k], brp[cb]], writes=[bkt])
                        else:
                            Tk.op("dve", lambda e: e.tensor_tensor(out=kt2[rows, :], in0=pm[k][rows, :], in1=rp[cb][rows, 1, :], op=ALU.mult), reads=[bpm[k], brp[cb]], writes=[bkt])
                            Tk.op("dve", lambda e: e.tensor_tensor(out=m["KR"][rows, cols], in0=kt1[rows, :], in1=kt2[rows, :], op=ALU.add), reads=[bkt], writes=[Tk.b("KR", l, c)])
                    else:
                        dst = m[name]
                        o = dst[0:M, cols] if sub is None else dst[0:M, sub, cols]
                        evac(o, pm[k][0:M, :], [bpm[k]], [Tk.b(name, l, c)])
                for tt in range(4):
                    i = 4 * c + tt
                    for kc in range(8):
                        Tk.op("pe", lambda e, kc=kc: e.matmul(pva[:, tt * 128:(tt + 1) * 128], lhsT=xnT[cb][:, kc, tt * 128:(tt + 1) * 128], rhs=w_in_bf[:, kc, 512:640], start=(kc == 0), stop=(kc == 7)),
                              reads=[bw, bxnT[cb]], writes=[bpva], inc=(kc == 7))
                    Tk.op("dve", lambda e: e.tensor_copy(out=m["VA"][:, i, :, 0:64], in_=pva[:, tt * 128:(tt + 1) * 128].rearrange("p (h d) -> p h d", h=2)), reads=[bpva], writes=[Tk.b("VA", l, i)])
            Tk.barrier()

    def alloc_mix(self, es_list):
        T, NT = self.T, self.NT
        m = self.mix = {}
        es_mla, es_u, es_attn = es_list
        m["CQ"] = self.sb(es_mla, "CQ", [128, 2, T], BF16)
        m["CKV"] = self.sb(es_mla, "CKV", [128, T], BF16)
        m["KR"] = self.sb(es_mla, "KR", [128, T], BF16)
        m["U"] = self.sb(es_u, "U", [128, 2, T], BF16)
        m["QA"] = self.sb(es_attn, "QA", [128, 3, T], BF16)
        m["KA"] = self.sb(es_attn, "KA", [128, T], BF16)
        m["VA"] = self.sb(es_attn, "VA", [128, NT, 2, 65], BF16)

    def build(self):
        Tk = self.trk
        self.consts()
        xsrc, xkey = self.din["x"], "xin"
        for l in range(self.depth):
            es_mla, es_u, es_attn = ExitStack(), ExitStack(), ExitStack()
            self.alloc_mix([es_mla, es_u, es_attn])
            self.p1(l, xsrc, xkey)
            if "p1" in self.debug:
                m, T, NT = self.mix, self.T, self.NT
                allb = list(Tk.bufs.values())
                self.debug |= {"QA", "KA", "VA", "U", "CQ", "CKV", "KR"}
                self.dump("QA", m["QA"][:], [128, 3, T], BF16, allb)
                self.dump("KA", m["KA"][:], [128, T], BF16, allb)
                self.dump("VA", m["VA"][:], [128, NT, 2, 65], BF16, allb)
                self.dump("U", m["U"][:], [128, 2, T], BF16, allb)
                self.dump("CQ", m["CQ"][:], [128, 2, T], BF16, allb)
                self.dump("CKV", m["CKV"][:], [128, T], BF16, allb)
                self.dump("KR", m["KR"][:], [128, T], BF16, allb)
            if self.phases == "p1":
                Tk.barrier()
                es_attn.close(); es_u.close(); es_mla.close()
                break
            self.p2(l)
            es_attn.close()
            if self.phases == "p2":
                self.dump_dram("HT", self.HT, [D, self.T], BF16)
                es_u.close(); es_mla.close()
                break
            self.p3(l)
            es_u.close()
            if self.phases == "p3":
                self.dump_dram("HT", self.HT, [D, self.T], BF16)
                es_mla.close()
                break
            self.p4(l)
            es_mla.close()
            if self.phases == "p4":
                self.dump_dram("HT", self.HT, [D, self.T], BF16)
                break
            self.p5(l, xsrc, xkey)
            if self.phases == "p5":
                self.dump("GATE", self.GATE[:], [128, self.NT, 32], F32, list(Tk.bufs.values()))
                self.dump_dram("XR", self.XR, [self.T, D], F32)
                break
            self.p6(l, last=(l == self.depth - 1))
            xsrc, xkey = self.XR, "XR"
        Tk.finish(self.dbg_out)
        Tk.finish([b for k, b in Tk.bufs.items() if k[0] == "OUT"])
        Tk.barrier()
        self.es.close()
        return self.nc


def _p2(self, l):
    nc, Tk, T, NT = self.nc, self.trk, self.T, self.NT
    m = self.mix
    QA, KA, VA = m["QA"], m["KA"], m["VA"]
    slopes = [2.0 ** (-8.0 * (h + 1) / 6) for h in range(6)]
    with ExitStack() as es:
        bias = self.sb(es, "bias", [128, 6, 384], F32)
        di = self.sb(es, "di", [128, 384], I32)
        df = self.sb(es, "df", [128, 384], F32)
        dn = self.sb(es, "dn", [128, 384], F32)
        pen = self.sb(es, "pen", [128, 384], F32)
        esk = self.sb(es, "esk", [128, 6], F32)
        gat = self.sb(es, "gat", [64, 6], F32)
        ones_f = self.sb(es, "ones_f", [128, 64], F32)
        NB = 3
        tmp = [self.sb(es, "tmp", [128, 384], F32) for _ in range(NB)]
        PT = [self.sb(es, "PT", [128, 384], BF16) for _ in range(NB)]
        Osb = self.sb(es, "Osb", [65, 768], F32)
        rd = self.sb(es, "rd", [65, 768], F32)
        rdh = self.sb(es, "rdh", [65, 768], F32)
        rdb = self.sb(es, "rdb", [65, 2, 768], BF16)
        brdh, brdb = Buf(), Buf()
        o = self.sb(es, "o", [64, 768], F32)
        sq = self.sb(es, "sq", [64, 768], BF16)
        rs = self.sb(es, "rs", [64, 128], F32)
        hT = [self.sb(es, "hT", [64, 6, 128], BF16) for _ in range(2)]
        pS = [self.ps(es, "pS", [128, 512], F32) for _ in range(NB)]
        pO = [self.ps(es, "pO", [128, 512], F32) for _ in range(2)]
        pB = [self.ps(es, "pB", [128, 512], F32) for _ in range(2)]
        pSS = self.ps(es, "pSS", [128, 512], F32)
        bb = Buf()
        Tk.op("pool", lambda e: e.iota(di[:].rearrange("p (a q) -> p a q", a=3), pattern=[[128, 3], [-1, 128]], base=-128, channel_multiplier=1), writes=[bb])
        Tk.op("dve", lambda e: e.tensor_copy(out=df[:], in_=di[:]), reads=[bb], writes=[bb])
        Tk.op("dve", lambda e: e.tensor_scalar(out=dn[:], in0=df[:], scalar1=-1.0, scalar2=None, op0=ALU.mult), reads=[bb], writes=[bb])
        Tk.op("dve", lambda e: e.tensor_tensor(out=df[:], in0=df[:], in1=dn[:], op=ALU.max), reads=[bb], writes=[bb])
        Tk.op("dve", lambda e: e.tensor_scalar(out=pen[:], in0=df[:], scalar1=128.0, scalar2=-30000.0, op0=ALU.is_gt, op1=ALU.mult), reads=[bb], writes=[bb])
        for h in range(6):
            Tk.op("dve", lambda e: e.scalar_tensor_tensor(out=bias[:, h, :], in0=df[:], scalar=float(-slopes[h]), in1=pen[:], op0=ALU.mult, op1=ALU.add), reads=[bb], writes=[bb])
        Tk.op("pool", lambda e: e.memset(ones_f[:], 1.0), writes=[bb])
        Tk.dma("sync", esk[64:65, :], self.din["attn_sink"][l:l + 1, :], writes=[bb])
        Tk.op("act", lambda e: e.activation(out=esk[64:65, :], in_=esk[64:65, :], func=AF.Exp), reads=[bb], writes=[bb])
        Tk.dma("sync", gat[:], self.din["out_g_attn"][l].rearrange("(h d) -> d h", d=64), writes=[bb], allow_slow_non_contiguous=True)
        allin = [Tk.b(n, l, c) for n in ("QA", "KA") for c in range(self.NCH)] + [Tk.b("VA", l, i) for i in range(NT)] + [Tk.b("VAones", l)]
        btmp, bPT, bpS = [[Buf() for _ in range(NB)] for _ in range(3)]
        bpO, bOsb, brd, bpB, bo, bsq, bpSS, brs = [Buf() for _ in range(8)]
        bhT = [Buf(), Buf()]
        steps = [(i, h) for i in range(NT) for h in range(6)]

        def geo(i):
            dds = [dd for dd in range(3) if 0 <= i + dd - 1 < NT]
            return dds, dds[0] * 128, (dds[-1] + 1) * 128

        def stS(n):
            i, h = steps[n]
            kv, j = h // 3, h % 3
            rows = slice(kv * 64, kv * 64 + 64)
            k = n % NB
            dds, c0, c1 = geo(i)
            qs = slice(i * 128, (i + 1) * 128)
            for dd in dds:
                ks = slice((i + dd - 1) * 128, (i + dd) * 128)
                Tk.op("pe", lambda e: e.matmul(pS[k][:, dd * 128:(dd + 1) * 128], lhsT=KA[rows, ks], rhs=QA[rows, j, qs], start=True, stop=True),
                      reads=allin, writes=[bpS[k]], inc=(dd == dds[-1]))

        def stP(n):
            i, h = steps[n]
            kv = h // 3
            k = n % NB
            dds, c0, c1 = geo(i)
            Tk.op("dve", lambda e: e.scalar_tensor_tensor(out=tmp[k][:, c0:c1], in0=pS[k][:, c0:c1], scalar=0.125, in1=bias[:, h, c0:c1], op0=ALU.mult, op1=ALU.add),
                  reads=[bpS[k], bb], writes=[btmp[k]])
            Tk.op("act", lambda e: e.activation(out=PT[k][:, c0:c1], in_=tmp[k][:, c0:c1], func=AF.Exp), reads=[btmp[k]], writes=[bPT[k]])
            po = pO[h // 4][0:65, (h % 4) * 128:(h % 4 + 1) * 128]
            for dd in dds:
                Tk.op("pe", lambda e: e.matmul(po, lhsT=VA[:, i + dd - 1, kv, :], rhs=PT[k][:, dd * 128:(dd + 1) * 128], start=(dd == dds[0]), stop=(dd == dds[-1])),
                      reads=allin + [bPT[k]], writes=[bpO], inc=(dd == dds[-1]))

        def epilogue(i):
            qs = slice(i * 128, (i + 1) * 128)
            hb = i % 2
            o3 = o[:].rearrange("p (h q) -> p h q", h=6)
            g = [[] for _ in range(6)]
            g[0].append(lambda: Tk.op("act", lambda e: e.copy(out=Osb[0:65, 0:512], in_=pO[0][0:65, :]), reads=[bpO], writes=[bOsb]))
            g[0].append(lambda: Tk.op("act", lambda e: e.copy(out=Osb[0:65, 512:768], in_=pO[1][0:65, 0:256]), reads=[bpO], writes=[bOsb]))
            g[1].append(lambda: Tk.op("dve", lambda e: e.tensor_tensor(out=rd[64:65, :].rearrange("p (h q) -> p h q", h=6), in0=Osb[64:65, :].rearrange("p (h q) -> p h q", h=6),
                                                                       in1=esk[64:65, :].unsqueeze(2).to_broadcast([1, 6, 128]), op=ALU.add), reads=[bOsb, bb], writes=[brd]))
            g[1].append(lambda: Tk.op("act", lambda e: e.activation(out=rd[64:65, :], in_=rd[64:65, :], func=AF.Ln), reads=[brd], writes=[brd]))
            g[1].append(lambda: Tk.op("act", lambda e: e.activation(out=rdh[64:65, :], in_=rd[64:65, :], func=AF.Exp, scale=-1.0), reads=[brd], writes=[brdh]))
            g[2].append(lambda: Tk.op("act", lambda e: e.copy(out=rdb[64:65, 0, :], in_=rdh[64:65, :]), reads=[brdh], writes=[brdb]))
            g[2].append(lambda: Tk.op("dve", lambda e: e.tensor_tensor(out=rdb[64:65, 1, :], in0=rdh[64:65, :], in1=rdb[64:65, 0, :], op=ALU.subtract), reads=[brdh, brdb], writes=[brdb]))

            def bcast():
                for a, (pb, c0, c1) in enumerate(((pB[0], 0, 512), (pB[1], 512, 768))):
                    for v in range(2):
                        Tk.op("pe", lambda e: e.matmul(pb[0:64, 0:c1 - c0], lhsT=self.ones_bf[64:65, 0:64], rhs=rdb[64:65, v, c0:c1], start=(v == 0), stop=(v == 1)), reads=[brdb, self.b_const], writes=[bpB], inc=(v == 1))
            g[3].append(bcast)
            g[3].append(lambda: Tk.op("dve", lambda e: e.tensor_tensor(out=o[:, 0:512], in0=Osb[0:64, 0:512], in1=pB[0][0:64, :], op=ALU.mult), reads=[bOsb, bpB], writes=[bo]))
            g[3].append(lambda: Tk.op("dve", lambda e: e.tensor_tensor(out=o[:, 512:768], in0=Osb[0:64, 512:768], in1=pB[1][0:64, 0:256], op=ALU.mult), reads=[bOsb, bpB], writes=[bo]))
            g[3].append(lambda: Tk.op("act", lambda e: e.activation(out=sq[:], in_=o[:], func=AF.Square), reads=[bo], writes=[bsq]))

            def ssq():
                for h in range(6):
                    Tk.op("pe", lambda e: e.matmul(pSS[0:64, 0:128], lhsT=self.ones_bf[0:64, 0:64], rhs=sq[:, h * 128:(h + 1) * 128], start=(h == 0), stop=(h == 5)),
                          reads=[bsq, self.b_const], writes=[bpSS], inc=(h == 5))
            g[4].append(ssq)
            g[4].append(lambda: Tk.op("dve", lambda e: e.tensor_scalar(out=rs[:], in0=pSS[0:64, 0:128], scalar1=1.0 / 384, scalar2=EPS, op0=ALU.mult, op1=ALU.add), reads=[bpSS], writes=[brs]))
            g[4].append(lambda: Tk.op("act", lambda e: e.activation(out=rs[:], in_=rs[:], func=AF.Ln), reads=[brs], writes=[brs]))
            g[5].append(lambda: Tk.op("act", lambda e: e.activation(out=rs[:], in_=rs[:], func=AF.Exp, scale=-0.5), reads=[brs], writes=[brs]))
            g[5].append(lambda: Tk.op("dve", lambda e: e.tensor_tensor(out=o3, in0=o3, in1=rs[:].unsqueeze(1).to_broadcast([64, 6, 128]), op=ALU.mult), reads=[bo, brs], writes=[bo]))
            g[5].append(lambda: Tk.op("dve", lambda e: e.tensor_tensor(out=hT[hb][:], in0=o3, in1=gat[:].unsqueeze(2).to_broadcast([64, 6, 128]), op=ALU.mult), reads=[bo, bb], writes=[bhT[hb]]))
            g[5].append(lambda: Tk.dma("sync", self.HT[0:384, qs].rearrange("(h d) q -> d h q", d=64), hT[hb][:], reads=[bhT[hb]], writes=[Tk.b("HT", l, "attn", i)]))
            return g

        NS_ = len(steps)
        pend = []
        for n in range(NS_ + 2):
            if n < NS_:
                stS(n)
            if n >= 2:
                stP(n - 2)
                i, h = steps[n - 2]
                if h == 5:
                    assert not pend
                    pend = epilogue(i)
                if pend:
                    for f in pend.pop(0):
                        f()
        while pend:
            for f in pend.pop(0):
                f()
        Tk.barrier()


MK.p2 = _p2


def rev_ap(a):
    ap = [list(d) for d in a.ap]
    step, n = ap[-1]
    ap[-1] = [-step, n]
    return bass.AP(a.tensor, a.offset + (n - 1) * step, ap)


def _p3(self, l):
    nc, Tk, T = self.nc, self.trk, self.T
    Lc = 128
    NC = T // Lc
    U = self.mix["U"]
    din = self.din
    allU = [Tk.b("U", l, c) for c in range(self.NCH)]
    with ExitStack() as es:
        CS = self.sb(es, "CS", [128, 32, 2 * Lc], F32)
        G1 = self.sb(es, "G1", [128, 32, 128], BF16)
        G2 = self.sb(es, "G2", [128, 32, 128], BF16)
        C1 = self.sb(es, "C1", [128, 32, 128], BF16)
        C2 = self.sb(es, "C2", [128, 32, 128], BF16)
        R = self.sb(es, "R", [128, 32, 128], F32)
        mag = self.sb(es, "mag", [128, 32], F32)
        diagD = self.sb(es, "diagD", [128, 2, 128], BF16)
        carry = self.sb(es, "carry", [128, 32], F32)
        bP = Buf()
        with ExitStack() as tes:
            xd = self.sb(tes, "xd", [32, 2, 128], F32)
            LR = self.sb(tes, "LR", [128, 32], F32)
            LI = self.sb(tes, "LI", [128, 32], F32)
            dt = self.sb(tes, "dt", [128, 32], F32)
            th = self.sb(tes, "th", [128, 32], F32)
            cth = self.sb(tes, "cth", [128, 32], F32)
            sth = self.sb(tes, "sth", [128, 32], F32)
            ar = self.sb(tes, "ar", [128, 32], F32)
            ai = self.sb(tes, "ai", [128, 32], F32)
            den = self.sb(tes, "den", [128, 32], F32)
            t1 = self.sb(tes, "t1", [128, 32], F32)
            t2 = self.sb(tes, "t2", [128, 32], F32)
            fr = self.sb(tes, "fr", [128, 32], F32)
            fi = self.sb(tes, "fi", [128, 32], F32)
            frs = self.sb(tes, "frs", [128, 32], F32)
            fis = self.sb(tes, "fis", [128, 32], F32)
            sgn = self.sb(tes, "sgn", [128, 1], F32)
            Pm = self.sb(tes, "Pm", [128, 32, 16], F32)
            Qm = self.sb(tes, "Qm", [128, 32, 16], F32)
            GT1 = self.sb(tes, "GT1", [128, 32, 16], F32)
            GT2 = self.sb(tes, "GT2", [128, 32, 16], F32)
            gtmp = self.sb(tes, "gtmp", [128, 32, 16], F32)
            TB = self.sb(tes, "TB", [128, 128], F32)
            rowmask = self.sb(tes, "rowmask", [128, 8], F32)
            CT = self.sb(tes, "CT", [128, 2, 128], F32)
            crl = self.sb(tes, "crl", [128, 64], F32)
            cil = self.sb(tes, "cil", [128, 64], F32)
            shift = self.sb(tes, "shift", [128, 128], F32)
            dcol = self.sb(tes, "dcol", [128, 2], F32)
            iot = self.sb(tes, "iot", [128, Lc], I32)
            iof = self.sb(tes, "iof", [128, Lc], F32)
            ang = self.sb(tes, "ang", [128, 8, Lc], F32)
            pp_ = self.ps(tes, "pprep", [128, 512], F32)
            b = Buf()
            bps = Buf()
            for name, dst in (("ssm_lam_re", LR), ("ssm_lam_im", LI)):
                src = din[name][l].rearrange("d g n -> (d g) n")
                Tk.dma("sync", xd[:, 0, 0:64], src, writes=[b])
                Tk.dma("sync", xd[:, 0, 64:128], src, writes=[b])
                Tk.op("pe", lambda e: e.matmul(pp_[:, 0:32], lhsT=xd[:, 0, :], rhs=self.ident_f[0:32, 0:32], start=True, stop=True), reads=[b, self.b_const], writes=[bps])
                Tk.op("dve", lambda e: e.tensor_copy(out=dst[:], in_=pp_[:, 0:32]), reads=[bps], writes=[b])
            Tk.dma("sync", dt[:], din["ssm_log_dt"][l:l + 1].rearrange("o d g -> o (d g)").partition_broadcast(128), writes=[b])
            Tk.op("act", lambda e: e.activation(out=dt[:], in_=dt[:], func=AF.Exp), reads=[b], writes=[b])
            Tk.op("dve", lambda e: e.tensor_tensor(out=t1[:], in0=LR[:], in1=dt[:], op=ALU.mult), reads=[b], writes=[b])
            Tk.op("act", lambda e: e.activation(out=mag[:], in_=t1[:], func=AF.Exp), reads=[b], writes=[bP])
            Tk.op("dve", lambda e: e.tensor_tensor(out=th[:], in0=LI[:], in1=dt[:], op=ALU.mult), reads=[b], writes=[b])
            rrs_s = (self.sb(tes, "rr_i", [128, 32], I32), self.sb(tes, "rr_k", [128, 32], F32), Buf(), Buf())
            for dst, shf in ((sth, 0.0), (cth, PI / 2)):
                Tk.op("dve", lambda e: e.tensor_scalar(out=t2[:], in0=th[:], scalar1=float(shf), scalar2=None, op0=ALU.add), reads=[b], writes=[b])
                self.range_reduce(tes, t2[:], [128, 32], [b], scratch=rrs_s)
                Tk.op("act", lambda e: e.activation(out=dst[:], in_=t2[:], func=AF.Sin), reads=[b], writes=[b])
            Tk.op("dve", lambda e: e.tensor_tensor(out=ar[:], in0=mag[:], in1=cth[:], op=ALU.mult), reads=[b, bP], writes=[b])
            Tk.op("dve", lambda e: e.tensor_tensor(out=ai[:], in0=mag[:], in1=sth[:], op=ALU.mult), reads=[b, bP], writes=[b])
            Tk.op("dve", lambda e: e.tensor_scalar(out=ar[:], in0=ar[:], scalar1=-1.0, scalar2=None, op0=ALU.add), reads=[b], writes=[b])
            Tk.op("dve", lambda e: e.tensor_tensor(out=den[:], in0=LR[:], in1=LR[:], op=ALU.mult), reads=[b], writes=[b])
            Tk.op("dve", lambda e: e.tensor_tensor(out=t1[:], in0=LI[:], in1=LI[:], op=ALU.mult), reads=[b], writes=[b])
            Tk.op("dve", lambda e: e.tensor_tensor(out=den[:], in0=den[:], in1=t1[:], op=ALU.add), reads=[b], writes=[b])
            Tk.op("dve", lambda e: e.reciprocal(out=den[:], in_=den[:]), reads=[b], writes=[b])
            Tk.op("dve", lambda e: e.tensor_tensor(out=t1[:], in0=ar[:], in1=LR[:], op=ALU.mult), reads=[b], writes=[b])
            Tk.op("dve", lambda e: e.tensor_tensor(out=t2[:], in0=ai[:], in1=LI[:], op=ALU.mult), reads=[b], writes=[b])
            Tk.op("dve", lambda e: e.tensor_tensor(out=t1[:], in0=t1[:], in1=t2[:], op=ALU.add), reads=[b], writes=[b])
            Tk.op("dve", lambda e: e.tensor_tensor(out=fr[:], in0=t1[:], in1=den[:], op=ALU.mult), reads=[b], writes=[b])
            Tk.op("dve", lambda e: e.tensor_tensor(out=t1[:], in0=ai[:], in1=LR[:], op=ALU.mult), reads=[b], writes=[b])
            Tk.op("dve", lambda e: e.tensor_tensor(out=t2[:], in0=ar[:], in1=LI[:], op=ALU.mult), reads=[b], writes=[b])
            Tk.op("dve", lambda e: e.tensor_tensor(out=t1[:], in0=t1[:], in1=t2[:], op=ALU.subtract), reads=[b], writes=[b])
            Tk.op("dve", lambda e: e.tensor_tensor(out=fi[:], in0=t1[:], in1=den[:], op=ALU.mult), reads=[b], writes=[b])
            Tk.op("pool", lambda e: e.memset(sgn[0:64, :], 1.0), writes=[b])
            Tk.op("pool", lambda e: e.memset(sgn[64:128, :], -1.0), writes=[b])
            Tk.op("dve", lambda e: e.tensor_scalar(out=frs[:], in0=fr[:], scalar1=sgn[:, 0:1], scalar2=None, op0=ALU.mult), reads=[b], writes=[b])
            Tk.op("dve", lambda e: e.tensor_scalar(out=fis[:], in0=fi[:], scalar1=sgn[:, 0:1], scalar2=-1.0, op0=ALU.mult, op1=ALU.mult), reads=[b], writes=[b])
            bre = din["ssm_b_re"][l].rearrange("d g n c -> n (d g) c")
            bim = din["ssm_b_im"][l].rearrange("d g n c -> n (d g) c")
            Tk.dma("sync", Pm[0:64], bre, writes=[b])
            Tk.dma("sync", Pm[64:128], bim, writes=[b])
            Tk.dma("sync", Qm[0:64], bim, writes=[b])
            Tk.dma("sync", Qm[64:128], bre, writes=[b])

            def bc3(t):
                return t[:].unsqueeze(2).to_broadcast([128, 32, 16])
            Tk.op("dve", lambda e: e.tensor_tensor(out=GT1[:], in0=Pm[:], in1=bc3(fr), op=ALU.mult), reads=[b], writes=[b])
            Tk.op("dve", lambda e: e.tensor_tensor(out=gtmp[:], in0=Qm[:], in1=bc3(fis), op=ALU.mult), reads=[b], writes=[b])
            Tk.op("dve", lambda e: e.tensor_tensor(out=GT1[:], in0=GT1[:], in1=gtmp[:], op=ALU.add), reads=[b], writes=[b])
            Tk.op("dve", lambda e: e.tensor_tensor(out=GT2[:], in0=Qm[:], in1=bc3(frs), op=ALU.mult), reads=[b], writes=[b])
            Tk.op("dve", lambda e: e.tensor_tensor(out=gtmp[:], in0=Pm[:], in1=bc3(fi), op=ALU.mult), reads=[b], writes=[b])
            Tk.op("dve", lambda e: e.tensor_tensor(out=GT2[:], in0=GT2[:], in1=gtmp[:], op=ALU.add), reads=[b], writes=[b])
            Tk.op("pool", lambda e: e.memset(rowmask[:], 1.0), writes=[b])
            Tk.op("pool", lambda e: e.affine_select(out=rowmask[:], in_=rowmask[:], pattern=[[-16, 8]], compare_op=ALU.is_ge, fill=0.0, base=0, channel_multiplier=1), reads=[b], writes=[b])
            Tk.op("pool", lambda e: e.affine_select(out=rowmask[:], in_=rowmask[:], pattern=[[16, 8]], compare_op=ALU.is_ge, fill=0.0, base=15, channel_multiplier=-1), reads=[b], writes=[b])
            for GT, Gd in ((GT1, G1), (GT2, G2)):
                for d in range(2):
                    for ct in range(2):
                        dg0 = d * 16 + ct * 8
                        Tk.op("pe", lambda e: e.transpose(pp_[:, 0:128], GT[:, dg0:dg0 + 8, :].rearrange("p g c -> p (g c)"), self.ident_f[:]), reads=[b, self.b_const], writes=[bps])
                        Tk.op("act", lambda e: e.copy(out=TB[:], in_=pp_[:, 0:128]), reads=[bps], writes=[b])
                        for gg in range(8):
                            Tk.op("dve", lambda e: e.tensor_scalar(out=Gd[:, dg0 + gg, :], in0=TB[:], scalar1=rowmask[:, gg:gg + 1], scalar2=None, op0=ALU.mult), reads=[b], writes=[bP])
            Tk.op("pool", lambda e: e.memset(C1[:], 0.0), writes=[bP])
            Tk.op("pool", lambda e: e.memset(C2[:], 0.0), writes=[bP])
            cre = din["ssm_c_re"][l].rearrange("d g c n -> (d g c) n")
            cim = din["ssm_c_im"][l].rearrange("d g c n -> (d g c) n")
            for d in range(2):
                for ct in range(2):
                    dg0 = d * 16 + ct * 8
                    r0 = (d * 2 + ct) * 128
                    Tk.dma("sync", crl[:], cre[r0:r0 + 128, :], writes=[b])
                    Tk.dma("sync", cil[:], cim[r0:r0 + 128, :], writes=[b])
                    Tk.op("act", lambda e: e.copy(out=CT[:, 0, 0:64], in_=crl[:]), reads=[b], writes=[b])
                    Tk.op("act", lambda e: e.mul(out=CT[:, 0, 64:128], in_=cil[:], mul=-1.0), reads=[b], writes=[b])
                    Tk.op("act", lambda e: e.mul(out=CT[:, 1, 0:64], in_=cil[:], mul=-1.0), reads=[b], writes=[b])
                    Tk.op("act", lambda e: e.mul(out=CT[:, 1, 64:128], in_=crl[:], mul=-1.0), reads=[b], writes=[b])
                    for v, Cd in ((0, C1), (1, C2)):
                        Tk.op("pe", lambda e: e.transpose(pp_[:, 0:128], CT[:, v, :], self.ident_f[:]), reads=[b, self.b_const], writes=[bps])
                        for gg in range(8):
                            Tk.op("dve", lambda e: e.tensor_copy(out=Cd[:, dg0 + gg, gg * 16:(gg + 1) * 16], in_=pp_[:, gg * 16:(gg + 1) * 16]), reads=[bps], writes=[bP])
            Tk.op("pool", lambda e: e.iota(iot[:], pattern=[[1, Lc]], base=1, channel_multiplier=0), writes=[b])
            Tk.op("dve", lambda e: e.tensor_copy(out=iof[:], in_=iot[:]), reads=[b], writes=[b])
            bang = Buf()
            rrs_b = (self.sb(tes, "rr_i", [128, 8, Lc], I32), self.sb(tes, "rr_k", [128, 8, Lc], F32), Buf(), Buf())
            for q8 in range(4):
                for off, shf in ((Lc, 0.0), (0, PI / 2)):
                    Tk.op("dve", lambda e: e.tensor_tensor(out=ang[:], in0=iof[:].unsqueeze(1).to_broadcast([128, 8, Lc]), in1=th[:, q8 * 8:(q8 + 1) * 8].unsqueeze(2).to_broadcast([128, 8, Lc]), op=ALU.mult), reads=[b], writes=[bang])
                    if shf:
                        Tk.op("dve", lambda e: e.tensor_scalar(out=ang[:], in0=ang[:], scalar1=float(shf), scalar2=None, op0=ALU.add), reads=[bang], writes=[bang])
                    self.range_reduce(tes, ang[:], [128, 8, Lc], [bang], scratch=rrs_b)
                    Tk.op("act", lambda e: e.activation(out=CS[:, q8 * 8:(q8 + 1) * 8, off:off + Lc], in_=ang[:], func=AF.Sin), reads=[bang], writes=[bP])
            Tk.op("dve", lambda e: e.tensor_copy(out=shift[:, 0:64], in_=self.ident_f[:, 64:128]), reads=[self.b_const], writes=[b])
            Tk.op("dve", lambda e: e.tensor_copy(out=shift[:, 64:128], in_=self.ident_f[:, 0:64]), reads=[self.b_const], writes=[b])
            Tk.op("dve", lambda e: e.tensor_scalar(out=t1[:], in0=CS[:, :, 2 * Lc - 1], scalar1=sgn[:, 0:1], scalar2=None, op0=ALU.mult), reads=[b, bP], writes=[b])
            for dg in range(32):
                Tk.op("dve", lambda e: e.tensor_scalar(out=R[:, dg, :], in0=self.ident_f[:], scalar1=CS[:, dg, Lc - 1:Lc], scalar2=None, op0=ALU.mult), reads=[b, bP, self.b_const], writes=[bP])
                Tk.op("dve", lambda e: e.scalar_tensor_tensor(out=R[:, dg, :], in0=shift[:], scalar=t1[:, dg:dg + 1], in1=R[:, dg, :], op0=ALU.mult, op1=ALU.add), reads=[b, bP], writes=[bP])
            Tk.dma("sync", dcol[:], din["ssm_d"][l].rearrange("(ct p) -> p ct", p=128), writes=[b], allow_slow_non_contiguous=True)
            for ct in range(2):
                Tk.op("dve", lambda e: e.tensor_scalar(out=diagD[:, ct, :], in0=self.ident_f[:], scalar1=dcol[:, ct:ct + 1], scalar2=None, op0=ALU.mult), reads=[b, self.b_const], writes=[bP])
            self.dump("ssm_G1", G1[:], [128, 32, 128], BF16, [bP])
            self.dump("ssm_C1", C1[:], [128, 32, 128], BF16, [bP])
            self.dump("ssm_CS", CS[:], [128, 32, 2 * Lc], F32, [bP])
            self.dump("ssm_mag", mag[:], [128, 32], F32, [bP])
            Tk.barrier()
        Y = self.sb(es, "Y", [128, 2, T], F32)
        with ExitStack() as tes:
            NB = 4
            zz = [self.sb(tes, "zz", [128, 2 * Lc], F32) for _ in range(NB)]
            z = [self.sb(tes, "z", [128, Lc], F32) for _ in range(NB)]
            w = [self.sb(tes, "w", [128, Lc], F32) for _ in range(NB)]
            pp = [self.sb(tes, "pp", [128, 2, Lc], BF16) for _ in range(NB)]
            pGb = [self.ps(tes, "pG", [128, 512], F32) for _ in range(NB)]
            pC = self.ps(tes, "pC", [128, 512], F32)
            pY = [self.ps(tes, "pY", [128, 512], F32) for _ in range(2)]
            bzz, bz, bpp = [[Buf() for _ in range(NB)] for _ in range(3)]
            bw = [Buf() for _ in range(NB)]
            bpG = [Buf() for _ in range(NB)]
            bpC = [Buf()] * 32
            bcar = [Buf() for _ in range(32)]
            bpY = [Buf(), Buf()]
            bY = [[Buf() for _ in range(NC)] for _ in range(2)]
            its = []
            yi = 0
            for j in range(NC):
                for d in range(2):
                    for ct in range(2):
                        if d == 0:
                            blk = j
                            usl = U[:, ct, j * Lc:(j + 1) * Lc]
                        else:
                            blk = NC - 1 - j
                            usl = rev_ap(U[:, ct, blk * Lc:(blk + 1) * Lc])
                        for gg in range(8):
                            its.append(dict(j=j, d=d, ct=ct, gg=gg, dg=d * 16 + ct * 8 + gg, blk=blk, usl=usl, yk=yi % 2, n=len(its)))
                        yi += 1

            def stA(q):
                kb, dg, usl = q["n"] % NB, q["dg"], q["usl"]
                pG = pGb[kb][:, 0:2 * Lc]
                Tk.op("pe", lambda e: e.matmul(pG[:, 0:Lc], lhsT=G1[:, dg, :], rhs=usl, start=True, stop=True), reads=allU + [bP], writes=[bpG[kb]], inc=False)
                Tk.op("pe", lambda e: e.matmul(pG[:, Lc:2 * Lc], lhsT=G2[:, dg, :], rhs=usl, start=True, stop=True), reads=allU + [bP], writes=[bpG[kb]])

            def stB(qs):
                for q in qs:
                    kb, dg = q["n"] % NB, q["dg"]
                    pG = pGb[kb][:, 0:2 * Lc]
                    Tk.op("dve", lambda e: e.tensor_tensor(out=zz[kb][:], in0=pG, in1=CS[:, dg, :], op=ALU.mult), reads=[bpG[kb], bP], writes=[bzz[kb]])
                for q in qs:
                    kb = q["n"] % NB
                    Tk.op("dve", lambda e: e.tensor_tensor(out=z[kb][:], in0=zz[kb][:, 0:Lc], in1=zz[kb][:, Lc:2 * Lc], op=ALU.add), reads=[bzz[kb]], writes=[bz[kb]])
                for q in qs:
                    kb, dg, j = q["n"] % NB, q["dg"], q["j"]
                    init = 0.0 if j == 0 else carry[:, dg:dg + 1]
                    Tk.op("dve", lambda e: e.tensor_tensor_scan(out=w[kb][:], data0=mag[:, dg:dg + 1].to_broadcast([128, Lc]), data1=z[kb][:], initial=init, op0=ALU.mult, op1=ALU.add),
                          reads=[bz[kb], bP, bcar[dg]], writes=[bw[kb]])

            def stC(q):
                kb, dg, j = q["n"] % NB, q["dg"], q["j"]
                if j < NC - 1:
                    Tk.op("pe", lambda e: e.matmul(pC[:, dg:dg + 1], lhsT=R[:, dg, :], rhs=w[kb][:, Lc - 1:Lc], start=True, stop=True), reads=[bw[kb], bP], writes=[bpC[dg]])
                    Tk.op("act", lambda e: e.copy(out=carry[:, dg:dg + 1], in_=pC[:, dg:dg + 1]), reads=[bpC[dg]], writes=[bcar[dg]])
                Tk.op("pool", lambda e: e.tensor_tensor(out=pp[kb][:], in0=CS[:, dg, :].rearrange("p (a q) -> p a q", a=2), in1=w[kb][:].unsqueeze(1).to_broadcast([128, 2, Lc]), op=ALU.mult),
                      reads=[bw[kb], bP], writes=[bpp[kb]])

            def stE(q):
                kb, dg, j, d, ct, gg, blk, usl = q["n"] % NB, q["dg"], q["j"], q["d"], q["ct"], q["gg"], q["blk"], q["usl"]
                py, bpy = pY[q["yk"]], bpY[q["yk"]]
                Tk.op("pe", lambda e: e.matmul(py[:, 0:Lc], lhsT=C1[:, dg, :], rhs=pp[kb][:, 0, :], start=(gg == 0), stop=False), reads=[bpp[kb], bP], writes=[bpy], inc=False)
                last = (gg == 7 and d == 1)
                Tk.op("pe", lambda e: e.matmul(py[:, 0:Lc], lhsT=C2[:, dg, :], rhs=pp[kb][:, 1, :], start=False, stop=last), reads=[bpp[kb], bP], writes=[bpy])
                if gg != 7:
                    return
                if d == 0:
                    Tk.op("pe", lambda e: e.matmul(py[:, 0:Lc], lhsT=diagD[:, ct, :], rhs=usl, start=False, stop=True), reads=allU + [bP], writes=[bpy])
                ysl = Y[:, ct, blk * Lc:(blk + 1) * Lc]
                first = (j < NC / 2)
                osl = ysl if d == 0 else rev_ap(ysl)
                if first:
                    Tk.op("act", lambda e: e.copy(out=osl, in_=py[:, 0:Lc]), reads=[bpy], writes=[bY[ct][blk]])
                else:
                    Tk.op("dve", lambda e: e.tensor_tensor(out=osl, in0=osl, in1=py[:, 0:Lc], op=ALU.add), reads=[bpy, bY[ct][blk]], writes=[bY[ct][blk]])

            NI = len(its)
            NP = NI // 2
            prs = [its[2 * m:2 * m + 2] for m in range(NP)]
            for sidx in range(NP + 2):
                if sidx < NP:
                    for q in prs[sidx]:
                        stA(q)
                if 1 <= sidx <= NP:
                    stB(prs[sidx - 1])
                    for q in prs[sidx - 1]:
                        stC(q)
                if sidx >= 2:
                    for q in prs[sidx - 2]:
                        stE(q)
            self.dump("ssm_Y", Y[:], [128, 2, T], F32, [bY[ct][k] for ct in range(2) for k in range(NC)])
            Tk.barrier()
        with ExitStack() as tes:
            wglu = self.sb(tes, "wglu", [128, 2, 512], BF16)
            bglu = self.sb(tes, "bglu", [128, 4], F32)
            gss = self.sb(tes, "gss", [128, 2], F32)
            gY = [self.sb(tes, "gY", [128, 2, 512], BF16) for _ in range(2)]
            sg = self.sb(tes, "sg", [128, 2, 512], F32)
            og = self.sb(tes, "og", [128, 2, 512], F32)
            sq = self.sb(tes, "sq3", [128, 2, 512], BF16)
            rs = self.sb(tes, "rs3", [128, 512], F32)
            ho = [self.sb(tes, "ho", [128, 2, 512], BF16) for _ in range(2)]
            pz = [self.ps(tes, "pz", [128, 512], F32) for _ in range(4)]
            pss = self.ps(tes, "pss3", [128, 512], F32)
            bw_ = Buf()
            Tk.dma("pool", wglu[:], din["ssm_w_glu"][l].rearrange("(ct p) n -> p ct n", p=128), writes=[bw_])
            Tk.dma("sync", bglu[:], din["ssm_b_glu"][l].rearrange("(j p) -> p j", p=128), writes=[bw_], allow_slow_non_contiguous=True)
            Tk.dma("sync", gss[:], din["out_g_ssm"][l].rearrange("(j p) -> p j", p=128), writes=[bw_], allow_slow_non_contiguous=True)
            bgY, bho = [Buf(), Buf()], [Buf(), Buf()]
            bpz = [Buf() for _ in range(4)]
            bsg, bog, bsq, bpss, brs = [Buf() for _ in range(5)]
            allY = [bY[ct][k] for ct in range(2) for k in range(NC)]
            for c in range(self.NCH):
                k = c % 2
                cols = slice(c * 512, (c + 1) * 512)
                Tk.op("act", lambda e: e.activation(out=gY[k][:], in_=Y[:, :, cols], func=AF.Gelu_apprx_tanh), reads=allY, writes=[bgY[k]])
                for jt in range(4):
                    for ct in range(2):
                        Tk.op("pe", lambda e: e.matmul(pz[jt][:], lhsT=wglu[:, ct, jt * 128:(jt + 1) * 128], rhs=gY[k][:, ct, :], start=(ct == 0), stop=(ct == 1)), reads=[bgY[k], bw_], writes=[bpz[jt]], inc=(ct == 1))
                for a in range(2):
                    Tk.op("act", lambda e: e.activation(out=sg[:, a, :], in_=pz[2 + a][:], func=AF.Sigmoid, bias=bglu[:, 2 + a:3 + a]), reads=[bpz[2 + a], bw_], writes=[bsg])
                    Tk.op("dve", lambda e: e.scalar_tensor_tensor(out=og[:, a, :], in0=pz[a][:], scalar=bglu[:, a:a + 1], in1=sg[:, a, :], op0=ALU.add, op1=ALU.mult), reads=[bpz[a], bsg, bw_], writes=[bog])
                Tk.op("act", lambda e: e.activation(out=sq[:], in_=og[:], func=AF.Square), reads=[bog], writes=[bsq])
                for a in range(2):
                    Tk.op("pe", lambda e: e.matmul(pss[:], lhsT=self.ones_bf[:], rhs=sq[:, a, :], start=(a == 0), stop=(a == 1)), reads=[bsq, self.b_const], writes=[bpss], inc=(a == 1))
                Tk.op("dve", lambda e: e.tensor_scalar(out=rs[:], in0=pss[:], scalar1=1.0 / 256, scalar2=EPS, op0=ALU.mult, op1=ALU.add), reads=[bpss], writes=[brs])
                Tk.op("act", lambda e: e.activation(out=rs[:], in_=rs[:], func=AF.Sqrt), reads=[brs], writes=[brs])
                Tk.op("dve", lambda e: e.reciprocal(out=rs[:], in_=rs[:]), reads=[brs], writes=[brs])
                for a in range(2):
                    Tk.op("dve", lambda e: e.scalar_tensor_tensor(out=ho[k][:, a, :], in0=og[:, a, :], scalar=gss[:, a:a + 1], in1=rs[:], op0=ALU.mult, op1=ALU.mult), reads=[bog, brs, bw_], writes=[bho[k]])
                Tk.dma("sync", self.HT[384:640, cols].rearrange("(a p) q -> p a q", p=128), ho[k][:], reads=[bho[k]], writes=[Tk.b("HT", l, "ssm", c)])
            Tk.barrier()


MK.p3 = _p3


def _p4(self, l):
    nc, Tk, T, NT, NCH = self.nc, self.trk, self.T, self.NT, self.NCH
    m = self.mix
    CQ, CKV, KR = m["CQ"], m["CKV"], m["KR"]
    din = self.din
    SCALE = 96.0 ** -0.5
    allin = [Tk.b(n, l, c) for n in ("CQ", "CKV", "KR") for c in range(NCH)]
    with ExitStack() as es:
        wuq = self.sb(es, "wuq", [128, 2, 576], BF16)
        wsw = self.sb(es, "wsw", [128, 2, 6, 96], BF16)
        wk = self.sb(es, "wk", [128, 6, 64], BF16)
        wv = self.sb(es, "wv", [128, 6, 64], BF16)
        gml = self.sb(es, "gml", [64, 6], F32)
        ones_f = self.sb(es, "ones_f4", [128, 64], F32)
        K = self.sb(es, "K", [128, 6, T], BF16)
        V = self.sb(es, "V", [128, NT, 6, 65], BF16)
        bW = Buf()
        bK = [Buf() for _ in range(NCH)]
        bKr = Buf()
        bV = [Buf() for _ in range(NT)]
        with ExitStack() as tes:
            sq_ = self.sb(tes, "squ", [128, 2, 576], F32)
            skv = self.sb(tes, "skv", [128, 768], F32)
            gq = self.sb(tes, "gq", [128, 2], F32)
            gkv = self.sb(tes, "gkv", [128, 1], F32)
            wukv = self.sb(tes, "wukv", [128, 6, 128], BF16)
            b = Buf()
            Tk.dma("sync", sq_[:, 0, :], din["mla_w_uq"][l, 0:128, :], writes=[b])
            Tk.dma("sync", sq_[0:64, 1, :], din["mla_w_uq"][l, 128:192, :], writes=[b])
            Tk.dma("sync", skv[:], din["mla_w_ukv"][l], writes=[b])
            gqs = din["mla_q_norm_g"][l]
            Tk.dma("sync", gq[:, 0:1], gqs[0:128].rearrange("(p o) -> p o", o=1), writes=[b])
            Tk.dma("sync", gq[0:64, 1:2], gqs[128:192].rearrange("(p o) -> p o", o=1), writes=[b])
            Tk.dma("sync", gkv[:], din["mla_kv_norm_g"][l].rearrange("(p o) -> p o", o=1), writes=[b])
            Tk.dma("sync", gml[:], din["out_g_mla"][l].rearrange("(h d) -> d h", d=64), writes=[bW], allow_slow_non_contiguous=True)
            Tk.op("dve", lambda e: e.tensor_scalar(out=wuq[:, 0, :], in0=sq_[:, 0, :], scalar1=gq[:, 0:1], scalar2=None, op0=ALU.mult), reads=[b], writes=[bW])
            Tk.op("dve", lambda e: e.tensor_scalar(out=wuq[0:64, 1, :], in0=sq_[0:64, 1, :], scalar1=gq[0:64, 1:2], scalar2=None, op0=ALU.mult), reads=[b], writes=[bW])
            Tk.op("dve", lambda e: e.tensor_scalar(out=wukv[:].rearrange("p h f -> p (h f)"), in0=skv[:], scalar1=gkv[:, 0:1], scalar2=None, op0=ALU.mult), reads=[b], writes=[b])
            Tk.op("pool", lambda e: e.memset(wsw[:], 0.0), writes=[bW])
            for kc, pr in ((0, 128), (1, 64)):
                src = wuq[0:pr, kc, :].rearrange("p (h f) -> p h f", h=6)
                Tk.op("act", lambda e: e.mul(out=wsw[0:pr, kc, :, 64:80], in_=src[:, :, 80:96], mul=-1.0), reads=[bW], writes=[bW])
                Tk.op("act", lambda e: e.copy(out=wsw[0:pr, kc, :, 80:96], in_=src[:, :, 64:80]), reads=[bW], writes=[bW])
            Tk.op("dve", lambda e: e.tensor_copy(out=wk[:], in_=wukv[:, :, 0:64]), reads=[b], writes=[bW])
            Tk.op("dve", lambda e: e.tensor_copy(out=wv[:], in_=wukv[:, :, 64:128]), reads=[b], writes=[bW])
            Tk.op("pool", lambda e: e.memset(ones_f[:], 1.0), writes=[bW])
            Tk.op("pool", lambda e: e.memset(V[:, :, :, 64:65], 1.0), writes=[bW])
            sqk = [self.sb(tes, "sqk", [128, 512], BF16) for _ in range(2)]
            rkv = [self.sb(tes, "rkv", [128, 512], F32) for _ in range(2)]
            rcol = [self.sb(tes, "rcol", [128, 1], F32) for _ in range(2)]
            pss = self.ps(tes, "pss4", [128, 512], F32)
            pk = [self.ps(tes, "pk", [128, 512], F32) for _ in range(2)]
            pv = [self.ps(tes, "pv", [128, 512], F32) for _ in range(2)]
            pc = self.ps(tes, "pc", [128, 512], F32)
            bsqk, brkv, brcol = [Buf(), Buf()], [Buf(), Buf()], [Buf(), Buf()]
            bpss, bpc = Buf(), Buf()
            bpk, bpv = [Buf(), Buf()], [Buf(), Buf()]
            n = 0
            for h in range(6):
                Tk.op(("act", "pool")[h % 2], lambda e: (e.copy(out=K[64:96, h, :], in_=KR[64:96, :]) if h % 2 == 0 else e.tensor_copy(out=K[64:96, h, :], in_=KR[64:96, :])), reads=allin, writes=[bKr])
            for c in range(NCH):
                k = c % 2
                cols = slice(c * 512, (c + 1) * 512)
                Tk.op("act", lambda e: e.activation(out=sqk[k][:], in_=CKV[:, cols], func=AF.Square), reads=allin, writes=[bsqk[k]])
                Tk.op("pe", lambda e: e.matmul(pss[:], lhsT=self.ones_bf[:], rhs=sqk[k][:], start=True, stop=True), reads=[bsqk[k], self.b_const], writes=[bpss])
                Tk.op("dve", lambda e: e.tensor_scalar(out=rkv[k][:], in0=pss[:], scalar1=1.0 / 128, scalar2=EPS, op0=ALU.mult, op1=ALU.add), reads=[bpss], writes=[brkv[k]])
                Tk.op("act", lambda e: e.activation(out=rkv[k][:], in_=rkv[k][:], func=AF.Sqrt), reads=[brkv[k]], writes=[brkv[k]])
                Tk.op("dve", lambda e: e.reciprocal(out=rkv[k][:], in_=rkv[k][:]), reads=[brkv[k]], writes=[brkv[k]])
                for h in range(6):
                    kk = n % 2
                    n += 1
                    Tk.op("pe", lambda e: e.matmul(pk[kk][0:64, :], lhsT=wk[:, h, :], rhs=CKV[:, cols], start=True, stop=True), reads=allin + [bW], writes=[bpk[kk]])
                    Tk.op("dve", lambda e: e.tensor_tensor(out=K[0:64, h, cols], in0=pk[kk][0:64, :], in1=rkv[k][0:64, :], op=ALU.mult), reads=[bpk[kk], brkv[k]], writes=[bK[c]])
                for tt in range(4):
                    i = 4 * c + tt
                    kk = i % 2
                    ts_ = slice(i * 128, (i + 1) * 128)
                    Tk.op("pe", lambda e: e.matmul(pc[:, i % 512:i % 512 + 1], lhsT=sqk[k][:, tt * 128:(tt + 1) * 128], rhs=self.ones_bf[:, 0:1], start=True, stop=True), reads=[bsqk[k], self.b_const], writes=[bpc])
                    Tk.op("dve", lambda e: e.tensor_scalar(out=rcol[kk][:], in0=pc[:, i % 512:i % 512 + 1], scalar1=1.0 / 128, scalar2=EPS, op0=ALU.mult, op1=ALU.add), reads=[bpc], writes=[brcol[kk]])
                    Tk.op("act", lambda e: e.activation(out=rcol[kk][:], in_=rcol[kk][:], func=AF.Sqrt), reads=[brcol[kk]], writes=[brcol[kk]])
                    Tk.op("dve", lambda e: e.reciprocal(out=rcol[kk][:], in_=rcol[kk][:]), reads=[brcol[kk]], writes=[brcol[kk]])
                    Tk.op("pe", lambda e: e.matmul(pv[kk][:, 0:384], lhsT=CKV[:, ts_], rhs=wv[:].rearrange("p h f -> p (h f)"), start=True, stop=True), reads=allin + [bW], writes=[bpv[kk]])
                    Tk.op("dve", lambda e: e.tensor_scalar(out=V[:, i, :, 0:64], in0=pv[kk][:, 0:384].rearrange("p (h f) -> p h f", h=6), scalar1=rcol[kk][:, 0:1], scalar2=None, op0=ALU.mult), reads=[bpv[kk], brcol[kk]], writes=[bV[i]])
            self.dump("mla_K", K[:], [128, 6, T], BF16, bK + [bKr])
            self.dump("mla_V", V[:], [128, NT, 6, 65], BF16, bV + [bW])
            Tk.barrier()
        with ExitStack() as tes:
            sqq = self.sb(tes, "sqq", [128, 2, 512], BF16)
            rq = self.sb(tes, "rq", [128, 512], F32)
            CR = self.sb(tes, "CR", [128, 512], F32)
            SR = self.sb(tes, "SR", [128, 512], F32)
            qt1 = self.sb(tes, "qt1", [128, 512], F32)
            qt2 = self.sb(tes, "qt2", [128, 512], F32)
            Q = [self.sb(tes, "Q", [128, 6, 512], BF16) for _ in range(2)]
            rp = self.sb(tes, "rp4", [128, 2, 512], F32)
            brp = Buf()
            PT = [self.sb(tes, "PT4", [128, 512], BF16) for _ in range(3)]
            Osb = self.sb(tes, "Osb4", [65, 512], F32)
            rd = self.sb(tes, "rd4", [65, 512], F32)
            oall = self.sb(tes, "oall", [64, 6, 512], F32)
            sqo = self.sb(tes, "sqo", [64, 6, 512], BF16)
            rs = self.sb(tes, "rs4", [64, 512], F32)
            hT = self.sb(tes, "hT4", [64, 6, 512], BF16)
            pS = [self.ps(tes, "pS4", [128, 512], F32) for _ in range(4)]
            pO = [self.ps(tes, "pO4", [128, 512], F32) for _ in range(2)]
            pM = [self.ps(tes, "pM4", [128, 512], F32) for _ in range(2)]
            bsqq, brq, bCR, bqt = Buf(), Buf(), Buf(), Buf()
            bQ, bhT = [Buf(), Buf()], Buf()
            bPT, bpS = [Buf() for _ in range(3)], [Buf() for _ in range(4)]
            bpO, bpM = [Buf(), Buf()], [Buf(), Buf()]
            bOsb, brd, boall, bsqo, brs = [Buf() for _ in range(5)]
            allK = bK + [bKr]
            allV = bV + [bW]
            r2 = slice(64, 96)

            def prepA1(c):
                cols = slice(c * 512, (c + 1) * 512)
                Tk.op("act", lambda e: e.activation(out=sqq[:, 0, :], in_=CQ[:, 0, cols], func=AF.Square), reads=allin, writes=[bsqq])
                Tk.op("act", lambda e: e.activation(out=sqq[0:64, 1, :], in_=CQ[0:64, 1, cols], func=AF.Square), reads=allin, writes=[bsqq])
                for v in range(2):
                    Tk.dma("sync", rp[64:96, v, :], self.ROPE[v, :, cols], reads=[self.b_rope], writes=[brp])

            def prepA2(c):
                Tk.op("pe", lambda e: e.matmul(pM[0][:], lhsT=self.ones_bf[:], rhs=sqq[:, 0, :], start=True, stop=False), reads=[bsqq, self.b_const], writes=[bpM[0]], inc=False)
                Tk.op("pe", lambda e: e.matmul(pM[0][:], lhsT=self.ones_bf[0:64, :], rhs=sqq[0:64, 1, :], start=False, stop=True), reads=[bsqq, self.b_const], writes=[bpM[0]])
                Tk.op("dve", lambda e: e.tensor_scalar(out=rq[:], in0=pM[0][:], scalar1=1.0 / 192, scalar2=EPS, op0=ALU.mult, op1=ALU.add), reads=[bpM[0]], writes=[brq])
                Tk.op("act", lambda e: e.activation(out=rq[:], in_=rq[:], func=AF.Sqrt), reads=[brq], writes=[brq])
                Tk.op("dve", lambda e: e.reciprocal(out=rq[:], in_=rq[:]), reads=[brq], writes=[brq])
                Tk.op("dve", lambda e: e.tensor_tensor(out=CR[r2, :], in0=rp[r2, 0, :], in1=rq[r2, :], op=ALU.mult), reads=[brq, brp], writes=[bCR])
                Tk.op("dve", lambda e: e.tensor_tensor(out=SR[r2, :], in0=rp[r2, 1, :], in1=rq[r2, :], op=ALU.mult), reads=[brq, brp], writes=[bCR])

            def prepQ(c, h):
                cols = slice(c * 512, (c + 1) * 512)
                qb = c % 2
                for v in range(2):
                    for kc, pr in ((0, 128), (1, 64)):
                        lhs = wuq[0:pr, kc, h * 96:(h + 1) * 96] if v == 0 else wsw[0:pr, kc, h, :]
                        Tk.op("pe", lambda e: e.matmul(pM[v][0:96, :], lhsT=lhs, rhs=CQ[0:pr, kc, cols], start=(kc == 0), stop=(kc == 1)), reads=allin + [bW], writes=[bpM[v]], inc=(kc == 1))
                Tk.op("dve", lambda e: e.tensor_tensor(out=Q[qb][0:64, h, :], in0=pM[0][0:64, :], in1=rq[0:64, :], op=ALU.mult), reads=[bpM[0], brq], writes=[bQ[qb]])
                Tk.op("dve", lambda e: e.tensor_tensor(out=qt1[r2, :], in0=pM[0][r2, :], in1=CR[r2, :], op=ALU.mult), reads=[bpM[0], bCR], writes=[bqt])
                Tk.op("dve", lambda e: e.tensor_tensor(out=qt2[r2, :], in0=pM[1][r2, :], in1=SR[r2, :], op=ALU.mult), reads=[bpM[1], bCR], writes=[bqt])
                Tk.op("dve", lambda e: e.tensor_tensor(out=Q[qb][r2, h, :], in0=qt1[r2, :], in1=qt2[r2, :], op=ALU.add), reads=[bqt], writes=[bQ[qb]])

            def epi(c):
                cols = slice(c * 512, (c + 1) * 512)
                Tk.op("act", lambda e: e.activation(out=sqo[:], in_=oall[:], func=AF.Square), reads=[boall], writes=[bsqo])
                for h in range(6):
                    Tk.op("pe", lambda e: e.matmul(pM[1][0:64, :], lhsT=self.ones_bf[0:64, 0:64], rhs=sqo[:, h, :], start=(h == 0), stop=(h == 5)), reads=[bsqo, self.b_const], writes=[bpM[1]], inc=(h == 5))
                Tk.op("dve", lambda e: e.tensor_scalar(out=rs[:], in0=pM[1][0:64, :], scalar1=1.0 / 384, scalar2=EPS, op0=ALU.mult, op1=ALU.add), reads=[bpM[1]], writes=[brs])
                Tk.op("act", lambda e: e.activation(out=rs[:], in_=rs[:], func=AF.Sqrt), reads=[brs], writes=[brs])
                Tk.op("dve", lambda e: e.reciprocal(out=rs[:], in_=rs[:]), reads=[brs], writes=[brs])
                Tk.op("dve", lambda e: e.tensor_tensor(out=oall[:], in0=oall[:], in1=rs[:].unsqueeze(1).to_broadcast([64, 6, 512]), op=ALU.mult), reads=[boall, brs], writes=[boall])
                Tk.op("dve", lambda e: e.tensor_tensor(out=hT[:], in0=oall[:], in1=gml[:].unsqueeze(2).to_broadcast([64, 6, 512]), op=ALU.mult), reads=[boall, bW], writes=[bhT])
                Tk.dma("sync", self.HT[640:1024, cols].rearrange("(h d) q -> d h q", d=64), hT[:], reads=[bhT], writes=[Tk.b("HT", l, "mla", c)])

            steps = [(c, h, kt) for c in range(NCH) for h in range(6) for kt in range(NT)]
            SPC = 6 * NT
            NS_ = len(steps)

            def stS(n):
                c, h, kt = steps[n]
                k4 = n % 4
                Tk.op("pe", lambda e: e.matmul(pS[k4][:], lhsT=K[0:96, h, kt * 128:(kt + 1) * 128], rhs=Q[c % 2][0:96, h, :], start=True, stop=True), reads=allK + [bQ[c % 2]], writes=[bpS[k4]])

            def stPV(n):
                c, h, kt = steps[n]
                k3, k4 = n % 3, n % 4
                po, bpo = pO[h % 2], bpO[h % 2]
                Tk.op("act", lambda e: e.activation(out=PT[k3][:], in_=pS[k4][:], func=AF.Exp, scale=SCALE), reads=[bpS[k4]], writes=[bPT[k3]])
                Tk.op("pe", lambda e: e.matmul(po[0:65, :], lhsT=V[:, kt, h, :], rhs=PT[k3][:], start=(kt == 0), stop=(kt == NT - 1)), reads=allV + [bPT[k3]], writes=[bpo], inc=(kt == NT - 1))
                if kt == NT - 1:
                    Tk.op("act", lambda e: e.copy(out=Osb[:], in_=po[0:65, :]), reads=[bpo], writes=[bOsb])
                    Tk.op("dve", lambda e: e.reciprocal(out=rd[64:65, :], in_=Osb[64:65, :]), reads=[bOsb], writes=[brd])

            def stEp(h):
                Tk.op("pe", lambda e: e.matmul(pM[0][0:64, :], lhsT=ones_f[64:65, 0:64], rhs=rd[64:65, :], start=True, stop=True), reads=[brd, bW], writes=[bpM[0]])
                Tk.op("dve", lambda e: e.tensor_tensor(out=oall[:, h, :], in0=Osb[0:64, :], in1=pM[0][0:64, :], op=ALU.mult), reads=[bOsb, bpM[0]], writes=[boall])

            hooks = {}

            def hook(n, f):
                hooks.setdefault(min(n, NS_), []).append(f)

            dly = min(4, NT - 2)
            for c in range(NCH):
                base = c * SPC
                if c + 1 < NCH:
                    hook(base + SPC // 8, lambda c=c: prepA1(c + 1))
                    hook(base + SPC // 8 + 3, lambda c=c: prepA2(c + 1))
                    for h in range(6):
                        hook(base + SPC // 4 + (h * SPC) // 10, lambda c=c, h=h: prepQ(c + 1, h))
                for h in range(6):
                    hook(base + (h + 1) * NT + dly, lambda h=h: stEp(h))
                hook(base + SPC + dly + 4, lambda c=c: epi(c))
            prepA1(0)
            prepA2(0)
            for h in range(6):
                prepQ(0, h)
            if True:
                self.dump("mla_Q", Q[0][:], [128, 6, 512], BF16, [bQ[0]])
            for n in range(NS_ + 2):
                if n < NS_:
                    stS(n)
                if n >= 2:
                    stPV(n - 2)
                for f in hooks.get(n - 1, ()):
                    f()
            Tk.barrier()


MK.p4 = _p4


def _p5(self, l, xsrc, xkey):
    nc, Tk, T, NT, NCH = self.nc, self.trk, self.T, self.NT, self.NCH
    din = self.din
    GATE = self.GATE
    with ExitStack() as es:
        wout = self.sb(es, "wout", [128, 8, D], BF16)
        wr = self.sb(es, "wr", [128, 8, 36], F32)
        brbc = self.sb(es, "brbc", [128, 36], F32)
        g2bc = self.sb(es, "g2bc", [128, D], F32)
        hTs = [self.sb(es, "hTs", [128, 8, 512], BF16) for _ in range(2)]
        xt = [self.sb(es, "xt5", [128, D], F32) for _ in range(2)]
        x1 = [self.sb(es, "x1", [128, D], F32) for _ in range(2)]
        junk = self.sb(es, "junk5", [128, D], BF16)
        xn2 = [self.sb(es, "xn2", [128, D], F32) for _ in range(2)]
        xTf = [self.sb(es, "xTf", [128, 8, 128], F32) for _ in range(2)]
        xTb = [self.sb(es, "xTb", [128, 8, 512], BF16) for _ in range(2)]
        ss = [self.sb(es, "ss5", [128, 1], F32) for _ in range(2)]
        rstd = [self.sb(es, "rstd5", [128, 1], F32) for _ in range(2)]
        sm = [self.sb(es, "sm", [128, 128], F32) for _ in range(2)]
        px = [self.ps(es, "px", [128, 512], F32) for _ in range(2)]
        pT = [self.ps(es, "pT5", [128, 1024], F32)]
        pr = self.ps(es, "pr", [128, 512], F32)
        bw = Buf()
        Tk.dma("pool", wout[:], din["w_out"][l].rearrange("(kc p) n -> p kc n", p=128), writes=[bw])
        Tk.dma("sync", wr[:, :, 0:4], din["w_router_group"][l].rearrange("(kc p) n -> p kc n", p=128), writes=[bw])
        Tk.dma("sync", wr[:, :, 4:36], din["w_router_expert"][l].rearrange("(kc p) n -> p kc n", p=128), writes=[bw])
        Tk.dma("sync", brbc[:, 0:4], din["b_router_group"][l:l + 1, :].partition_broadcast(128), writes=[bw])
        Tk.dma("sync", brbc[:, 4:36], din["b_router_expert"][l:l + 1, :].partition_broadcast(128), writes=[bw])
        Tk.dma("sync", g2bc[:], din["ln2_g"][l:l + 1, :].partition_broadcast(128), writes=[bw])
        bhTs, bxt, bx1, bxn2, bxTf, bxTb, bss, bsm = [[Buf(), Buf()] for _ in range(8)]
        bpx = [Buf(), Buf()]
        bpT = Buf()
        bpr, bjunk = Buf(), Buf()
        pT1 = pT[0]

        def s0(i):
            c, tt = divmod(i, 4)
            cb, p = c % 2, i % 2
            cols = slice(c * 512, (c + 1) * 512)
            if tt == 0:
                Tk.dma("sync", hTs[cb][:], self.HT[:, cols].rearrange("(kc p) q -> p kc q", p=128),
                       reads=[Tk.b("HT", l, "attn", j) for j in range(4 * c, 4 * c + 4)] + [Tk.b("HT", l, "ssm", c), Tk.b("HT", l, "mla", c)], writes=[bhTs[cb]])
            Tk.dma("sync", xt[p][:], xsrc[i * 128:(i + 1) * 128, :], reads=[Tk.b(xkey, i)], writes=[bxt[p]])
            for half in range(2):
                for kc in range(8):
                    Tk.op("pe", lambda e: e.matmul(px[half][:], lhsT=hTs[cb][:, kc, tt * 128:(tt + 1) * 128], rhs=wout[:, kc, half * 512:(half + 1) * 512], start=(kc == 0), stop=(kc == 7)),
                          reads=[bhTs[cb], bw], writes=[bpx[half]], inc=(kc == 7))

        def s1(i):
            p = i % 2
            rows = slice(i * 128, (i + 1) * 128)
            for half in range(2):
                Tk.op("dve", lambda e: e.tensor_tensor(out=x1[p][:, half * 512:(half + 1) * 512], in0=xt[p][:, half * 512:(half + 1) * 512], in1=px[half][:], op=ALU.add), reads=[bxt[p], bpx[half]], writes=[bx1[p]])
            Tk.dma("sync", self.XR[rows, :], x1[p][:], reads=[bx1[p], Tk.b(xkey, i)], writes=[Tk.b("XR", i)])
            Tk.op("dve", lambda e: e.memset(ss[p][:], 0.0), writes=[bss[p]])
            Tk.op("act", lambda e: e.activation(out=junk[:], in_=x1[p][:], func=AF.Square, accum_out=ss[p][:]), reads=[bx1[p], bss[p]], writes=[bjunk, bss[p]])
            Tk.op("dve", lambda e: e.tensor_scalar(out=ss[p][:], in0=ss[p][:], scalar1=1.0 / D, scalar2=EPS, op0=ALU.mult, op1=ALU.add), reads=[bss[p]], writes=[bss[p]])
            Tk.op("act", lambda e: e.activation(out=ss[p][:], in_=ss[p][:], func=AF.Ln), reads=[bss[p]], writes=[bss[p]])
            Tk.op("act", lambda e: e.activation(out=rstd[p][:], in_=ss[p][:], func=AF.Exp, scale=-0.5), reads=[bss[p]], writes=[bss[p]])
            Tk.op("dve", lambda e: e.scalar_tensor_tensor(out=xn2[p][:], in0=x1[p][:], scalar=rstd[p][:, 0:1], in1=g2bc[:], op0=ALU.mult, op1=ALU.mult), reads=[bx1[p], bss[p], bw], writes=[bxn2[p]])

        def s2(i):
            c, tt = divmod(i, 4)
            cb, p = c % 2, i % 2
            cols = slice(c * 512, (c + 1) * 512)
            for kc in range(8):
                Tk.op("pe", lambda e: e.transpose(pT1[:, kc * 128:(kc + 1) * 128], xn2[p][:, kc * 128:(kc + 1) * 128], self.ident_f[:]), reads=[bxn2[p], self.b_const], writes=[bpT], inc=(kc == 7))
            for hb_ in range(2):
                pT3 = pT1[:, hb_ * 512:(hb_ + 1) * 512].rearrange("p (k t) -> p k t", k=4)
                Tk.op("act", lambda e: e.copy(out=xTf[p][:, hb_ * 4:(hb_ + 1) * 4, :], in_=pT3), reads=[bpT], writes=[bxTf[p]])
                Tk.op("dve", lambda e: e.tensor_copy(out=xTb[cb][:, hb_ * 4:(hb_ + 1) * 4, tt * 128:(tt + 1) * 128], in_=xTf[p][:, hb_ * 4:(hb_ + 1) * 4, :]), reads=[bxTf[p]], writes=[bxTb[cb]])
            if tt == 3:
                Tk.dma("sync", self.XN2T[:, cols].rearrange("(kc p) q -> p kc q", p=128), xTb[cb][:], reads=[bxTb[cb]], writes=[Tk.b("XN2T", l, c)])

        def s3(i):
            p = i % 2
            for kc in range(8):
                Tk.op("pe", lambda e: e.matmul(pr[:, 0:36], lhsT=xTf[p][:, kc, :], rhs=wr[:, kc, :], start=(kc == 0), stop=(kc == 7)), reads=[bxTf[p], bw], writes=[bpr], inc=(kc == 7))

        def s4(i):
            p = i % 2
            s = sm[p]
            bs = bsm[p]
            lg = s[:, 0:36]
            gmax, ngmax, gsum, pg = s[:, 36:37], s[:, 37:38], s[:, 38:39], s[:, 39:40]
            ghot, m1, gex = s[:, 40:44], s[:, 44:48], s[:, 48:52]
            top8 = s[:, 52:60]
            d21, e21, w1, w2 = s[:, 60:61], s[:, 61:62], s[:, 62:63], s[:, 63:64]
            msk = s[:, 64:96]
            g1 = s[:, 96:128]
            msk3 = msk.rearrange("p (g e) -> p g e", g=4)

            def dv(fn, extra=()):
                Tk.op("dve", fn, reads=[bs] + list(extra), writes=[bs])

            dv(lambda e: e.tensor_tensor(out=lg, in0=pr[:, 0:36], in1=brbc[:], op=ALU.add), [bpr, bw])
            dv(lambda e: e.tensor_reduce(out=gmax, in_=lg[:, 0:4], axis=AX.X, op=ALU.max))
            dv(lambda e: e.tensor_scalar(out=ghot, in0=lg[:, 0:4], scalar1=gmax, scalar2=None, op0=ALU.is_ge))
            dv(lambda e: e.tensor_scalar(out=ngmax, in0=gmax, scalar1=-1.0, scalar2=None, op0=ALU.mult))
            dv(lambda e: e.memset(gsum, 0.0))
            Tk.op("act", lambda e: e.activation(out=gex, in_=lg[:, 0:4], func=AF.Exp, bias=ngmax, accum_out=gsum), reads=[bs], writes=[bs])
            dv(lambda e: e.reciprocal(out=pg, in_=gsum))
            dv(lambda e: e.tensor_scalar(out=m1, in0=ghot, scalar1=-1.0, scalar2=1.0e4, op0=ALU.add, op1=ALU.mult))
            dv(lambda e: e.tensor_tensor(out=msk3, in0=lg[:, 4:36].rearrange("p (g e) -> p g e", g=4), in1=ghot.unsqueeze(2).to_broadcast([128, 4, 8]), op=ALU.mult))
            dv(lambda e: e.tensor_tensor(out=msk3, in0=msk3, in1=m1.unsqueeze(2).to_broadcast([128, 4, 8]), op=ALU.add))
            dv(lambda e: e.max(out=top8, in_=msk))
            dv(lambda e: e.tensor_tensor(out=d21, in0=top8[:, 1:2], in1=top8[:, 0:1], op=ALU.subtract))
            Tk.op("act", lambda e: e.activation(out=e21, in_=d21, func=AF.Exp), reads=[bs], writes=[bs])
            dv(lambda e: e.tensor_scalar(out=w1, in0=e21, scalar1=1.0, scalar2=None, op0=ALU.add))
            dv(lambda e: e.reciprocal(out=w1, in_=w1))
            dv(lambda e: e.tensor_tensor(out=w1, in0=w1, in1=pg, op=ALU.mult))
            dv(lambda e: e.tensor_tensor(out=w2, in0=w1, in1=e21, op=ALU.mult))
            dv(lambda e: e.tensor_scalar(out=g1, in0=msk, scalar1=top8[:, 0:1], scalar2=w1, op0=ALU.is_equal, op1=ALU.mult))
            dv(lambda e: e.tensor_scalar(out=msk, in0=msk, scalar1=top8[:, 1:2], scalar2=w2, op0=ALU.is_equal, op1=ALU.mult))
            Tk.op("dve", lambda e: e.tensor_tensor(out=GATE[:, i, :], in0=g1, in1=msk, op=ALU.add), reads=[bs], writes=[Tk.b("GATE", l, i)])

        for k in range(NT + 4):
            if 0 <= k - 4 < NT:
                s4(k - 4)
            if 0 <= k - 1 < NT:
                s1(k - 1)
            if k < NT:
                s0(k)
            if 0 <= k - 2 < NT:
                s2(k - 2)
            if 0 <= k - 3 < NT:
                s3(k - 3)
        Tk.barrier()


def _p6(self, l, last):
    nc, Tk, T, NT = self.nc, self.trk, self.T, self.NT
    din = self.din
    GATE = self.GATE
    SC = min(getattr(self, "moe_sc", 2048), T)
    NS = T // SC
    NTS = SC // 128
    NC4 = SC // 512
    NW = 3
    with ExitStack() as es:
        XN = self.sb(es, "XN", [128, 8, SC], BF16)
        yacc = self.sb(es, "yacc", [128, NTS, D], F32)
        wg = [self.sb(es, "wg", [128, 8, 256], BF16) for _ in range(NW)]
        wu = [self.sb(es, "wu", [128, 8, 256], BF16) for _ in range(NW)]
        wd = [self.sb(es, "wd", [128, 2, D], BF16) for _ in range(NW)]
        sgl = [self.sb(es, "sgl", [128, 2, 512], F32) for _ in range(2)]
        hT = [self.sb(es, "hT6", [128, 2, 512], BF16) for _ in range(2)]
        xt = [self.sb(es, "xt6", [128, D], F32) for _ in range(4)]
        fgbc = self.sb(es, "fgbc", [128, D], F32)
        junk = self.sb(es, "junk6", [128, D], BF16)
        ss = [self.sb(es, "ss6", [128, 1], F32) for _ in range(2)]
        pgu = [self.ps(es, "pgu", [128, 512], F32) for _ in range(4)]
        py = [self.ps(es, "py", [128, 512], F32) for _ in range(4)]
        bfg = Buf()
        if last:
            Tk.dma("sync", fgbc[:], din["final_g"].rearrange("(o n) -> o n", o=1).partition_broadcast(128), writes=[bfg])
        bXN = [Buf() for _ in range(NC4)]
        bw = [Buf() for _ in range(NW)]
        bsgl, bhT = [[[Buf(), Buf()] for _ in range(2)] for _ in range(2)]
        bxt = [Buf() for _ in range(4)]
        bss = [Buf(), Buf()]
        bpgu, bpy = [Buf() for _ in range(4)], [Buf() for _ in range(4)]
        byacc = [Buf() for _ in range(NTS)]
        bjunk = Buf()
        allgate = [Tk.b("GATE", l, i) for i in range(NT)]
        allxn = [Tk.b("XN2T", l, c) for c in range(self.NCH)]
        cnt = {"py": 0}

        def load_xn(s, c4):
            t0 = s * SC + c4 * 512
            Tk.dma("sync", XN[:, :, c4 * 512:(c4 + 1) * 512], self.XN2T[:, t0:t0 + 512].rearrange("(kc p) q -> p kc q", p=128), reads=allxn, writes=[bXN[c4]])

        def load_w(gex):
            ex = gex % 32
            g, ee = divmod(ex, 8)
            wb = gex % NW
            Tk.dma("pool", wg[wb][:], din["w_gate"][l, g, ee].rearrange("(kc p) f -> p kc f", p=128), writes=[bw[wb]])
            Tk.dma("pool", wu[wb][:], din["w_up"][l, g, ee].rearrange("(kc p) f -> p kc f", p=128), writes=[bw[wb]])
            Tk.dma("pool", wd[wb][:], din["w_down"][l, g, ee].rearrange("(fc p) n -> p fc n", p=128), writes=[bw[wb]])

        blocks = [(s, ex, c4) for s in range(NS) for ex in range(32) for c4 in range(NC4)]
        NBk = len(blocks)

        def stA(n, ft):
            s, ex, c4 = blocks[n]
            wb = (s * 32 + ex) % NW
            hb = n % 2
            cols = slice(c4 * 512, (c4 + 1) * 512)
            for v, wt in ((0, wg[wb]), (1, wu[wb])):
                pp = pgu[v * 2 + ft]
                for kc in range(8):
                    Tk.op("pe", lambda e: e.matmul(pp[:], lhsT=wt[:, kc, ft * 128:(ft + 1) * 128], rhs=XN[:, kc, cols], start=(kc == 0), stop=(kc == 7)),
                          reads=[bw[wb], bXN[c4]], writes=[bpgu[v * 2 + ft]], inc=(kc == 7))
            Tk.op("act", lambda e: e.activation(out=sgl[hb][:, ft, :], in_=pgu[ft][:], func=AF.Silu), reads=[bpgu[ft]], writes=[bsgl[hb][ft]])
            Tk.op("dve", lambda e: e.tensor_tensor(out=hT[hb][:, ft, :], in0=sgl[hb][:, ft, :], in1=pgu[2 + ft][:], op=ALU.mult), reads=[bsgl[hb][ft], bpgu[2 + ft]], writes=[bhT[hb][ft]])
            if ft == 1 and ex == 31 and s + 1 < NS:
                load_xn(s + 1, c4)

        def stD(n):
            s, ex, c4 = blocks[n]
            wb = (s * 32 + ex) % NW
            hb = n % 2
            for tt in range(4):
                ti = c4 * 4 + tt
                gi = s * NTS + ti
                if ex == 0:
                    xb = gi % 4
                    Tk.dma("sync", xt[xb][:], self.XR[gi * 128:(gi + 1) * 128, :], reads=[Tk.b("XR", gi)], writes=[bxt[xb]])
                for half in range(2):
                    k4 = cnt["py"] % 4
                    cnt["py"] += 1
                    for ft in range(2):
                        Tk.op("pe", lambda e: e.matmul(py[k4][:], lhsT=hT[hb][:, ft, tt * 128:(tt + 1) * 128], rhs=wd[wb][:, ft, half * 512:(half + 1) * 512], start=(ft == 0), stop=(ft == 1)),
                              reads=[bhT[hb][ft], bw[wb]], writes=[bpy[k4]], inc=(ft == 1))
                    hs = slice(half * 512, (half + 1) * 512)
                    ya = yacc[:, ti, hs]
                    gcol = GATE[:, gi, ex:ex + 1]
                    if ex == 0:
                        Tk.op("dve", lambda e: e.scalar_tensor_tensor(out=ya, in0=py[k4][:], scalar=gcol, in1=xt[gi % 4][:, hs], op0=ALU.mult, op1=ALU.add), reads=[bpy[k4], bxt[gi % 4]] + allgate, writes=[byacc[ti]])
                    else:
                        Tk.op("dve", lambda e: e.scalar_tensor_tensor(out=ya, in0=py[k4][:], scalar=gcol, in1=ya, op0=ALU.mult, op1=ALU.add), reads=[bpy[k4], byacc[ti]] + allgate, writes=[byacc[ti]])

        def finish(s):
            for ti in range(NTS):
                gi = s * NTS + ti
                p = gi % 2
                rows = slice(gi * 128, (gi + 1) * 128)
                if not last:
                    Tk.dma("sync", self.XR[rows, :], yacc[:, ti, :], reads=[byacc[ti]], writes=[Tk.b("XR", gi)])
                else:
                    Tk.op("dve", lambda e: e.memset(ss[p][:], 0.0), writes=[bss[p]])
                    Tk.op("act", lambda e: e.activation(out=junk[:], in_=yacc[:, ti, :], func=AF.Square, accum_out=ss[p][:]), reads=[byacc[ti], bss[p]], writes=[bjunk, bss[p]])
                    Tk.op("dve", lambda e: e.tensor_scalar(out=ss[p][:], in0=ss[p][:], scalar1=1.0 / D, scalar2=EPS, op0=ALU.mult, op1=ALU.add), reads=[bss[p]], writes=[bss[p]])
                    Tk.op("act", lambda e: e.activation(out=ss[p][:], in_=ss[p][:], func=AF.Sqrt), reads=[bss[p]], writes=[bss[p]])
                    Tk.op("dve", lambda e: e.reciprocal(out=ss[p][:], in_=ss[p][:]), reads=[bss[p]], writes=[bss[p]])
                    xb = gi % 4
                    Tk.op("dve", lambda e: e.scalar_tensor_tensor(out=xt[xb][:], in0=yacc[:, ti, :], scalar=ss[p][:, 0:1], in1=fgbc[:], op0=ALU.mult, op1=ALU.mult), reads=[byacc[ti], bss[p], bfg], writes=[bxt[xb]])
                    Tk.dma("sync", self.out[rows, :], xt[xb][:], reads=[bxt[xb]], writes=[Tk.b("OUT", gi)])

        for c4 in range(NC4):
            load_xn(0, c4)
        for gex in range(min(NW, NS * 32)):
            load_w(gex)
        stA(0, 0)
        stA(0, 1)
        for n in range(NBk):
            s, ex, c4 = blocks[n]
            if n + 1 < NBk:
                stA(n + 1, 0)
            stD(n)
            if c4 == NC4 - 1 and s * 32 + ex + NW < NS * 32:
                load_w(s * 32 + ex + NW)
            if ex == 31 and c4 == NC4 - 1:
                finish(s)
            if n + 1 < NBk:
                stA(n + 1, 1)
        Tk.barrier()


MK.p5 = _p5
MK.p6 = _p6


def kernel(**inputs):
    x = np.asarray(inputs["x"], dtype=np.float32)
    B, T, _ = x.shape
    mk = MK(T=T, depth=2)
    nc = mk.build()
    shared = {k: np.ascontiguousarray(np.asarray(inputs[k], dtype=np.float32)) for k in INPUT_SHAPES}
    in_maps = []
    for b in range(B):
        d = dict(shared)
        d["x"] = np.ascontiguousarray(x[b])
        in_maps.append(d)
    res = run_bass_kernel_spmd(nc, in_maps, core_ids=list(range(B)))
    return np.stack([np.asarray(r["out"], dtype=np.float32) for r in res.results], axis=0)
```

```python
import math
from contextlib import ExitStack

import numpy as np
import concourse.bass as bass
import concourse.mybir as mybir
from concourse.bass_utils import run_bass_kernel_spmd

F32 = mybir.dt.float32
BF16 = mybir.dt.bfloat16
I32 = mybir.dt.int32
AF = mybir.ActivationFunctionType
ALU = mybir.AluOpType
AX = mybir.AxisListType

D = 1024
HD = 64
IN_COLS = 1248
EPS = 1e-6
PI = math.pi


class Buf:
    __slots__ = ("last_w", "readers")

    def __init__(self):
        self.last_w = None
        self.readers = {}


class Trk:
    def __init__(self, nc, es):
        self.nc = nc
        self.eng = {"pe": nc.tensor, "act": nc.scalar, "dve": nc.vector, "pool": nc.gpsimd, "sync": nc.sync}
        self.sem = {}
        self.cnt = {}
        self.waited = {k: {} for k in self.eng}
        for k in ("pe", "act", "dve", "pool"):
            self.sem[k] = es.enter_context(nc.semaphore("s_" + k))
            self.cnt[k] = 0
        self.lanes = {}
        self.lane_sem = {}
        self.lane_i = {}
        for q, n in {"sync": 16, "act": 8, "pool": 8}.items():
            self.lanes[q] = []
            for i in range(n):
                nm = "%s%d" % (q, i)
                s = es.enter_context(nc.semaphore("l_" + nm))
                self.lanes[q].append([s, 0, nm])
                self.lane_sem[nm] = s
            self.lane_i[q] = 0
        self.bufs = {}

    def b(self, *key):
        v = self.bufs.get(key)
        if v is None:
            v = self.bufs[key] = Buf()
        return v

    def _wait(self, e, tok):
        kind, key, val = tok
        w = self.waited[e]
        if w.get(key, 0) >= val:
            return
        w[key] = val
        sem = self.sem[key] if kind == "e" else self.lane_sem[key]
        self.eng[e].wait_ge(sem, val)

    def _deps(self, e, reads, writes):
        deps = []
        for b in reads:
            if b.last_w is not None:
                deps.append(b.last_w)
        for b in writes:
            lw = b.last_w
            if lw is not None and not (e == "pe" and lw[0] == "e" and lw[1] == e):
                deps.append(lw)
            for t in b.readers.values():
                if e == "pe" and t[0] == "e" and t[1] == e:
                    continue
                deps.append(t)
        for d in deps:
            self._wait(e, d)

    def _record(self, tok, reads, writes):
        for b in reads:
            b.readers[tok[1]] = tok
        for b in writes:
            b.last_w = tok
            b.readers = {}

    def op(self, e, fn, reads=(), writes=(), inc=True):
        self._deps(e, reads, writes)
        ins = fn(self.eng[e])
        if inc:
            self.cnt[e] += 1
            ins.then_inc(self.sem[e], 1)
            tok = ("e", e, self.cnt[e])
        else:
            tok = ("e", e, self.cnt[e] + 1)
        self._record(tok, reads, writes)
        return ins

    def dma(self, q, out, in_, reads=(), writes=(), **kw):
        lanes = self.lanes[q]
        i = self.lane_i[q]
        self.lane_i[q] = (i + 1) % len(lanes)
        lane = lanes[i]
        if lane[1] > 0:
            self._wait(q, ("d", lane[2], lane[1]))
        self._deps(q, reads, writes)
        ins = self.eng[q].dma_start(out=out, in_=in_, **kw)
        lane[1] += 16
        ins.then_inc(lane[0], 16)
        self._record(("d", lane[2], lane[1]), reads, writes)
        return ins

    def barrier(self):
        for e in self.eng:
            for k in self.sem:
                if k != e and self.cnt[k] > 0:
                    self._wait(e, ("e", k, self.cnt[k]))
            for q, lanes in self.lanes.items():
                for lane in lanes:
                    if lane[1] > 0:
                        self._wait(e, ("d", lane[2], lane[1]))

    def finish(self, bufs, e="sync"):
        for b in bufs:
            if b.last_w is not None:
                self._wait(e, b.last_w)


INPUT_SHAPES = {
    "ln1_g": [2, 1024], "w_in": [2, 1024, 1248], "attn_sink": [2, 6],
    "ssm_lam_re": [2, 2, 16, 64], "ssm_lam_im": [2, 2, 16, 64], "ssm_log_dt": [2, 2, 16],
    "ssm_b_re": [2, 2, 16, 64, 16], "ssm_b_im": [2, 2, 16, 64, 16],
    "ssm_c_re": [2, 2, 16, 16, 64], "ssm_c_im": [2, 2, 16, 16, 64],
    "ssm_d": [2, 256], "ssm_w_glu": [2, 256, 512], "ssm_b_glu": [2, 512],
    "mla_q_norm_g": [2, 192], "mla_w_uq": [2, 192, 576], "mla_kv_norm_g": [2, 128], "mla_w_ukv": [2, 128, 768],
    "out_g_attn": [2, 384], "out_g_ssm": [2, 256], "out_g_mla": [2, 384], "w_out": [2, 1024, 1024],
    "ln2_g": [2, 1024], "w_router_group": [2, 1024, 4], "b_router_group": [2, 4],
    "w_router_expert": [2, 1024, 32], "b_router_expert": [2, 32],
    "w_gate": [2, 4, 8, 1024, 256], "w_up": [2, 4, 8, 1024, 256], "w_down": [2, 4, 8, 256, 1024],
    "final_g": [1024],
}


class MK:
    def __init__(self, T=4096, depth=2, phases=None, debug=()):
        self.T = T
        self.NT = T // 128
        self.NCH = T // 512
        self.depth = depth
        self.debug = set(debug)
        self.phases = phases
        self.nc = nc = bass.Bass("TRN2", target_bir_lowering=False)
        self.es = ExitStack()
        self.trk = Trk(nc, self.es)
        self.din = {}
        self.din["x"] = nc.dram_tensor("x", [T, D], F32, kind="ExternalInput").ap()
        for k, shp in INPUT_SHAPES.items():
            self.din[k] = nc.dram_tensor(k, shp, F32, kind="ExternalInput").ap()
        self.out = nc.dram_tensor("out", [T, D], F32, kind="ExternalOutput").ap()
        self.dbg_out = []
        self.uid = 0
        self.HT = self.dram("HT", [D, T], BF16)
        self.XN2T = self.dram("XN2T", [D, T], BF16)
        self.XR = self.dram("XR", [T, D], F32)

    def sb(self, es, name, shape, dt):
        self.uid += 1
        return es.enter_context(self.nc.sbuf_tensor("%s_%d" % (name, self.uid), list(shape), dt))

    def ps(self, es, name, shape, dt):
        self.uid += 1
        return es.enter_context(self.nc.psum_tensor("%s_%d" % (name, self.uid), list(shape), dt))

    def dram(self, name, shape, dt):
        return self.nc.dram_tensor(name, list(shape), dt, kind="Internal").ap()

    def dump(self, name, ap, shape, dt, bufs):
        if name not in self.debug:
            return
        o = self.nc.dram_tensor("dbg_" + name, list(shape), dt, kind="ExternalOutput").ap()
        ob = Buf()
        self.trk.dma("sync", o, ap, reads=bufs, writes=[ob])
        self.dbg_out.append(ob)

    def dump_dram(self, name, ap, shape, dt):
        o = self.nc.dram_tensor("dbg_" + name, list(shape), dt, kind="ExternalOutput").ap()
        with ExitStack() as es:
            rows = shape[0]
            t = self.sb(es, "dd", [128, shape[1]], dt)
            for r0 in range(0, rows, 128):
                b1, ob = Buf(), Buf()
                n = min(128, rows - r0)
                self.trk.barrier()
                self.trk.dma("sync", t[0:n, :], ap[r0:r0 + n, :], writes=[b1])
                self.trk.dma("sync", o[r0:r0 + n, :], t[0:n, :], reads=[b1], writes=[ob])
                self.dbg_out.append(ob)
            self.trk.finish(self.dbg_out)
            self.trk.barrier()

    def range_reduce(self, es, ang, shape, bufs, eng="dve", scratch=None):
        Tk = self.trk
        if scratch is None:
            it = self.sb(es, "rr_i", shape, I32)
            kt = self.sb(es, "rr_k", shape, F32)
            bi, bk = Buf(), Buf()
        else:
            it, kt, bi, bk = scratch
        sl = tuple(slice(None) for _ in shape)
        Tk.op(eng, lambda e: e.tensor_scalar(out=it[sl], in0=ang, scalar1=float(1 / (2 * PI)), scalar2=None, op0=ALU.mult), reads=bufs, writes=[bi])
        Tk.op(eng, lambda e: e.tensor_copy(out=kt[sl], in_=it[sl]), reads=[bi], writes=[bk])
        Tk.op(eng, lambda e: e.scalar_tensor_tensor(out=ang, in0=kt[sl], scalar=float(-2 * PI), in1=ang, op0=ALU.mult, op1=ALU.add), reads=[bk] + bufs, writes=bufs)
        Tk.op(eng, lambda e: e.tensor_scalar(out=kt[sl], in0=ang, scalar1=float(PI), scalar2=float(-2 * PI), op0=ALU.is_gt, op1=ALU.mult), reads=bufs, writes=[bk])
        Tk.op(eng, lambda e: e.tensor_tensor(out=ang, in0=ang, in1=kt[sl], op=ALU.add), reads=[bk] + bufs, writes=bufs)
        Tk.op(eng, lambda e: e.tensor_scalar(out=kt[sl], in0=ang, scalar1=float(-PI), scalar2=float(2 * PI), op0=ALU.is_lt, op1=ALU.mult), reads=bufs, writes=[bk])
        Tk.op(eng, lambda e: e.tensor_tensor(out=ang, in0=ang, in1=kt[sl], op=ALU.add), reads=[bk] + bufs, writes=bufs)

    def consts(self):
        nc, Tk, es, T = self.nc, self.trk, self.es, self.T
        self.ident_bf = self.sb(es, "ident_bf", [128, 128], BF16)
        self.ident_f = self.sb(es, "ident_f", [128, 128], F32)
        self.ones_bf = self.sb(es, "ones_bf", [128, 128], BF16)
        self.b_const = Buf()
        bc = self.b_const
        for t in (self.ident_bf, self.ident_f):
            Tk.op("pool", lambda e, t=t: e.memset(t[:], 1.0), writes=[bc])
            Tk.op("pool", lambda e, t=t: e.affine_select(out=t[:], in_=t[:], pattern=[[-1, 128]], compare_op=ALU.is_equal, fill=0.0, base=0, channel_multiplier=1), reads=[bc], writes=[bc])
        Tk.op("pool", lambda e: e.memset(self.ones_bf[:], 1.0), writes=[bc])
        self.ROPE = self.dram("ROPE", [2, 32, T], F32)
        self.b_rope = Buf()
        with ExitStack() as tes:
            self.rope_cos = self.sb(tes, "rope_cos", [128, T], F32)
            self.rope_sin = self.sb(tes, "rope_sin", [128, T], F32)
            pi_ = self.sb(tes, "pi", [128, 1], I32)
            pf = self.sb(tes, "pf", [128, 1], F32)
            qi = self.sb(tes, "qi", [128, 1], I32)
            qf = self.sb(tes, "qf", [128, 1], F32)
            inv = self.sb(tes, "inv", [128, 1], F32)
            ti = self.sb(tes, "ti", [128, T], I32)
            tf = self.sb(tes, "tf", [128, T], F32)
            ang = self.sb(tes, "ang", [128, T], F32)
            b1, b2, b3 = Buf(), Buf(), Buf()
            Tk.op("pool", lambda e: e.iota(pi_[:], pattern=[[0, 1]], base=0, channel_multiplier=1), writes=[b1])
            Tk.op("dve", lambda e: e.tensor_copy(out=pf[:], in_=pi_[:]), reads=[b1], writes=[b1])
            Tk.op("dve", lambda e: e.tensor_scalar(out=qi[:], in0=pf[:], scalar1=-7.5, scalar2=1.0 / 16, op0=ALU.add, op1=ALU.mult), reads=[b1], writes=[b2])
            Tk.op("dve", lambda e: e.tensor_copy(out=qf[:], in_=qi[:]), reads=[b2], writes=[b2])
            Tk.op("dve", lambda e: e.scalar_tensor_tensor(out=pf[:], in0=qf[:], scalar=-16.0, in1=pf[:], op0=ALU.mult, op1=ALU.add), reads=[b1, b2], writes=[b1])
            Tk.op("act", lambda e: e.activation(out=inv[:], in_=pf[:], func=AF.Exp, scale=float(-math.log(10000.0) / 16)), reads=[b1], writes=[b3])
            Tk.op("pool", lambda e: e.iota(ti[:], pattern=[[1, T]], base=0, channel_multiplier=0), writes=[b2])
            Tk.op("dve", lambda e: e.tensor_copy(out=tf[:], in_=ti[:]), reads=[b2], writes=[b2])
            bang = Buf()
            rrs = (self.sb(tes, "rr_i", [128, T], I32), self.sb(tes, "rr_k", [128, T], F32), Buf(), Buf())
            for tab, shift in ((self.rope_sin, 0.0), (self.rope_cos, PI / 2)):
                Tk.op("dve", lambda e: e.tensor_scalar(out=ang[:], in0=tf[:], scalar1=inv[:, 0:1], scalar2=float(shift), op0=ALU.mult, op1=ALU.add), reads=[b2, b3], writes=[bang])
                self.range_reduce(tes, ang[:], [128, T], [bang], scratch=rrs)
                Tk.op("act", lambda e, tab=tab: e.activation(out=tab[:], in_=ang[:], func=AF.Sin), reads=[bang], writes=[self.b_rope])
            bt = self.b_rope
            self.b_rope = Buf()
            Tk.dma("sync", self.ROPE[0], self.rope_cos[64:96, :], reads=[bt], writes=[self.b_rope])
            Tk.dma("sync", self.ROPE[1], self.rope_sin[64:96, :], reads=[bt], writes=[self.b_rope])
            Tk.barrier()
        self.GATE = self.sb(es, "GATE", [128, self.NT, 32], F32)

    def p1(self, l, xsrc, xbuf_key):
        nc, Tk, T, NCH = self.nc, self.trk, self.T, self.NCH
        m = self.mix
        with ExitStack() as es:
            w_in_bf = self.sb(es, "w_in_bf", [128, 8, IN_COLS], BF16)
            w_qa = self.sb(es, "w_qa", [128, 8, 384], BF16)
            w_kr = self.sb(es, "w_kr", [128, 8, 96], BF16)
            w_sw = self.sb(es, "w_sw", [128, 8, 96], BF16)
            g1bc = self.sb(es, "g1bc", [128, D], F32)
            xt = [self.sb(es, "xt", [128, D], F32) for _ in range(2)]
            junk = self.sb(es, "junk", [128, D], BF16)
            xn = [self.sb(es, "xn", [128, D], BF16) for _ in range(2)]
            xnT = [self.sb(es, "xnT", [128, 8, 512], BF16) for _ in range(2)]
            ss = [self.sb(es, "ss", [128, 1], F32) for _ in range(2)]
            rstd = [self.sb(es, "rstd", [128, 1], F32) for _ in range(2)]
            kt1 = self.sb(es, "kt1", [128, 512], F32)
            kt2 = self.sb(es, "kt2", [128, 512], F32)
            rp = [self.sb(es, "rp", [128, 2, 512], F32) for _ in range(2)]
            brp = [Buf(), Buf()]
            pT = [self.ps(es, "pT", [128, 1024], BF16) for _ in range(2)]
            pm = [self.ps(es, "pm", [128, 512], F32) for _ in range(4)]
            pva = self.ps(es, "pva", [128, 512], F32)
            bw = Buf()
            bg = Buf()
            Tk.dma("pool", w_in_bf[:], self.din["w_in"][l].rearrange("(kc p) n -> p kc n", p=128), writes=[bw])
            Tk.dma("sync", g1bc[:], self.din["ln1_g"][l:l + 1, :].partition_broadcast(128), writes=[bg])
            bw2 = Buf()
            for j, h in enumerate([0, 3, 1, 4, 2, 5]):
                Tk.op("pool", lambda e, j=j, h=h: e.tensor_copy(out=w_qa[:, :, j * 64:(j + 1) * 64], in_=w_in_bf[:, :, h * 64:(h + 1) * 64]), reads=[bw], writes=[bw2])
            Tk.op("pool", lambda e: e.memset(w_kr[:, :, 0:64], 0.0), writes=[bw2])
            Tk.op("pool", lambda e: e.memset(w_sw[:, :, 0:64], 0.0), writes=[bw2])
            Tk.op("pool", lambda e: e.tensor_copy(out=w_kr[:, :, 64:96], in_=w_in_bf[:, :, 1216:1248]), reads=[bw], writes=[bw2])
            Tk.op("act", lambda e: e.mul(out=w_sw[:, :, 64:80], in_=w_in_bf[:, :, 1232:1248], mul=-1.0), reads=[bw], writes=[bw2])
            Tk.op("act", lambda e: e.copy(out=w_sw[:, :, 80:96], in_=w_in_bf[:, :, 1216:1232]), reads=[bw], writes=[bw2])
            Tk.op("pool", lambda e: e.memset(m["VA"][:, :, :, 64:65], 1.0), writes=[Tk.b("VAones", l)])
            bxt = [Buf(), Buf()]
            bxn = [Buf(), Buf()]
            bss = [Buf(), Buf()]
            bxnT = [Buf(), Buf()]
            bpT = [Buf(), Buf()]
            bpm = [Buf() for _ in range(4)]
            bpva = Buf()
            bjunk = Buf()
            bkt = Buf()
            ev = [0]

            def evac(out, in_, reads, writes):
                e = ("act", "dve")[ev[0] % 2]
                ev[0] += 1
                if e == "act":
                    Tk.op("act", lambda en: en.copy(out=out, in_=in_), reads=reads, writes=writes)
                else:
                    Tk.op("dve", lambda en: en.tensor_copy(out=out, in_=in_), reads=reads, writes=writes)

            pmi = [0]
            for c in range(NCH):
                cb = c % 2
                cols = slice(c * 512, (c + 1) * 512)
                for tt in range(4):
                    i = 4 * c + tt
                    p = i % 2
                    Tk.dma("sync", xt[p][:], xsrc[i * 128:(i + 1) * 128, :], reads=[Tk.b(xbuf_key, i)], writes=[bxt[p]])
                    Tk.op("dve", lambda e: e.memset(ss[p][:], 0.0), writes=[bss[p]])
                    Tk.op("act", lambda e: e.activation(out=junk[:], in_=xt[p][:], func=AF.Square, accum_out=ss[p][:]), reads=[bxt[p], bss[p]], writes=[bjunk, bss[p]])
                    Tk.op("dve", lambda e: e.tensor_scalar(out=ss[p][:], in0=ss[p][:], scalar1=1.0 / D, scalar2=EPS, op0=ALU.mult, op1=ALU.add), reads=[bss[p]], writes=[bss[p]])
                    Tk.op("act", lambda e: e.activation(out=ss[p][:], in_=ss[p][:], func=AF.Sqrt), reads=[bss[p]], writes=[bss[p]])
                    Tk.op("dve", lambda e: e.reciprocal(out=rstd[p][:], in_=ss[p][:]), reads=[bss[p]], writes=[bss[p]])
                    Tk.op("dve", lambda e: e.scalar_tensor_tensor(out=xn[p][:], in0=xt[p][:], scalar=rstd[p][:, 0:1], in1=g1bc[:], op0=ALU.mult, op1=ALU.mult), reads=[bxt[p], bss[p], bg], writes=[bxn[p]])
                    for kc in range(8):
                        Tk.op("pe", lambda e, kc=kc: e.transpose(pT[p][:, kc * 128:(kc + 1) * 128], xn[p][:, kc * 128:(kc + 1) * 128], self.ident_bf[:]),
                              reads=[bxn[p], self.b_const], writes=[bpT[p]], inc=(kc == 7))
                    evac(xnT[cb][:, :, tt * 128:(tt + 1) * 128], pT[p][:].rearrange("p (k t) -> p k t", k=8), [bpT[p]], [bxnT[cb]])
                for v in range(2):
                    Tk.dma("sync", rp[cb][64:96, v, :], self.ROPE[v, :, cols], reads=[self.b_rope], writes=[brp[cb]])
                groups = [
                    (w_qa, 0, 128, ("QA", 0)), (w_qa, 128, 128, ("QA", 1)), (w_qa, 256, 128, ("QA", 2)),
                    (w_in_bf, 384, 128, ("KA", None)),
                    (w_in_bf, 640, 128, ("U", 0)), (w_in_bf, 768, 128, ("U", 1)),
                    (w_in_bf, 896, 128, ("CQ", 0)), (w_in_bf, 1024, 64, ("CQ", 1)),
                    (w_in_bf, 1088, 128, ("CKV", None)),
                    (w_kr, 0, 96, ("KR", "main")), (w_sw, 0, 96, ("KR", "swap")),
                ]
                for (wt, c0, M, (name, sub)) in groups:
                    k = pmi[0] % 4
                    pmi[0] += 1
                    for kc in range(8):
                        Tk.op("pe", lambda e, kc=kc: e.matmul(pm[k][0:M, :], lhsT=wt[:, kc, c0:c0 + M], rhs=xnT[cb][:, kc, :], start=(kc == 0), stop=(kc == 7)),
                              reads=[bw, bw2, bxnT[cb]], writes=[bpm[k]], inc=(kc == 7))
                    if name == "KR":
                        rows = slice(64, 96)
                        if sub == "main":
                            Tk.op("dve", lambda e: e.tensor_tensor(out=kt1[rows, :], in0=pm[k][rows, :], in1=rp[cb][rows, 0, :], op=ALU.mult), reads=[bpm[k], brp[cb]], writes=[bkt])
                        else:
                            Tk.op("dve", lambda e: e.tensor_tensor(out=kt2[rows, :], in0=pm[k][rows, :], in1=rp[cb][rows, 1, :], op=ALU.mult), reads=[bpm[k], brp[cb]], writes=[bkt])
                            Tk.op("dve", lambda e: e.tensor_tensor(out=m["KR"][rows, cols], in0=kt1[rows, :], in1=kt2[rows, :], op=ALU.add), reads=[bkt], writes=[Tk.b("KR", l, c)])
                    else:
                        dst = m[name]
                        o = dst[0:M, cols] if sub is None else dst[0:M, sub, cols]
                        evac(o, pm[k][0:M, :], [bpm[k]], [Tk.b(name, l, c)])
                for tt in range(4):
                    i = 4 * c + tt
                    for kc in range(8):
                        Tk.op("pe", lambda e, kc=kc: e.matmul(pva[:, tt * 128:(tt + 1) * 128], lhsT=xnT[cb][:, kc, tt * 128:(tt + 1) * 128], rhs=w_in_bf[:, kc, 512:640], start=(kc == 0), stop=(kc == 7)),
                              reads=[bw, bxnT[cb]], writes=[bpva], inc=(kc == 7))
                    Tk.op("dve", lambda e: e.tensor_copy(out=m["VA"][:, i, :, 0:64], in_=pva[:, tt * 128:(tt + 1) * 128].rearrange("p (h d) -> p h d", h=2)), reads=[bpva], writes=[Tk.b("VA", l, i)])
            Tk.barrier()

    def alloc_mix(self, es_list):
        T, NT = self.T, self.NT
        m = self.mix = {}
        es_mla, es_u, es_attn = es_list
        m["CQ"] = self.sb(es_mla, "CQ", [128, 2, T], BF16)
        m["CKV"] = self.sb(es_mla, "CKV", [128, T], BF16)
        m["KR"] = self.sb(es_mla, "KR", [128, T], BF16)
        m["U"] = self.sb(es_u, "U", [128, 2, T], BF16)
        m["QA"] = self.sb(es_attn, "QA", [128, 3, T], BF16)
        m["KA"] = self.sb(es_attn, "KA", [128, T], BF16)
        m["VA"] = self.sb(es_attn, "VA", [128, NT, 2, 65], BF16)

    def build(self):
        Tk = self.trk
        self.consts()
        xsrc, xkey = self.din["x"], "xin"
        for l in range(self.depth):
            es_mla, es_u, es_attn = ExitStack(), ExitStack(), ExitStack()
            self.alloc_mix([es_mla, es_u, es_attn])
            self.p1(l, xsrc, xkey)
            if "p1" in self.debug:
                m, T, NT = self.mix, self.T, self.NT
                allb = list(Tk.bufs.values())
                self.debug |= {"QA", "KA", "VA", "U", "CQ", "CKV", "KR"}
                self.dump("QA", m["QA"][:], [128, 3, T], BF16, allb)
                self.dump("KA", m["KA"][:], [128, T], BF16, allb)
                self.dump("VA", m["VA"][:], [128, NT, 2, 65], BF16, allb)
                self.dump("U", m["U"][:], [128, 2, T], BF16, allb)
                self.dump("CQ", m["CQ"][:], [128, 2, T], BF16, allb)
                self.dump("CKV", m["CKV"][:], [128, T], BF16, allb)
                self.dump("KR", m["KR"][:], [128, T], BF16, allb)
            if self.phases == "p1":
                Tk.barrier()
                es_attn.close(); es_u.close(); es_mla.close()
                break
            self.p2(l)
            es_attn.close()
            if self.phases == "p2":
                self.dump_dram("HT", self.HT, [D, self.T], BF16)
                es_u.close(); es_mla.close()
                break
            self.p3(l)
            es_u.close()
            if self.phases == "p3":
                self.dump_dram("HT", self.HT, [D, self.T], BF16)
                es_mla.close()
                break
            self.p4(l)
            es_mla.close()
            if self.phases == "p4":
                self.dump_dram("HT", self.HT, [D, self.T], BF16)
                break
            self.p5(l, xsrc, xkey)
            if self.phases == "p5":
                self.dump("GATE", self.GATE[:], [128, self.NT, 32], F32, list(Tk.bufs.values()))
                self.dump_dram("XR", self.XR, [self.T, D], F32)
                break
            self.p6(l, last=(l == self.depth - 1))
            xsrc, xkey = self.XR, "XR"
        Tk.finish(self.dbg_out)
        Tk.finish([b for k, b in Tk.bufs.items() if k[0] == "OUT"])
        Tk.barrier()
        self.es.close()
        return self.nc


def _p2(self, l):
    nc, Tk, T, NT = self.nc, self.trk, self.T, self.NT
    m = self.mix
    QA, KA, VA = m["QA"], m["KA"], m["VA"]
    slopes = [2.0 ** (-8.0 * (h + 1) / 6) for h in range(6)]
    with ExitStack() as es:
        bias = self.sb(es, "bias", [128, 6, 384], F32)
        di = self.sb(es, "di", [128, 384], I32)
        df = self.sb(es, "df", [128, 384], F32)
        dn = self.sb(es, "dn", [128, 384], F32)
        pen = self.sb(es, "pen", [128, 384], F32)
        esk = self.sb(es, "esk", [128, 6], F32)
        gat = self.sb(es, "gat", [64, 6], F32)
        ones_f = self.sb(es, "ones_f", [128, 64], F32)
        NB = 3
        tmp = [self.sb(es, "tmp", [128, 384], F32) for _ in range(NB)]
        PT = [self.sb(es, "PT", [128, 384], BF16) for _ in range(NB)]
        Osb = self.sb(es, "Osb", [65, 768], F32)
        rd = self.sb(es, "rd", [65, 768], F32)
        o = self.sb(es, "o", [64, 768], F32)
        sq = self.sb(es, "sq", [64, 768], BF16)
        rs = self.sb(es, "rs", [64, 128], F32)
        hT = [self.sb(es, "hT", [64, 6, 128], BF16) for _ in range(2)]
        pS = [self.ps(es, "pS", [128, 512], F32) for _ in range(NB)]
        pO = [self.ps(es, "pO", [128, 512], F32) for _ in range(2)]
        pB = [self.ps(es, "pB", [128, 512], F32) for _ in range(2)]
        pSS = self.ps(es, "pSS", [128, 512], F32)
        bb = Buf()
        Tk.op("pool", lambda e: e.iota(di[:].rearrange("p (a q) -> p a q", a=3), pattern=[[128, 3], [-1, 128]], base=-128, channel_multiplier=1), writes=[bb])
        Tk.op("dve", lambda e: e.tensor_copy(out=df[:], in_=di[:]), reads=[bb], writes=[bb])
        Tk.op("dve", lambda e: e.tensor_scalar(out=dn[:], in0=df[:], scalar1=-1.0, scalar2=None, op0=ALU.mult), reads=[bb], writes=[bb])
        Tk.op("dve", lambda e: e.tensor_tensor(out=df[:], in0=df[:], in1=dn[:], op=ALU.max), reads=[bb], writes=[bb])
        Tk.op("dve", lambda e: e.tensor_scalar(out=pen[:], in0=df[:], scalar1=128.0, scalar2=-30000.0, op0=ALU.is_gt, op1=ALU.mult), reads=[bb], writes=[bb])
        for h in range(6):
            Tk.op("dve", lambda e: e.scalar_tensor_tensor(out=bias[:, h, :], in0=df[:], scalar=float(-slopes[h]), in1=pen[:], op0=ALU.mult, op1=ALU.add), reads=[bb], writes=[bb])
        Tk.op("pool", lambda e: e.memset(ones_f[:], 1.0), writes=[bb])
        Tk.dma("sync", esk[64:65, :], self.din["attn_sink"][l:l + 1, :], writes=[bb])
        Tk.op("act", lambda e: e.activation(out=esk[64:65, :], in_=esk[64:65, :], func=AF.Exp), reads=[bb], writes=[bb])
        Tk.dma("sync", gat[:], self.din["out_g_attn"][l].rearrange("(h d) -> d h", d=64), writes=[bb], allow_slow_non_contiguous=True)
        allin = [Tk.b(n, l, c) for n in ("QA", "KA") for c in range(self.NCH)] + [Tk.b("VA", l, i) for i in range(NT)] + [Tk.b("VAones", l)]
        btmp, bPT, bpS = [[Buf() for _ in range(NB)] for _ in range(3)]
        bpO, bOsb, brd, bpB, bo, bsq, bpSS, brs = [Buf() for _ in range(8)]
        bhT = [Buf(), Buf()]
        steps = [(i, h) for i in range(NT) for h in range(6)]

        def geo(i):
            dds = [dd for dd in range(3) if 0 <= i + dd - 1 < NT]
            return dds, dds[0] * 128, (dds[-1] + 1) * 128

        def stS(n):
            i, h = steps[n]
            kv, j = h // 3, h % 3
            rows = slice(kv * 64, kv * 64 + 64)
            k = n % NB
            dds, c0, c1 = geo(i)
            qs = slice(i * 128, (i + 1) * 128)
            for dd in dds:
                ks = slice((i + dd - 1) * 128, (i + dd) * 128)
                Tk.op("pe", lambda e: e.matmul(pS[k][:, dd * 128:(dd + 1) * 128], lhsT=KA[rows, ks], rhs=QA[rows, j, qs], start=True, stop=True),
                      reads=allin, writes=[bpS[k]], inc=(dd == dds[-1]))

        def stP(n):
            i, h = steps[n]
            kv = h // 3
            k = n % NB
            dds, c0, c1 = geo(i)
            Tk.op("dve", lambda e: e.scalar_tensor_tensor(out=tmp[k][:, c0:c1], in0=pS[k][:, c0:c1], scalar=0.125, in1=bias[:, h, c0:c1], op0=ALU.mult, op1=ALU.add),
                  reads=[bpS[k], bb], writes=[btmp[k]])
            Tk.op("act", lambda e: e.activation(out=PT[k][:, c0:c1], in_=tmp[k][:, c0:c1], func=AF.Exp), reads=[btmp[k]], writes=[bPT[k]])
            po = pO[h // 4][0:65, (h % 4) * 128:(h % 4 + 1) * 128]
            for dd in dds:
                Tk.op("pe", lambda e: e.matmul(po, lhsT=VA[:, i + dd - 1, kv, :], rhs=PT[k][:, dd * 128:(dd + 1) * 128], start=(dd == dds[0]), stop=(dd == dds[-1])),
                      reads=allin + [bPT[k]], writes=[bpO], inc=(dd == dds[-1]))

        def epilogue(i):
            qs = slice(i * 128, (i + 1) * 128)
            hb = i % 2
            o3 = o[:].rearrange("p (h q) -> p h q", h=6)
            g = [[] for _ in range(6)]
            g[0].append(lambda: Tk.op("act", lambda e: e.copy(out=Osb[0:65, 0:512], in_=pO[0][0:65, :]), reads=[bpO], writes=[bOsb]))
            g[0].append(lambda: Tk.op("act", lambda e: e.copy(out=Osb[0:65, 512:768], in_=pO[1][0:65, 0:256]), reads=[bpO], writes=[bOsb]))
            g[1].append(lambda: Tk.op("dve", lambda e: e.tensor_tensor(out=rd[64:65, :].rearrange("p (h q) -> p h q", h=6), in0=Osb[64:65, :].rearrange("p (h q) -> p h q", h=6),
                                                                       in1=esk[64:65, :].unsqueeze(2).to_broadcast([1, 6, 128]), op=ALU.add), reads=[bOsb, bb], writes=[brd]))
            g[1].append(lambda: Tk.op("act", lambda e: e.activation(out=rd[64:65, :], in_=rd[64:65, :], func=AF.Ln), reads=[brd], writes=[brd]))
            g[1].append(lambda: Tk.op("act", lambda e: e.activation(out=rd[64:65, :], in_=rd[64:65, :], func=AF.Exp, scale=-1.0), reads=[brd], writes=[brd]))
            g[2].append(lambda: Tk.op("pe", lambda e: e.matmul(pB[0][0:64, :], lhsT=ones_f[64:65, 0:64], rhs=rd[64:65, 0:512], start=True, stop=True), reads=[brd, bb], writes=[bpB]))
            g[2].append(lambda: Tk.op("pe", lambda e: e.matmul(pB[1][0:64, 0:256], lhsT=ones_f[64:65, 0:64], rhs=rd[64:65, 512:768], start=True, stop=True), reads=[brd, bb], writes=[bpB]))
            g[2].append(lambda: Tk.op("dve", lambda e: e.tensor_tensor(out=o[:, 0:512], in0=Osb[0:64, 0:512], in1=pB[0][0:64, :], op=ALU.mult), reads=[bOsb, bpB], writes=[bo]))
            g[2].append(lambda: Tk.op("dve", lambda e: e.tensor_tensor(out=o[:, 512:768], in0=Osb[0:64, 512:768], in1=pB[1][0:64, 0:256], op=ALU.mult), reads=[bOsb, bpB], writes=[bo]))
            g[3].append(lambda: Tk.op("act", lambda e: e.activation(out=sq[:], in_=o[:], func=AF.Square), reads=[bo], writes=[bsq]))

            def ssq():
                for h in range(6):
                    Tk.op("pe", lambda e: e.matmul(pSS[0:64, 0:128], lhsT=self.ones_bf[0:64, 0:64], rhs=sq[:, h * 128:(h + 1) * 128], start=(h == 0), stop=(h == 5)),
                          reads=[bsq, self.b_const], writes=[bpSS], inc=(h == 5))
            g[4].append(ssq)
            g[4].append(lambda: Tk.op("dve", lambda e: e.tensor_scalar(out=rs[:], in0=pSS[0:64, 0:128], scalar1=1.0 / 384, scalar2=EPS, op0=ALU.mult, op1=ALU.add), reads=[bpSS], writes=[brs]))
            g[4].append(lambda: Tk.op("act", lambda e: e.activation(out=rs[:], in_=rs[:], func=AF.Ln), reads=[brs], writes=[brs]))
            g[5].append(lambda: Tk.op("act", lambda e: e.activation(out=rs[:], in_=rs[:], func=AF.Exp, scale=-0.5), reads=[brs], writes=[brs]))
            g[5].append(lambda: Tk.op("dve", lambda e: e.tensor_tensor(out=o3, in0=o3, in1=rs[:].unsqueeze(1).to_broadcast([64, 6, 128]), op=ALU.mult), reads=[bo, brs], writes=[bo]))
            g[5].append(lambda: Tk.op("dve", lambda e: e.tensor_tensor(out=hT[hb][:], in0=o3, in1=gat[:].unsqueeze(2).to_broadcast([64, 6, 128]), op=ALU.mult), reads=[bo, bb], writes=[bhT[hb]]))
            g[5].append(lambda: Tk.dma("sync", self.HT[0:384, qs].rearrange("(h d) q -> d h q", d=64), hT[hb][:], reads=[bhT[hb]], writes=[Tk.b("HT", l, "attn", i)]))
            return g

        NS_ = len(steps)
        pend = []
        for n in range(NS_ + 1):
            if n < NS_:
                stS(n)
            if n >= 1:
                stP(n - 1)
                i, h = steps[n - 1]
                if h == 5:
                    assert not pend
                    pend = epilogue(i)
                if pend:
                    for f in pend.pop(0):
                        f()
        while pend:
            for f in pend.pop(0):
                f()
        Tk.barrier()


MK.p2 = _p2


def rev_ap(a):
    ap = [list(d) for d in a.ap]
    step, n = ap[-1]
    ap[-1] = [-step, n]
    return bass.AP(a.tensor, a.offset + (n - 1) * step, ap)


def _p3(self, l):
    nc, Tk, T = self.nc, self.trk, self.T
    Lc = 128
    NC = T // Lc
    U = self.mix["U"]
    din = self.din
    allU = [Tk.b("U", l, c) for c in range(self.NCH)]
    with ExitStack() as es:
        CS = self.sb(es, "CS", [128, 32, 2 * Lc], F32)
        G1 = self.sb(es, "G1", [128, 32, 128], BF16)
        G2 = self.sb(es, "G2", [128, 32, 128], BF16)
        C1 = self.sb(es, "C1", [128, 32, 128], BF16)
        C2 = self.sb(es, "C2", [128, 32, 128], BF16)
        R = self.sb(es, "R", [128, 32, 128], F32)
        mag = self.sb(es, "mag", [128, 32], F32)
        diagD = self.sb(es, "diagD", [128, 2, 128], BF16)
        carry = self.sb(es, "carry", [128, 32], F32)
        bP = Buf()
        with ExitStack() as tes:
            xd = self.sb(tes, "xd", [32, 2, 128], F32)
            LR = self.sb(tes, "LR", [128, 32], F32)
            LI = self.sb(tes, "LI", [128, 32], F32)
            dt = self.sb(tes, "dt", [128, 32], F32)
            th = self.sb(tes, "th", [128, 32], F32)
            cth = self.sb(tes, "cth", [128, 32], F32)
            sth = self.sb(tes, "sth", [128, 32], F32)
            ar = self.sb(tes, "ar", [128, 32], F32)
            ai = self.sb(tes, "ai", [128, 32], F32)
            den = self.sb(tes, "den", [128, 32], F32)
            t1 = self.sb(tes, "t1", [128, 32], F32)
            t2 = self.sb(tes, "t2", [128, 32], F32)
            fr = self.sb(tes, "fr", [128, 32], F32)
            fi = self.sb(tes, "fi", [128, 32], F32)
            frs = self.sb(tes, "frs", [128, 32], F32)
            fis = self.sb(tes, "fis", [128, 32], F32)
            sgn = self.sb(tes, "sgn", [128, 1], F32)
            Pm = self.sb(tes, "Pm", [128, 32, 16], F32)
            Qm = self.sb(tes, "Qm", [128, 32, 16], F32)
            GT1 = self.sb(tes, "GT1", [128, 32, 16], F32)
            GT2 = self.sb(tes, "GT2", [128, 32, 16], F32)
            gtmp = self.sb(tes, "gtmp", [128, 32, 16], F32)
            TB = self.sb(tes, "TB", [128, 128], F32)
            rowmask = self.sb(tes, "rowmask", [128, 8], F32)
            CT = self.sb(tes, "CT", [128, 2, 128], F32)
            crl = self.sb(tes, "crl", [128, 64], F32)
            cil = self.sb(tes, "cil", [128, 64], F32)
            shift = self.sb(tes, "shift", [128, 128], F32)
            dcol = self.sb(tes, "dcol", [128, 2], F32)
            iot = self.sb(tes, "iot", [128, Lc], I32)
            iof = self.sb(tes, "iof", [128, Lc], F32)
            ang = self.sb(tes, "ang", [128, 8, Lc], F32)
            pp_ = self.ps(tes, "pprep", [128, 512], F32)
            b = Buf()
            bps = Buf()
            for name, dst in (("ssm_lam_re", LR), ("ssm_lam_im", LI)):
                src = din[name][l].rearrange("d g n -> (d g) n")
                Tk.dma("sync", xd[:, 0, 0:64], src, writes=[b])
                Tk.dma("sync", xd[:, 0, 64:128], src, writes=[b])
                Tk.op("pe", lambda e: e.matmul(pp_[:, 0:32], lhsT=xd[:, 0, :], rhs=self.ident_f[0:32, 0:32], start=True, stop=True), reads=[b, self.b_const], writes=[bps])
                Tk.op("dve", lambda e: e.tensor_copy(out=dst[:], in_=pp_[:, 0:32]), reads=[bps], writes=[b])
            Tk.dma("sync", dt[:], din["ssm_log_dt"][l:l + 1].rearrange("o d g -> o (d g)").partition_broadcast(128), writes=[b])
            Tk.op("act", lambda e: e.activation(out=dt[:], in_=dt[:], func=AF.Exp), reads=[b], writes=[b])
            Tk.op("dve", lambda e: e.tensor_tensor(out=t1[:], in0=LR[:], in1=dt[:], op=ALU.mult), reads=[b], writes=[b])
            Tk.op("act", lambda e: e.activation(out=mag[:], in_=t1[:], func=AF.Exp), reads=[b], writes=[bP])
            Tk.op("dve", lambda e: e.tensor_tensor(out=th[:], in0=LI[:], in1=dt[:], op=ALU.mult), reads=[b], writes=[b])
            rrs_s = (self.sb(tes, "rr_i", [128, 32], I32), self.sb(tes, "rr_k", [128, 32], F32), Buf(), Buf())
            for dst, shf in ((sth, 0.0), (cth, PI / 2)):
                Tk.op("dve", lambda e: e.tensor_scalar(out=t2[:], in0=th[:], scalar1=float(shf), scalar2=None, op0=ALU.add), reads=[b], writes=[b])
                self.range_reduce(tes, t2[:], [128, 32], [b], scratch=rrs_s)
                Tk.op("act", lambda e: e.activation(out=dst[:], in_=t2[:], func=AF.Sin), reads=[b], writes=[b])
            Tk.op("dve", lambda e: e.tensor_tensor(out=ar[:], in0=mag[:], in1=cth[:], op=ALU.mult), reads=[b, bP], writes=[b])
            Tk.op("dve", lambda e: e.tensor_tensor(out=ai[:], in0=mag[:], in1=sth[:], op=ALU.mult), reads=[b, bP], writes=[b])
            Tk.op("dve", lambda e: e.tensor_scalar(out=ar[:], in0=ar[:], scalar1=-1.0, scalar2=None, op0=ALU.add), reads=[b], writes=[b])
            Tk.op("dve", lambda e: e.tensor_tensor(out=den[:], in0=LR[:], in1=LR[:], op=ALU.mult), reads=[b], writes=[b])
            Tk.op("dve", lambda e: e.tensor_tensor(out=t1[:], in0=LI[:], in1=LI[:], op=ALU.mult), reads=[b], writes=[b])
            Tk.op("dve", lambda e: e.tensor_tensor(out=den[:], in0=den[:], in1=t1[:], op=ALU.add), reads=[b], writes=[b])
            Tk.op("dve", lambda e: e.reciprocal(out=den[:], in_=den[:]), reads=[b], writes=[b])
            Tk.op("dve", lambda e: e.tensor_tensor(out=t1[:], in0=ar[:], in1=LR[:], op=ALU.mult), reads=[b], writes=[b])
            Tk.op("dve", lambda e: e.tensor_tensor(out=t2[:], in0=ai[:], in1=LI[:], op=ALU.mult), reads=[b], writes=[b])
            Tk.op("dve", lambda e: e.tensor_tensor(out=t1[:], in0=t1[:], in1=t2[:], op=ALU.add), reads=[b], writes=[b])
            Tk.op("dve", lambda e: e.tensor_tensor(out=fr[:], in0=t1[:], in1=den[:], op=ALU.mult), reads=[b], writes=[b])
            Tk.op("dve", lambda e: e.tensor_tensor(out=t1[:], in0=ai[:], in1=LR[:], op=ALU.mult), reads=[b], writes=[b])
            Tk.op("dve", lambda e: e.tensor_tensor(out=t2[:], in0=ar[:], in1=LI[:], op=ALU.mult), reads=[b], writes=[b])
            Tk.op("dve", lambda e: e.tensor_tensor(out=t1[:], in0=t1[:], in1=t2[:], op=ALU.subtract), reads=[b], writes=[b])
            Tk.op("dve", lambda e: e.tensor_tensor(out=fi[:], in0=t1[:], in1=den[:], op=ALU.mult), reads=[b], writes=[b])
            Tk.op("pool", lambda e: e.memset(sgn[0:64, :], 1.0), writes=[b])
            Tk.op("pool", lambda e: e.memset(sgn[64:128, :], -1.0), writes=[b])
            Tk.op("dve", lambda e: e.tensor_scalar(out=frs[:], in0=fr[:], scalar1=sgn[:, 0:1], scalar2=None, op0=ALU.mult), reads=[b], writes=[b])
            Tk.op("dve", lambda e: e.tensor_scalar(out=fis[:], in0=fi[:], scalar1=sgn[:, 0:1], scalar2=-1.0, op0=ALU.mult, op1=ALU.mult), reads=[b], writes=[b])
            bre = din["ssm_b_re"][l].rearrange("d g n c -> n (d g) c")
            bim = din["ssm_b_im"][l].rearrange("d g n c -> n (d g) c")
            Tk.dma("sync", Pm[0:64], bre, writes=[b])
            Tk.dma("sync", Pm[64:128], bim, writes=[b])
            Tk.dma("sync", Qm[0:64], bim, writes=[b])
            Tk.dma("sync", Qm[64:128], bre, writes=[b])

            def bc3(t):
                return t[:].unsqueeze(2).to_broadcast([128, 32, 16])
            Tk.op("dve", lambda e: e.tensor_tensor(out=GT1[:], in0=Pm[:], in1=bc3(fr), op=ALU.mult), reads=[b], writes=[b])
            Tk.op("dve", lambda e: e.tensor_tensor(out=gtmp[:], in0=Qm[:], in1=bc3(fis), op=ALU.mult), reads=[b], writes=[b])
            Tk.op("dve", lambda e: e.tensor_tensor(out=GT1[:], in0=GT1[:], in1=gtmp[:], op=ALU.add), reads=[b], writes=[b])
            Tk.op("dve", lambda e: e.tensor_tensor(out=GT2[:], in0=Qm[:], in1=bc3(frs), op=ALU.mult), reads=[b], writes=[b])
            Tk.op("dve", lambda e: e.tensor_tensor(out=gtmp[:], in0=Pm[:], in1=bc3(fi), op=ALU.mult), reads=[b], writes=[b])
            Tk.op("dve", lambda e: e.tensor_tensor(out=GT2[:], in0=GT2[:], in1=gtmp[:], op=ALU.add), reads=[b], writes=[b])
            Tk.op("pool", lambda e: e.memset(rowmask[:], 1.0), writes=[b])
            Tk.op("pool", lambda e: e.affine_select(out=rowmask[:], in_=rowmask[:], pattern=[[-16, 8]], compare_op=ALU.is_ge, fill=0.0, base=0, channel_multiplier=1), reads=[b], writes=[b])
            Tk.op("pool", lambda e: e.affine_select(out=rowmask[:], in_=rowmask[:], pattern=[[16, 8]], compare_op=ALU.is_ge, fill=0.0, base=15, channel_multiplier=-1), reads=[b], writes=[b])
            for GT, Gd in ((GT1, G1), (GT2, G2)):
                for d in range(2):
                    for ct in range(2):
                        dg0 = d * 16 + ct * 8
                        Tk.op("pe", lambda e: e.transpose(pp_[:, 0:128], GT[:, dg0:dg0 + 8, :].rearrange("p g c -> p (g c)"), self.ident_f[:]), reads=[b, self.b_const], writes=[bps])
                        Tk.op("act", lambda e: e.copy(out=TB[:], in_=pp_[:, 0:128]), reads=[bps], writes=[b])
                        for gg in range(8):
                            Tk.op("dve", lambda e: e.tensor_scalar(out=Gd[:, dg0 + gg, :], in0=TB[:], scalar1=rowmask[:, gg:gg + 1], scalar2=None, op0=ALU.mult), reads=[b], writes=[bP])
            Tk.op("pool", lambda e: e.memset(C1[:], 0.0), writes=[bP])
            Tk.op("pool", lambda e: e.memset(C2[:], 0.0), writes=[bP])
            cre = din["ssm_c_re"][l].rearrange("d g c n -> (d g c) n")
            cim = din["ssm_c_im"][l].rearrange("d g c n -> (d g c) n")
            for d in range(2):
                for ct in range(2):
                    dg0 = d * 16 + ct * 8
                    r0 = (d * 2 + ct) * 128
                    Tk.dma("sync", crl[:], cre[r0:r0 + 128, :], writes=[b])
                    Tk.dma("sync", cil[:], cim[r0:r0 + 128, :], writes=[b])
                    Tk.op("act", lambda e: e.copy(out=CT[:, 0, 0:64], in_=crl[:]), reads=[b], writes=[b])
                    Tk.op("act", lambda e: e.mul(out=CT[:, 0, 64:128], in_=cil[:], mul=-1.0), reads=[b], writes=[b])
                    Tk.op("act", lambda e: e.mul(out=CT[:, 1, 0:64], in_=cil[:], mul=-1.0), reads=[b], writes=[b])
                    Tk.op("act", lambda e: e.mul(out=CT[:, 1, 64:128], in_=crl[:], mul=-1.0), reads=[b], writes=[b])
                    for v, Cd in ((0, C1), (1, C2)):
                        Tk.op("pe", lambda e: e.transpose(pp_[:, 0:128], CT[:, v, :], self.ident_f[:]), reads=[b, self.b_const], writes=[bps])
                        for gg in range(8):
                            Tk.op("dve", lambda e: e.tensor_copy(out=Cd[:, dg0 + gg, gg * 16:(gg + 1) * 16], in_=pp_[:, gg * 16:(gg + 1) * 16]), reads=[bps], writes=[bP])
            Tk.op("pool", lambda e: e.iota(iot[:], pattern=[[1, Lc]], base=1, channel_multiplier=0), writes=[b])
            Tk.op("dve", lambda e: e.tensor_copy(out=iof[:], in_=iot[:]), reads=[b], writes=[b])
            bang = Buf()
            rrs_b = (self.sb(tes, "rr_i", [128, 8, Lc], I32), self.sb(tes, "rr_k", [128, 8, Lc], F32), Buf(), Buf())
            for q8 in range(4):
                for off, shf in ((Lc, 0.0), (0, PI / 2)):
                    Tk.op("dve", lambda e: e.tensor_tensor(out=ang[:], in0=iof[:].unsqueeze(1).to_broadcast([128, 8, Lc]), in1=th[:, q8 * 8:(q8 + 1) * 8].unsqueeze(2).to_broadcast([128, 8, Lc]), op=ALU.mult), reads=[b], writes=[bang])
                    if shf:
                        Tk.op("dve", lambda e: e.tensor_scalar(out=ang[:], in0=ang[:], scalar1=float(shf), scalar2=None, op0=ALU.add), reads=[bang], writes=[bang])
                    self.range_reduce(tes, ang[:], [128, 8, Lc], [bang], scratch=rrs_b)
                    Tk.op("act", lambda e: e.activation(out=CS[:, q8 * 8:(q8 + 1) * 8, off:off + Lc], in_=ang[:], func=AF.Sin), reads=[bang], writes=[bP])
            Tk.op("dve", lambda e: e.tensor_copy(out=shift[:, 0:64], in_=self.ident_f[:, 64:128]), reads=[self.b_const], writes=[b])
            Tk.op("dve", lambda e: e.tensor_copy(out=shift[:, 64:128], in_=self.ident_f[:, 0:64]), reads=[self.b_const], writes=[b])
            Tk.op("dve", lambda e: e.tensor_scalar(out=t1[:], in0=CS[:, :, 2 * Lc - 1], scalar1=sgn[:, 0:1], scalar2=None, op0=ALU.mult), reads=[b, bP], writes=[b])
            for dg in range(32):
                Tk.op("dve", lambda e: e.tensor_scalar(out=R[:, dg, :], in0=self.ident_f[:], scalar1=CS[:, dg, Lc - 1:Lc], scalar2=None, op0=ALU.mult), reads=[b, bP, self.b_const], writes=[bP])
                Tk.op("dve", lambda e: e.scalar_tensor_tensor(out=R[:, dg, :], in0=shift[:], scalar=t1[:, dg:dg + 1], in1=R[:, dg, :], op0=ALU.mult, op1=ALU.add), reads=[b, bP], writes=[bP])
            Tk.dma("sync", dcol[:], din["ssm_d"][l].rearrange("(ct p) -> p ct", p=128), writes=[b], allow_slow_non_contiguous=True)
            for ct in range(2):
                Tk.op("dve", lambda e: e.tensor_scalar(out=diagD[:, ct, :], in0=self.ident_f[:], scalar1=dcol[:, ct:ct + 1], scalar2=None, op0=ALU.mult), reads=[b, self.b_const], writes=[bP])
            self.dump("ssm_G1", G1[:], [128, 32, 128], BF16, [bP])
            self.dump("ssm_C1", C1[:], [128, 32, 128], BF16, [bP])
            self.dump("ssm_CS", CS[:], [128, 32, 2 * Lc], F32, [bP])
            self.dump("ssm_mag", mag[:], [128, 32], F32, [bP])
            Tk.barrier()
        Y = self.sb(es, "Y", [128, 2, T], F32)
        with ExitStack() as tes:
            NB = 4
            zz = [self.sb(tes, "zz", [128, 2 * Lc], F32) for _ in range(NB)]
            z = [self.sb(tes, "z", [128, Lc], F32) for _ in range(NB)]
            w = [self.sb(tes, "w", [128, Lc], F32) for _ in range(NB)]
            pp = [self.sb(tes, "pp", [128, 2, Lc], BF16) for _ in range(NB)]
            pGb = [self.ps(tes, "pG", [128, 512], F32) for _ in range(NB)]
            pC = self.ps(tes, "pC", [128, 512], F32)
            pY = [self.ps(tes, "pY", [128, 512], F32) for _ in range(2)]
            bzz, bz, bpp = [[Buf() for _ in range(NB)] for _ in range(3)]
            bw = [Buf() for _ in range(NB)]
            bpG = [Buf() for _ in range(NB)]
            bpC = [Buf()] * 32
            bcar = [Buf() for _ in range(32)]
            bpY = [Buf(), Buf()]
            bY = [[Buf() for _ in range(NC)] for _ in range(2)]
            its = []
            yi = 0
            for j in range(NC):
                for d in range(2):
                    for ct in range(2):
                        if d == 0:
                            blk = j
                            usl = U[:, ct, j * Lc:(j + 1) * Lc]
                        else:
                            blk = NC - 1 - j
                            usl = rev_ap(U[:, ct, blk * Lc:(blk + 1) * Lc])
                        for gg in range(8):
                            its.append(dict(j=j, d=d, ct=ct, gg=gg, dg=d * 16 + ct * 8 + gg, blk=blk, usl=usl, yk=yi % 2, n=len(its)))
                        yi += 1

            def stA(q):
                kb, dg, usl = q["n"] % NB, q["dg"], q["usl"]
                pG = pGb[kb][:, 0:2 * Lc]
                Tk.op("pe", lambda e: e.matmul(pG[:, 0:Lc], lhsT=G1[:, dg, :], rhs=usl, start=True, stop=True), reads=allU + [bP], writes=[bpG[kb]], inc=False)
                Tk.op("pe", lambda e: e.matmul(pG[:, Lc:2 * Lc], lhsT=G2[:, dg, :], rhs=usl, start=True, stop=True), reads=allU + [bP], writes=[bpG[kb]])

            def stB(qs):
                for q in qs:
                    kb, dg = q["n"] % NB, q["dg"]
                    pG = pGb[kb][:, 0:2 * Lc]
                    Tk.op("dve", lambda e: e.tensor_tensor(out=zz[kb][:], in0=pG, in1=CS[:, dg, :], op=ALU.mult), reads=[bpG[kb], bP], writes=[bzz[kb]])
                for q in qs:
                    kb = q["n"] % NB
                    Tk.op("dve", lambda e: e.tensor_tensor(out=z[kb][:], in0=zz[kb][:, 0:Lc], in1=zz[kb][:, Lc:2 * Lc], op=ALU.add), reads=[bzz[kb]], writes=[bz[kb]])
                for q in qs:
                    kb, dg, j = q["n"] % NB, q["dg"], q["j"]
                    init = 0.0 if j == 0 else carry[:, dg:dg + 1]
                    Tk.op("dve", lambda e: e.tensor_tensor_scan(out=w[kb][:], data0=mag[:, dg:dg + 1].to_broadcast([128, Lc]), data1=z[kb][:], initial=init, op0=ALU.mult, op1=ALU.add),
                          reads=[bz[kb], bP, bcar[dg]], writes=[bw[kb]])

            def stC(q):
                kb, dg, j = q["n"] % NB, q["dg"], q["j"]
                if j < NC - 1:
                    Tk.op("pe", lambda e: e.matmul(pC[:, dg:dg + 1], lhsT=R[:, dg, :], rhs=w[kb][:, Lc - 1:Lc], start=True, stop=True), reads=[bw[kb], bP], writes=[bpC[dg]])
                    Tk.op("act", lambda e: e.copy(out=carry[:, dg:dg + 1], in_=pC[:, dg:dg + 1]), reads=[bpC[dg]], writes=[bcar[dg]])
                Tk.op("pool", lambda e: e.tensor_tensor(out=pp[kb][:], in0=CS[:, dg, :].rearrange("p (a q) -> p a q", a=2), in1=w[kb][:].unsqueeze(1).to_broadcast([128, 2, Lc]), op=ALU.mult),
                      reads=[bw[kb], bP], writes=[bpp[kb]])

            def stE(q):
                kb, dg, j, d, ct, gg, blk, usl = q["n"] % NB, q["dg"], q["j"], q["d"], q["ct"], q["gg"], q["blk"], q["usl"]
                py, bpy = pY[q["yk"]], bpY[q["yk"]]
                Tk.op("pe", lambda e: e.matmul(py[:, 0:Lc], lhsT=C1[:, dg, :], rhs=pp[kb][:, 0, :], start=(gg == 0), stop=False), reads=[bpp[kb], bP], writes=[bpy], inc=False)
                first = (j < NC / 2)
                last = (gg == 7 and d == 1 and first)
                Tk.op("pe", lambda e: e.matmul(py[:, 0:Lc], lhsT=C2[:, dg, :], rhs=pp[kb][:, 1, :], start=False, stop=last), reads=[bpp[kb], bP], writes=[bpy])
                if gg != 7:
                    return
                ysl = Y[:, ct, blk * Lc:(blk + 1) * Lc]
                osl = ysl if d == 0 else rev_ap(ysl)
                if d == 0:
                    Tk.op("pe", lambda e: e.matmul(py[:, 0:Lc], lhsT=diagD[:, ct, :], rhs=usl, start=False, stop=first), reads=allU + [bP], writes=[bpy])
                if not first:
                    Tk.op("pe", lambda e: e.matmul(py[:, 0:Lc], lhsT=self.ident_f[:], rhs=osl, start=False, stop=True), reads=[bY[ct][blk], self.b_const], writes=[bpy])
                Tk.op("act", lambda e: e.copy(out=osl, in_=py[:, 0:Lc]), reads=[bpy], writes=[bY[ct][blk]])

            NI = len(its)
            NP = NI // 2
            prs = [its[2 * m:2 * m + 2] for m in range(NP)]
            for sidx in range(NP + 2):
                if sidx < NP:
                    for q in prs[sidx]:
                        stA(q)
                if 1 <= sidx <= NP:
                    stB(prs[sidx - 1])
                    for q in prs[sidx - 1]:
                        stC(q)
                if sidx >= 2:
                    for q in prs[sidx - 2]:
                        stE(q)
            self.dump("ssm_Y", Y[:], [128, 2, T], F32, [bY[ct][k] for ct in range(2) for k in range(NC)])
            Tk.barrier()
        with ExitStack() as tes:
            wglu = self.sb(tes, "wglu", [128, 2, 512], BF16)
            bglu = self.sb(tes, "bglu", [128, 4], F32)
            gss = self.sb(tes, "gss", [128, 2], F32)
            gY = [self.sb(tes, "gY", [128, 2, 512], BF16) for _ in range(2)]
            sg = self.sb(tes, "sg", [128, 2, 512], F32)
            og = self.sb(tes, "og", [128, 2, 512], F32)
            sq = self.sb(tes, "sq3", [128, 2, 512], BF16)
            rs = self.sb(tes, "rs3", [128, 512], F32)
            ho = [self.sb(tes, "ho", [128, 2, 512], BF16) for _ in range(2)]
            pz = [self.ps(tes, "pz", [128, 512], F32) for _ in range(4)]
            pss = self.ps(tes, "pss3", [128, 512], F32)
            bw_ = Buf()
            Tk.dma("pool", wglu[:], din["ssm_w_glu"][l].rearrange("(ct p) n -> p ct n", p=128), writes=[bw_])
            Tk.dma("sync", bglu[:], din["ssm_b_glu"][l].rearrange("(j p) -> p j", p=128), writes=[bw_], allow_slow_non_contiguous=True)
            Tk.dma("sync", gss[:], din["out_g_ssm"][l].rearrange("(j p) -> p j", p=128), writes=[bw_], allow_slow_non_contiguous=True)
            bgY, bho = [Buf(), Buf()], [Buf(), Buf()]
            bpz = [Buf() for _ in range(4)]
            bsg, bog, bsq, bpss, brs = [Buf() for _ in range(5)]
            allY = [bY[ct][k] for ct in range(2) for k in range(NC)]
            for c in range(self.NCH):
                k = c % 2
                cols = slice(c * 512, (c + 1) * 512)
                Tk.op("act", lambda e: e.activation(out=gY[k][:], in_=Y[:, :, cols], func=AF.Gelu_apprx_tanh), reads=allY, writes=[bgY[k]])
                for jt in range(4):
                    for ct in range(2):
                        Tk.op("pe", lambda e: e.matmul(pz[jt][:], lhsT=wglu[:, ct, jt * 128:(jt + 1) * 128], rhs=gY[k][:, ct, :], start=(ct == 0), stop=(ct == 1)), reads=[bgY[k], bw_], writes=[bpz[jt]], inc=(ct == 1))
                for a in range(2):
                    Tk.op("act", lambda e: e.activation(out=sg[:, a, :], in_=pz[2 + a][:], func=AF.Sigmoid, bias=bglu[:, 2 + a:3 + a]), reads=[bpz[2 + a], bw_], writes=[bsg])
                    Tk.op("dve", lambda e: e.scalar_tensor_tensor(out=og[:, a, :], in0=pz[a][:], scalar=bglu[:, a:a + 1], in1=sg[:, a, :], op0=ALU.add, op1=ALU.mult), reads=[bpz[a], bsg, bw_], writes=[bog])
                Tk.op("act", lambda e: e.activation(out=sq[:], in_=og[:], func=AF.Square), reads=[bog], writes=[bsq])
                for a in range(2):
                    Tk.op("pe", lambda e: e.matmul(pss[:], lhsT=self.ones_bf[:], rhs=sq[:, a, :], start=(a == 0), stop=(a == 1)), reads=[bsq, self.b_const], writes=[bpss], inc=(a == 1))
                Tk.op("dve", lambda e: e.tensor_scalar(out=rs[:], in0=pss[:], scalar1=1.0 / 256, scalar2=EPS, op0=ALU.mult, op1=ALU.add), reads=[bpss], writes=[brs])
                Tk.op("act", lambda e: e.activation(out=rs[:], in_=rs[:], func=AF.Sqrt), reads=[brs], writes=[brs])
                Tk.op("dve", lambda e: e.reciprocal(out=rs[:], in_=rs[:]), reads=[brs], writes=[brs])
                for a in range(2):
                    Tk.op("dve", lambda e: e.scalar_tensor_tensor(out=ho[k][:, a, :], in0=og[:, a, :], scalar=gss[:, a:a + 1], in1=rs[:], op0=ALU.mult, op1=ALU.mult), reads=[bog, brs, bw_], writes=[bho[k]])
                Tk.dma("sync", self.HT[384:640, cols].rearrange("(a p) q -> p a q", p=128), ho[k][:], reads=[bho[k]], writes=[Tk.b("HT", l, "ssm", c)])
            Tk.barrier()


MK.p3 = _p3


def _p4(self, l):
    nc, Tk, T, NT, NCH = self.nc, self.trk, self.T, self.NT, self.NCH
    m = self.mix
    CQ, CKV, KR = m["CQ"], m["CKV"], m["KR"]
    din = self.din
    SCALE = 96.0 ** -0.5
    allin = [Tk.b(n, l, c) for n in ("CQ", "CKV", "KR") for c in range(NCH)]
    with ExitStack() as es:
        wuq = self.sb(es, "wuq", [128, 2, 576], BF16)
        wsw = self.sb(es, "wsw", [128, 2, 6, 96], BF16)
        wk = self.sb(es, "wk", [128, 6, 64], BF16)
        wv = self.sb(es, "wv", [128, 6, 64], BF16)
        gml = self.sb(es, "gml", [64, 6], F32)
        ones_f = self.sb(es, "ones_f4", [128, 64], F32)
        K = self.sb(es, "K", [128, 6, T], BF16)
        V = self.sb(es, "V", [128, NT, 6, 65], BF16)
        bW = Buf()
        bK = [Buf() for _ in range(NCH)]
        bKr = Buf()
        bV = [Buf() for _ in range(NT)]
        with ExitStack() as tes:
            sq_ = self.sb(tes, "squ", [128, 2, 576], F32)
            skv = self.sb(tes, "skv", [128, 768], F32)
            gq = self.sb(tes, "gq", [128, 2], F32)
            gkv = self.sb(tes, "gkv", [128, 1], F32)
            wukv = self.sb(tes, "wukv", [128, 6, 128], BF16)
            b = Buf()
            Tk.dma("sync", sq_[:, 0, :], din["mla_w_uq"][l, 0:128, :], writes=[b])
            Tk.dma("sync", sq_[0:64, 1, :], din["mla_w_uq"][l, 128:192, :], writes=[b])
            Tk.dma("sync", skv[:], din["mla_w_ukv"][l], writes=[b])
            gqs = din["mla_q_norm_g"][l]
            Tk.dma("sync", gq[:, 0:1], gqs[0:128].rearrange("(p o) -> p o", o=1), writes=[b])
            Tk.dma("sync", gq[0:64, 1:2], gqs[128:192].rearrange("(p o) -> p o", o=1), writes=[b])
            Tk.dma("sync", gkv[:], din["mla_kv_norm_g"][l].rearrange("(p o) -> p o", o=1), writes=[b])
            Tk.dma("sync", gml[:], din["out_g_mla"][l].rearrange("(h d) -> d h", d=64), writes=[bW], allow_slow_non_contiguous=True)
            Tk.op("dve", lambda e: e.tensor_scalar(out=wuq[:, 0, :], in0=sq_[:, 0, :], scalar1=gq[:, 0:1], scalar2=None, op0=ALU.mult), reads=[b], writes=[bW])
            Tk.op("dve", lambda e: e.tensor_scalar(out=wuq[0:64, 1, :], in0=sq_[0:64, 1, :], scalar1=gq[0:64, 1:2], scalar2=None, op0=ALU.mult), reads=[b], writes=[bW])
            Tk.op("dve", lambda e: e.tensor_scalar(out=wukv[:].rearrange("p h f -> p (h f)"), in0=skv[:], scalar1=gkv[:, 0:1], scalar2=None, op0=ALU.mult), reads=[b], writes=[b])
            Tk.op("pool", lambda e: e.memset(wsw[:], 0.0), writes=[bW])
            for kc, pr in ((0, 128), (1, 64)):
                src = wuq[0:pr, kc, :].rearrange("p (h f) -> p h f", h=6)
                Tk.op("act", lambda e: e.mul(out=wsw[0:pr, kc, :, 64:80], in_=src[:, :, 80:96], mul=-1.0), reads=[bW], writes=[bW])
                Tk.op("act", lambda e: e.copy(out=wsw[0:pr, kc, :, 80:96], in_=src[:, :, 64:80]), reads=[bW], writes=[bW])
            Tk.op("dve", lambda e: e.tensor_copy(out=wk[:], in_=wukv[:, :, 0:64]), reads=[b], writes=[bW])
            Tk.op("dve", lambda e: e.tensor_copy(out=wv[:], in_=wukv[:, :, 64:128]), reads=[b], writes=[bW])
            Tk.op("pool", lambda e: e.memset(ones_f[:], 1.0), writes=[bW])
            Tk.op("pool", lambda e: e.memset(V[:, :, :, 64:65], 1.0), writes=[bW])
            sqk = [self.sb(tes, "sqk", [128, 512], BF16) for _ in range(2)]
            rkv = [self.sb(tes, "rkv", [128, 512], F32) for _ in range(2)]
            rcol = [self.sb(tes, "rcol", [128, 1], F32) for _ in range(2)]
            pss = self.ps(tes, "pss4", [128, 512], F32)
            pk = [self.ps(tes, "pk", [128, 512], F32) for _ in range(2)]
            pv = [self.ps(tes, "pv", [128, 512], F32) for _ in range(2)]
            pc = self.ps(tes, "pc", [128, 512], F32)
            bsqk, brkv, brcol = [Buf(), Buf()], [Buf(), Buf()], [Buf(), Buf()]
            bpss, bpc = Buf(), Buf()
            bpk, bpv = [Buf(), Buf()], [Buf(), Buf()]
            n = 0
            for h in range(6):
                Tk.op(("act", "pool")[h % 2], lambda e: (e.copy(out=K[64:96, h, :], in_=KR[64:96, :]) if h % 2 == 0 else e.tensor_copy(out=K[64:96, h, :], in_=KR[64:96, :])), reads=allin, writes=[bKr])
            for c in range(NCH):
                k = c % 2
                cols = slice(c * 512, (c + 1) * 512)
                Tk.op("act", lambda e: e.activation(out=sqk[k][:], in_=CKV[:, cols], func=AF.Square), reads=allin, writes=[bsqk[k]])
                Tk.op("pe", lambda e: e.matmul(pss[:], lhsT=self.ones_bf[:], rhs=sqk[k][:], start=True, stop=True), reads=[bsqk[k], self.b_const], writes=[bpss])
                Tk.op("dve", lambda e: e.tensor_scalar(out=rkv[k][:], in0=pss[:], scalar1=1.0 / 128, scalar2=EPS, op0=ALU.mult, op1=ALU.add), reads=[bpss], writes=[brkv[k]])
                Tk.op("act", lambda e: e.activation(out=rkv[k][:], in_=rkv[k][:], func=AF.Sqrt), reads=[brkv[k]], writes=[brkv[k]])
                Tk.op("dve", lambda e: e.reciprocal(out=rkv[k][:], in_=rkv[k][:]), reads=[brkv[k]], writes=[brkv[k]])
                for h in range(6):
                    kk = n % 2
                    n += 1
                    Tk.op("pe", lambda e: e.matmul(pk[kk][0:64, :], lhsT=wk[:, h, :], rhs=CKV[:, cols], start=True, stop=True), reads=allin + [bW], writes=[bpk[kk]])
                    Tk.op("dve", lambda e: e.tensor_tensor(out=K[0:64, h, cols], in0=pk[kk][0:64, :], in1=rkv[k][0:64, :], op=ALU.mult), reads=[bpk[kk], brkv[k]], writes=[bK[c]])
                for tt in range(4):
                    i = 4 * c + tt
                    kk = i % 2
                    ts_ = slice(i * 128, (i + 1) * 128)
                    Tk.op("pe", lambda e: e.matmul(pc[:, i % 512:i % 512 + 1], lhsT=sqk[k][:, tt * 128:(tt + 1) * 128], rhs=self.ones_bf[:, 0:1], start=True, stop=True), reads=[bsqk[k], self.b_const], writes=[bpc])
                    Tk.op("dve", lambda e: e.tensor_scalar(out=rcol[kk][:], in0=pc[:, i % 512:i % 512 + 1], scalar1=1.0 / 128, scalar2=EPS, op0=ALU.mult, op1=ALU.add), reads=[bpc], writes=[brcol[kk]])
                    Tk.op("act", lambda e: e.activation(out=rcol[kk][:], in_=rcol[kk][:], func=AF.Sqrt), reads=[brcol[kk]], writes=[brcol[kk]])
                    Tk.op("dve", lambda e: e.reciprocal(out=rcol[kk][:], in_=rcol[kk][:]), reads=[brcol[kk]], writes=[brcol[kk]])
                    Tk.op("pe", lambda e: e.matmul(pv[kk][:, 0:384], lhsT=CKV[:, ts_], rhs=wv[:].rearrange("p h f -> p (h f)"), start=True, stop=True), reads=allin + [bW], writes=[bpv[kk]])
                    Tk.op("dve", lambda e: e.tensor_scalar(out=V[:, i, :, 0:64], in0=pv[kk][:, 0:384].rearrange("p (h f) -> p h f", h=6), scalar1=rcol[kk][:, 0:1], scalar2=None, op0=ALU.mult), reads=[bpv[kk], brcol[kk]], writes=[bV[i]])
            self.dump("mla_K", K[:], [128, 6, T], BF16, bK + [bKr])
            self.dump("mla_V", V[:], [128, NT, 6, 65], BF16, bV + [bW])
            Tk.barrier()
        with ExitStack() as tes:
            sqq = self.sb(tes, "sqq", [128, 2, 512], BF16)
            rq = self.sb(tes, "rq", [128, 512], F32)
            CR = self.sb(tes, "CR", [128, 512], F32)
            SR = self.sb(tes, "SR", [128, 512], F32)
            qt1 = self.sb(tes, "qt1", [128, 512], F32)
            qt2 = self.sb(tes, "qt2", [128, 512], F32)
            Q = [self.sb(tes, "Q", [128, 6, 512], BF16) for _ in range(2)]
            rp = self.sb(tes, "rp4", [128, 2, 512], F32)
            brp = Buf()
            PT = [self.sb(tes, "PT4", [128, 512], BF16) for _ in range(3)]
            Osb = self.sb(tes, "Osb4", [65, 512], F32)
            rd = self.sb(tes, "rd4", [65, 512], F32)
            oall = self.sb(tes, "oall", [64, 6, 512], F32)
            sqo = self.sb(tes, "sqo", [64, 6, 512], BF16)
            rs = self.sb(tes, "rs4", [64, 512], F32)
            hT = self.sb(tes, "hT4", [64, 6, 512], BF16)
            pS = [self.ps(tes, "pS4", [128, 512], F32) for _ in range(4)]
            pO = [self.ps(tes, "pO4", [128, 512], F32) for _ in range(2)]
            pM = [self.ps(tes, "pM4", [128, 512], F32) for _ in range(2)]
            bsqq, brq, bCR, bqt = Buf(), Buf(), Buf(), Buf()
            bQ, bhT = [Buf(), Buf()], Buf()
            bPT, bpS = [Buf() for _ in range(3)], [Buf() for _ in range(4)]
            bpO, bpM = [Buf(), Buf()], [Buf(), Buf()]
            bOsb, brd, boall, bsqo, brs = [Buf() for _ in range(5)]
            allK = bK + [bKr]
            allV = bV + [bW]
            r2 = slice(64, 96)

            def prepA1(c):
                cols = slice(c * 512, (c + 1) * 512)
                Tk.op("act", lambda e: e.activation(out=sqq[:, 0, :], in_=CQ[:, 0, cols], func=AF.Square), reads=allin, writes=[bsqq])
                Tk.op("act", lambda e: e.activation(out=sqq[0:64, 1, :], in_=CQ[0:64, 1, cols], func=AF.Square), reads=allin, writes=[bsqq])
                for v in range(2):
                    Tk.dma("sync", rp[64:96, v, :], self.ROPE[v, :, cols], reads=[self.b_rope], writes=[brp])

            def prepA2(c):
                Tk.op("pe", lambda e: e.matmul(pM[0][:], lhsT=self.ones_bf[:], rhs=sqq[:, 0, :], start=True, stop=False), reads=[bsqq, self.b_const], writes=[bpM[0]], inc=False)
                Tk.op("pe", lambda e: e.matmul(pM[0][:], lhsT=self.ones_bf[0:64, :], rhs=sqq[0:64, 1, :], start=False, stop=True), reads=[bsqq, self.b_const], writes=[bpM[0]])
                Tk.op("dve", lambda e: e.tensor_scalar(out=rq[:], in0=pM[0][:], scalar1=1.0 / 192, scalar2=EPS, op0=ALU.mult, op1=ALU.add), reads=[bpM[0]], writes=[brq])
                Tk.op("act", lambda e: e.activation(out=rq[:], in_=rq[:], func=AF.Sqrt), reads=[brq], writes=[brq])
                Tk.op("dve", lambda e: e.reciprocal(out=rq[:], in_=rq[:]), reads=[brq], writes=[brq])
                Tk.op("dve", lambda e: e.tensor_tensor(out=CR[r2, :], in0=rp[r2, 0, :], in1=rq[r2, :], op=ALU.mult), reads=[brq, brp], writes=[bCR])
                Tk.op("dve", lambda e: e.tensor_tensor(out=SR[r2, :], in0=rp[r2, 1, :], in1=rq[r2, :], op=ALU.mult), reads=[brq, brp], writes=[bCR])

            def prepQ(c, h):
                cols = slice(c * 512, (c + 1) * 512)
                qb = c % 2
                for v in range(2):
                    for kc, pr in ((0, 128), (1, 64)):
                        lhs = wuq[0:pr, kc, h * 96:(h + 1) * 96] if v == 0 else wsw[0:pr, kc, h, :]
                        Tk.op("pe", lambda e: e.matmul(pM[v][0:96, :], lhsT=lhs, rhs=CQ[0:pr, kc, cols], start=(kc == 0), stop=(kc == 1)), reads=allin + [bW], writes=[bpM[v]], inc=(kc == 1))
                Tk.op("dve", lambda e: e.tensor_tensor(out=Q[qb][0:64, h, :], in0=pM[0][0:64, :], in1=rq[0:64, :], op=ALU.mult), reads=[bpM[0], brq], writes=[bQ[qb]])
                Tk.op("dve", lambda e: e.tensor_tensor(out=qt1[r2, :], in0=pM[0][r2, :], in1=CR[r2, :], op=ALU.mult), reads=[bpM[0], bCR], writes=[bqt])
                Tk.op("dve", lambda e: e.tensor_tensor(out=qt2[r2, :], in0=pM[1][r2, :], in1=SR[r2, :], op=ALU.mult), reads=[bpM[1], bCR], writes=[bqt])
                Tk.op("dve", lambda e: e.tensor_tensor(out=Q[qb][r2, h, :], in0=qt1[r2, :], in1=qt2[r2, :], op=ALU.add), reads=[bqt], writes=[bQ[qb]])

            def epi(c):
                cols = slice(c * 512, (c + 1) * 512)
                Tk.op("act", lambda e: e.activation(out=sqo[:], in_=oall[:], func=AF.Square), reads=[boall], writes=[bsqo])
                for h in range(6):
                    Tk.op("pe", lambda e: e.matmul(pM[1][0:64, :], lhsT=self.ones_bf[0:64, 0:64], rhs=sqo[:, h, :], start=(h == 0), stop=(h == 5)), reads=[bsqo, self.b_const], writes=[bpM[1]], inc=(h == 5))
                Tk.op("dve", lambda e: e.tensor_scalar(out=rs[:], in0=pM[1][0:64, :], scalar1=1.0 / 384, scalar2=EPS, op0=ALU.mult, op1=ALU.add), reads=[bpM[1]], writes=[brs])
                Tk.op("act", lambda e: e.activation(out=rs[:], in_=rs[:], func=AF.Sqrt), reads=[brs], writes=[brs])
                Tk.op("dve", lambda e: e.reciprocal(out=rs[:], in_=rs[:]), reads=[brs], writes=[brs])
                Tk.op("dve", lambda e: e.tensor_tensor(out=oall[:], in0=oall[:], in1=rs[:].unsqueeze(1).to_broadcast([64, 6, 512]), op=ALU.mult), reads=[boall, brs], writes=[boall])
                Tk.op("dve", lambda e: e.tensor_tensor(out=hT[:], in0=oall[:], in1=gml[:].unsqueeze(2).to_broadcast([64, 6, 512]), op=ALU.mult), reads=[boall, bW], writes=[bhT])
                Tk.dma("sync", self.HT[640:1024, cols].rearrange("(h d) q -> d h q", d=64), hT[:], reads=[bhT], writes=[Tk.b("HT", l, "mla", c)])

            steps = [(c, h, kt) for c in range(NCH) for h in range(6) for kt in range(NT)]
            SPC = 6 * NT
            NS_ = len(steps)

            def stS(n):
                c, h, kt = steps[n]
                k4 = n % 4
                Tk.op("pe", lambda e: e.matmul(pS[k4][:], lhsT=K[0:96, h, kt * 128:(kt + 1) * 128], rhs=Q[c % 2][0:96, h, :], start=True, stop=True), reads=allK + [bQ[c % 2]], writes=[bpS[k4]])

            def stPV(n):
                c, h, kt = steps[n]
                k3, k4 = n % 3, n % 4
                po, bpo = pO[h % 2], bpO[h % 2]
                Tk.op("act", lambda e: e.activation(out=PT[k3][:], in_=pS[k4][:], func=AF.Exp, scale=SCALE), reads=[bpS[k4]], writes=[bPT[k3]])
                Tk.op("pe", lambda e: e.matmul(po[0:65, :], lhsT=V[:, kt, h, :], rhs=PT[k3][:], start=(kt == 0), stop=(kt == NT - 1)), reads=allV + [bPT[k3]], writes=[bpo], inc=(kt == NT - 1))
                if kt == NT - 1:
                    Tk.op("act", lambda e: e.copy(out=Osb[:], in_=po[0:65, :]), reads=[bpo], writes=[bOsb])
                    Tk.op("dve", lambda e: e.reciprocal(out=rd[64:65, :], in_=Osb[64:65, :]), reads=[bOsb], writes=[brd])

            def stEp(h):
                Tk.op("pe", lambda e: e.matmul(pM[0][0:64, :], lhsT=ones_f[64:65, 0:64], rhs=rd[64:65, :], start=True, stop=True), reads=[brd, bW], writes=[bpM[0]])
                Tk.op("dve", lambda e: e.tensor_tensor(out=oall[:, h, :], in0=Osb[0:64, :], in1=pM[0][0:64, :], op=ALU.mult), reads=[bOsb, bpM[0]], writes=[boall])

            hooks = {}

            def hook(n, f):
                hooks.setdefault(min(n, NS_), []).append(f)

            dly = min(4, NT - 2)
            for c in range(NCH):
                base = c * SPC
                if c + 1 < NCH:
                    hook(base + SPC // 8, lambda c=c: prepA1(c + 1))
                    hook(base + SPC // 8 + 3, lambda c=c: prepA2(c + 1))
                    for h in range(6):
                        hook(base + SPC // 4 + (h * SPC) // 10, lambda c=c, h=h: prepQ(c + 1, h))
                for h in range(6):
                    hook(base + (h + 1) * NT + dly, lambda h=h: stEp(h))
                hook(base + SPC + dly + 4, lambda c=c: epi(c))
            prepA1(0)
            prepA2(0)
            for h in range(6):
                prepQ(0, h)
            if True:
                self.dump("mla_Q", Q[0][:], [128, 6, 512], BF16, [bQ[0]])
            for n in range(NS_ + 2):
                if n < NS_:
                    stS(n)
                if n >= 2:
                    stPV(n - 2)
                for f in hooks.get(n - 1, ()):
                    f()
            Tk.barrier()


MK.p4 = _p4


def _p5(self, l, xsrc, xkey):
    nc, Tk, T, NT, NCH = self.nc, self.trk, self.T, self.NT, self.NCH
    din = self.din
    GATE = self.GATE
    with ExitStack() as es:
        wout = self.sb(es, "wout", [128, 8, D], BF16)
        wr = self.sb(es, "wr", [128, 8, 36], F32)
        brbc = self.sb(es, "brbc", [128, 36], F32)
        g2bc = self.sb(es, "g2bc", [128, D], F32)
        hTs = [self.sb(es, "hTs", [128, 8, 512], BF16) for _ in range(2)]
        xt = [self.sb(es, "xt5", [128, D], F32) for _ in range(2)]
        x1 = [self.sb(es, "x1", [128, D], F32) for _ in range(2)]
        junk = self.sb(es, "junk5", [128, D], BF16)
        xn2 = [self.sb(es, "xn2", [128, D], F32) for _ in range(2)]
        xTf = [self.sb(es, "xTf", [128, 8, 128], F32) for _ in range(2)]
        xTb = [self.sb(es, "xTb", [128, 8, 512], BF16) for _ in range(2)]
        ss = [self.sb(es, "ss5", [128, 1], F32) for _ in range(2)]
        rstd = [self.sb(es, "rstd5", [128, 1], F32) for _ in range(2)]
        sm = [self.sb(es, "sm", [128, 128], F32) for _ in range(2)]
        px = [self.ps(es, "px", [128, 512], F32) for _ in range(2)]
        pT = [self.ps(es, "pT5", [128, 1024], F32)]
        pr = self.ps(es, "pr", [128, 512], F32)
        bw = Buf()
        Tk.dma("pool", wout[:], din["w_out"][l].rearrange("(kc p) n -> p kc n", p=128), writes=[bw])
        Tk.dma("sync", wr[:, :, 0:4], din["w_router_group"][l].rearrange("(kc p) n -> p kc n", p=128), writes=[bw])
        Tk.dma("sync", wr[:, :, 4:36], din["w_router_expert"][l].rearrange("(kc p) n -> p kc n", p=128), writes=[bw])
        Tk.dma("sync", brbc[:, 0:4], din["b_router_group"][l:l + 1, :].partition_broadcast(128), writes=[bw])
        Tk.dma("sync", brbc[:, 4:36], din["b_router_expert"][l:l + 1, :].partition_broadcast(128), writes=[bw])
        Tk.dma("sync", g2bc[:], din["ln2_g"][l:l + 1, :].partition_broadcast(128), writes=[bw])
        bhTs, bxt, bx1, bxn2, bxTf, bxTb, bss, bsm = [[Buf(), Buf()] for _ in range(8)]
        bpx = [Buf(), Buf()]
        bpT = Buf()
        bpr, bjunk = Buf(), Buf()
        pT1 = pT[0]

        def s0(i):
            c, tt = divmod(i, 4)
            cb, p = c % 2, i % 2
            cols = slice(c * 512, (c + 1) * 512)
            if tt == 0:
                Tk.dma("sync", hTs[cb][:], self.HT[:, cols].rearrange("(kc p) q -> p kc q", p=128),
                       reads=[Tk.b("HT", l, "attn", j) for j in range(4 * c, 4 * c + 4)] + [Tk.b("HT", l, "ssm", c), Tk.b("HT", l, "mla", c)], writes=[bhTs[cb]])
            Tk.dma("sync", xt[p][:], xsrc[i * 128:(i + 1) * 128, :], reads=[Tk.b(xkey, i)], writes=[bxt[p]])
            for half in range(2):
                for kc in range(8):
                    Tk.op("pe", lambda e: e.matmul(px[half][:], lhsT=hTs[cb][:, kc, tt * 128:(tt + 1) * 128], rhs=wout[:, kc, half * 512:(half + 1) * 512], start=(kc == 0), stop=(kc == 7)),
                          reads=[bhTs[cb], bw], writes=[bpx[half]], inc=(kc == 7))

        def s1(i):
            p = i % 2
            rows = slice(i * 128, (i + 1) * 128)
            for half in range(2):
                Tk.op("dve", lambda e: e.tensor_tensor(out=x1[p][:, half * 512:(half + 1) * 512], in0=xt[p][:, half * 512:(half + 1) * 512], in1=px[half][:], op=ALU.add), reads=[bxt[p], bpx[half]], writes=[bx1[p]])
            Tk.dma("sync", self.XR[rows, :], x1[p][:], reads=[bx1[p], Tk.b(xkey, i)], writes=[Tk.b("XR", i)])
            Tk.op("dve", lambda e: e.memset(ss[p][:], 0.0), writes=[bss[p]])
            Tk.op("act", lambda e: e.activation(out=junk[:], in_=x1[p][:], func=AF.Square, accum_out=ss[p][:]), reads=[bx1[p], bss[p]], writes=[bjunk, bss[p]])
            Tk.op("dve", lambda e: e.tensor_scalar(out=ss[p][:], in0=ss[p][:], scalar1=1.0 / D, scalar2=EPS, op0=ALU.mult, op1=ALU.add), reads=[bss[p]], writes=[bss[p]])
            Tk.op("act", lambda e: e.activation(out=ss[p][:], in_=ss[p][:], func=AF.Ln), reads=[bss[p]], writes=[bss[p]])
            Tk.op("act", lambda e: e.activation(out=rstd[p][:], in_=ss[p][:], func=AF.Exp, scale=-0.5), reads=[bss[p]], writes=[bss[p]])
            Tk.op("dve", lambda e: e.scalar_tensor_tensor(out=xn2[p][:], in0=x1[p][:], scalar=rstd[p][:, 0:1], in1=g2bc[:], op0=ALU.mult, op1=ALU.mult), reads=[bx1[p], bss[p], bw], writes=[bxn2[p]])

        def s2(i):
            c, tt = divmod(i, 4)
            cb, p = c % 2, i % 2
            cols = slice(c * 512, (c + 1) * 512)
            for kc in range(8):
                Tk.op("pe", lambda e: e.transpose(pT1[:, kc * 128:(kc + 1) * 128], xn2[p][:, kc * 128:(kc + 1) * 128], self.ident_f[:]), reads=[bxn2[p], self.b_const], writes=[bpT], inc=(kc == 7))
            for hb_ in range(2):
                pT3 = pT1[:, hb_ * 512:(hb_ + 1) * 512].rearrange("p (k t) -> p k t", k=4)
                Tk.op("act", lambda e: e.copy(out=xTf[p][:, hb_ * 4:(hb_ + 1) * 4, :], in_=pT3), reads=[bpT], writes=[bxTf[p]])
                Tk.op("dve", lambda e: e.tensor_copy(out=xTb[cb][:, hb_ * 4:(hb_ + 1) * 4, tt * 128:(tt + 1) * 128], in_=xTf[p][:, hb_ * 4:(hb_ + 1) * 4, :]), reads=[bxTf[p]], writes=[bxTb[cb]])
            if tt == 3:
                Tk.dma("sync", self.XN2T[:, cols].rearrange("(kc p) q -> p kc q", p=128), xTb[cb][:], reads=[bxTb[cb]], writes=[Tk.b("XN2T", l, c)])

        def s3(i):
            p = i % 2
            for kc in range(8):
                Tk.op("pe", lambda e: e.matmul(pr[:, 0:36], lhsT=xTf[p][:, kc, :], rhs=wr[:, kc, :], start=(kc == 0), stop=(kc == 7)), reads=[bxTf[p], bw], writes=[bpr], inc=(kc == 7))

        def s4(i):
            p = i % 2
            s = sm[p]
            bs = bsm[p]
            lg = s[:, 0:36]
            gmax, ngmax, gsum, pg = s[:, 36:37], s[:, 37:38], s[:, 38:39], s[:, 39:40]
            ghot, m1, gex = s[:, 40:44], s[:, 44:48], s[:, 48:52]
            top8 = s[:, 52:60]
            d21, e21, w1, w2 = s[:, 60:61], s[:, 61:62], s[:, 62:63], s[:, 63:64]
            msk = s[:, 64:96]
            g1 = s[:, 96:128]
            msk3 = msk.rearrange("p (g e) -> p g e", g=4)

            def dv(fn, extra=()):
                Tk.op("dve", fn, reads=[bs] + list(extra), writes=[bs])

            dv(lambda e: e.tensor_tensor(out=lg, in0=pr[:, 0:36], in1=brbc[:], op=ALU.add), [bpr, bw])
            dv(lambda e: e.tensor_reduce(out=gmax, in_=lg[:, 0:4], axis=AX.X, op=ALU.max))
            dv(lambda e: e.tensor_scalar(out=ghot, in0=lg[:, 0:4], scalar1=gmax, scalar2=None, op0=ALU.is_ge))
            dv(lambda e: e.tensor_scalar(out=ngmax, in0=gmax, scalar1=-1.0, scalar2=None, op0=ALU.mult))
            dv(lambda e: e.memset(gsum, 0.0))
            Tk.op("act", lambda e: e.activation(out=gex, in_=lg[:, 0:4], func=AF.Exp, bias=ngmax, accum_out=gsum), reads=[bs], writes=[bs])
            dv(lambda e: e.reciprocal(out=pg, in_=gsum))
            dv(lambda e: e.tensor_scalar(out=m1, in0=ghot, scalar1=-1.0, scalar2=1.0e4, op0=ALU.add, op1=ALU.mult))
            dv(lambda e: e.tensor_tensor(out=msk3, in0=lg[:, 4:36].rearrange("p (g e) -> p g e", g=4), in1=ghot.unsqueeze(2).to_broadcast([128, 4, 8]), op=ALU.mult))
            dv(lambda e: e.tensor_tensor(out=msk3, in0=msk3, in1=m1.unsqueeze(2).to_broadcast([128, 4, 8]), op=ALU.add))
            dv(lambda e: e.max(out=top8, in_=msk))
            dv(lambda e: e.tensor_tensor(out=d21, in0=top8[:, 1:2], in1=top8[:, 0:1], op=ALU.subtract))
            Tk.op("act", lambda e: e.activation(out=e21, in_=d21, func=AF.Exp), reads=[bs], writes=[bs])
            dv(lambda e: e.tensor_scalar(out=w1, in0=e21, scalar1=1.0, scalar2=None, op0=ALU.add))
            dv(lambda e: e.reciprocal(out=w1, in_=w1))
            dv(lambda e: e.tensor_tensor(out=w1, in0=w1, in1=pg, op=ALU.mult))
            dv(lambda e: e.tensor_tensor(out=w2, in0=w1, in1=e21, op=ALU.mult))
            dv(lambda e: e.tensor_scalar(out=g1, in0=msk, scalar1=top8[:, 0:1], scalar2=w1, op0=ALU.is_equal, op1=ALU.mult))
            dv(lambda e: e.tensor_scalar(out=msk, in0=msk, scalar1=top8[:, 1:2], scalar2=w2, op0=ALU.is_equal, op1=ALU.mult))
            Tk.op("dve", lambda e: e.tensor_tensor(out=GATE[:, i, :], in0=g1, in1=msk, op=ALU.add), reads=[bs], writes=[Tk.b("GATE", l, i)])

        for k in range(NT + 4):
            if 0 <= k - 4 < NT:
                s4(k - 4)
            if 0 <= k - 1 < NT:
                s1(k - 1)
            if k < NT:
                s0(k)
            if 0 <= k - 2 < NT:
                s2(k - 2)
            if 0 <= k - 3 < NT:
                s3(k - 3)
        Tk.barrier()


def _p6(self, l, last):
    nc, Tk, T, NT = self.nc, self.trk, self.T, self.NT
    din = self.din
    GATE = self.GATE
    SC = min(getattr(self, "moe_sc", 2048), T)
    NS = T // SC
    NTS = SC // 128
    NC4 = SC // 512
    NW = 3
    with ExitStack() as es:
        XN = self.sb(es, "XN", [128, 8, SC], BF16)
        yacc = self.sb(es, "yacc", [128, NTS, D], F32)
        wg = [self.sb(es, "wg", [128, 8, 256], BF16) for _ in range(NW)]
        wu = [self.sb(es, "wu", [128, 8, 256], BF16) for _ in range(NW)]
        wd = [self.sb(es, "wd", [128, 2, D], BF16) for _ in range(NW)]
        sgl = [self.sb(es, "sgl", [128, 2, 512], F32) for _ in range(2)]
        hT = [self.sb(es, "hT6", [128, 2, 512], BF16) for _ in range(2)]
        xt = [self.sb(es, "xt6", [128, D], F32) for _ in range(4)]
        fgbc = self.sb(es, "fgbc", [128, D], F32)
        junk = self.sb(es, "junk6", [128, D], BF16)
        ss = [self.sb(es, "ss6", [128, 1], F32) for _ in range(2)]
        pgu = [self.ps(es, "pgu", [128, 512], F32) for _ in range(4)]
        py = [self.ps(es, "py", [128, 512], F32) for _ in range(4)]
        bfg = Buf()
        if last:
            Tk.dma("sync", fgbc[:], din["final_g"].rearrange("(o n) -> o n", o=1).partition_broadcast(128), writes=[bfg])
        bXN = [Buf() for _ in range(NC4)]
        bw = [Buf() for _ in range(NW)]
        bsgl, bhT = [[[Buf(), Buf()] for _ in range(2)] for _ in range(2)]
        bxt = [Buf() for _ in range(4)]
        bss = [Buf(), Buf()]
        bpgu, bpy = [Buf() for _ in range(4)], [Buf() for _ in range(4)]
        byacc = [Buf() for _ in range(NTS)]
        bjunk = Buf()
        allgate = [Tk.b("GATE", l, i) for i in range(NT)]
        allxn = [Tk.b("XN2T", l, c) for c in range(self.NCH)]
        cnt = {"py": 0}

        def load_xn(s, c4):
            t0 = s * SC + c4 * 512
            Tk.dma("sync", XN[:, :, c4 * 512:(c4 + 1) * 512], self.XN2T[:, t0:t0 + 512].rearrange("(kc p) q -> p kc q", p=128), reads=allxn, writes=[bXN[c4]])

        def load_w(gex):
            ex = gex % 32
            g, ee = divmod(ex, 8)
            wb = gex % NW
            Tk.dma("pool", wg[wb][:], din["w_gate"][l, g, ee].rearrange("(kc p) f -> p kc f", p=128), writes=[bw[wb]])
            Tk.dma("pool", wu[wb][:], din["w_up"][l, g, ee].rearrange("(kc p) f -> p kc f", p=128), writes=[bw[wb]])
            Tk.dma("pool", wd[wb][:], din["w_down"][l, g, ee].rearrange("(fc p) n -> p fc n", p=128), writes=[bw[wb]])

        blocks = [(s, ex, c4) for s in range(NS) for ex in range(32) for c4 in range(NC4)]
        NBk = len(blocks)

        def stA(n, ft):
            s, ex, c4 = blocks[n]
            wb = (s * 32 + ex) % NW
            hb = n % 2
            cols = slice(c4 * 512, (c4 + 1) * 512)
            for v, wt in ((0, wg[wb]), (1, wu[wb])):
                pp = pgu[v * 2 + ft]
                for kc in range(8):
                    Tk.op("pe", lambda e: e.matmul(pp[:], lhsT=wt[:, kc, ft * 128:(ft + 1) * 128], rhs=XN[:, kc, cols], start=(kc == 0), stop=(kc == 7)),
                          reads=[bw[wb], bXN[c4]], writes=[bpgu[v * 2 + ft]], inc=(kc == 7))
            Tk.op("act", lambda e: e.activation(out=sgl[hb][:, ft, :], in_=pgu[ft][:], func=AF.Silu), reads=[bpgu[ft]], writes=[bsgl[hb][ft]])
            Tk.op("dve", lambda e: e.tensor_tensor(out=hT[hb][:, ft, :], in0=sgl[hb][:, ft, :], in1=pgu[2 + ft][:], op=ALU.mult), reads=[bsgl[hb][ft], bpgu[2 + ft]], writes=[bhT[hb][ft]])
            if ft == 1 and ex == 31 and s + 1 < NS:
                load_xn(s + 1, c4)

        def stD(n):
            s, ex, c4 = blocks[n]
            wb = (s * 32 + ex) % NW
            hb = n % 2
            for tt in range(4):
                ti = c4 * 4 + tt
                gi = s * NTS + ti
                if ex == 0:
                    xb = gi % 4
                    Tk.dma("sync", xt[xb][:], self.XR[gi * 128:(gi + 1) * 128, :], reads=[Tk.b("XR", gi)], writes=[bxt[xb]])
                for half in range(2):
                    k4 = cnt["py"] % 4
                    cnt["py"] += 1
                    for ft in range(2):
                        Tk.op("pe", lambda e: e.matmul(py[k4][:], lhsT=hT[hb][:, ft, tt * 128:(tt + 1) * 128], rhs=wd[wb][:, ft, half * 512:(half + 1) * 512], start=(ft == 0), stop=(ft == 1)),
                              reads=[bhT[hb][ft], bw[wb]], writes=[bpy[k4]], inc=(ft == 1))
                    hs = slice(half * 512, (half + 1) * 512)
                    ya = yacc[:, ti, hs]
                    gcol = GATE[:, gi, ex:ex + 1]
                    if ex == 0:
                        Tk.op("dve", lambda e: e.scalar_tensor_tensor(out=ya, in0=py[k4][:], scalar=gcol, in1=xt[gi % 4][:, hs], op0=ALU.mult, op1=ALU.add), reads=[bpy[k4], bxt[gi % 4]] + allgate, writes=[byacc[ti]])
                    else:
                        Tk.op("dve", lambda e: e.scalar_tensor_tensor(out=ya, in0=py[k4][:], scalar=gcol, in1=ya, op0=ALU.mult, op1=ALU.add), reads=[bpy[k4], byacc[ti]] + allgate, writes=[byacc[ti]])

        def finish(s):
            for ti in range(NTS):
                gi = s * NTS + ti
                p = gi % 2
                rows = slice(gi * 128, (gi + 1) * 128)
                if not last:
                    Tk.dma("sync", self.XR[rows, :], yacc[:, ti, :], reads=[byacc[ti]], writes=[Tk.b("XR", gi)])
                else:
                    Tk.op("dve", lambda e: e.memset(ss[p][:], 0.0), writes=[bss[p]])
                    Tk.op("act", lambda e: e.activation(out=junk[:], in_=yacc[:, ti, :], func=AF.Square, accum_out=ss[p][:]), reads=[byacc[ti], bss[p]], writes=[bjunk, bss[p]])
                    Tk.op("dve", lambda e: e.tensor_scalar(out=ss[p][:], in0=ss[p][:], scalar1=1.0 / D, scalar2=EPS, op0=ALU.mult, op1=ALU.add), reads=[bss[p]], writes=[bss[p]])
                    Tk.op("act", lambda e: e.activation(out=ss[p][:], in_=ss[p][:], func=AF.Sqrt), reads=[bss[p]], writes=[bss[p]])
                    Tk.op("dve", lambda e: e.reciprocal(out=ss[p][:], in_=ss[p][:]), reads=[bss[p]], writes=[bss[p]])
                    xb = gi % 4
                    Tk.op("dve", lambda e: e.scalar_tensor_tensor(out=xt[xb][:], in0=yacc[:, ti, :], scalar=ss[p][:, 0:1], in1=fgbc[:], op0=ALU.mult, op1=ALU.mult), reads=[byacc[ti], bss[p], bfg], writes=[bxt[xb]])
                    Tk.dma("sync", self.out[rows, :], xt[xb][:], reads=[bxt[xb]], writes=[Tk.b("OUT", gi)])

        for c4 in range(NC4):
            load_xn(0, c4)
        for gex in range(min(NW, NS * 32)):
            load_w(gex)
        stA(0, 0)
        stA(0, 1)
        for n in range(NBk):
            s, ex, c4 = blocks[n]
            if n + 1 < NBk:
                stA(n + 1, 0)
            stD(n)
            if c4 == NC4 - 1 and s * 32 + ex + NW < NS * 32:
                load_w(s * 32 + ex + NW)
            if ex == 31 and c4 == NC4 - 1:
                finish(s)
            if n + 1 < NBk:
                stA(n + 1, 1)
        Tk.barrier()


MK.p5 = _p5
MK.p6 = _p6


def kernel(**inputs):
    x = np.asarray(inputs["x"], dtype=np.float32)
    B, T, _ = x.shape
    mk = MK(T=T, depth=2)
    nc = mk.build()
    shared = {k: np.ascontiguousarray(np.asarray(inputs[k], dtype=np.float32)) for k in INPUT_SHAPES}
    in_maps = []
    for b in range(B):
        d = dict(shared)
        d["x"] = np.ascontiguousarray(x[b])
        in_maps.append(d)
    res = run_bass_kernel_spmd(nc, in_maps, core_ids=list(range(B)))
    return np.stack([np.asarray(r["out"], dtype=np.float32) for r in res.results], axis=0)
```

```python
import math
from contextlib import ExitStack

import numpy as np
import concourse.bass as bass
import concourse.mybir as mybir
from concourse.bass_utils import run_bass_kernel_spmd

F32 = mybir.dt.float32
BF16 = mybir.dt.bfloat16
I32 = mybir.dt.int32
AF = mybir.ActivationFunctionType
ALU = mybir.AluOpType
AX = mybir.AxisListType

D = 1024
HD = 64
IN_COLS = 1248
EPS = 1e-6
PI = math.pi


class Buf:
    __slots__ = ("last_w", "readers")

    def __init__(self):
        self.last_w = None
        self.readers = {}


class Trk:
    def __init__(self, nc, es):
        self.nc = nc
        self.eng = {"pe": nc.tensor, "act": nc.scalar, "dve": nc.vector, "pool": nc.gpsimd, "sync": nc.sync}
        self.sem = {}
        self.cnt = {}
        self.waited = {k: {} for k in self.eng}
        for k in ("pe", "act", "dve", "pool"):
            self.sem[k] = es.enter_context(nc.semaphore("s_" + k))
            self.cnt[k] = 0
        self.lanes = {}
        self.lane_sem = {}
        self.lane_i = {}
        for q, n in {"sync": 16, "act": 8, "pool": 8}.items():
            self.lanes[q] = []
            for i in range(n):
                nm = "%s%d" % (q, i)
                s = es.enter_context(nc.semaphore("l_" + nm))
                self.lanes[q].append([s, 0, nm])
                self.lane_sem[nm] = s
            self.lane_i[q] = 0
        self.bufs = {}

    def b(self, *key):
        v = self.bufs.get(key)
        if v is None:
            v = self.bufs[key] = Buf()
        return v

    def _wait(self, e, tok):
        kind, key, val = tok
        w = self.waited[e]
        if w.get(key, 0) >= val:
            return
        w[key] = val
        sem = self.sem[key] if kind == "e" else self.lane_sem[key]
        self.eng[e].wait_ge(sem, val)

    def _deps(self, e, reads, writes):
        deps = []
        for b in reads:
            if b.last_w is not None:
                deps.append(b.last_w)
        for b in writes:
            lw = b.last_w
            if lw is not None and not (e == "pe" and lw[0] == "e" and lw[1] == e):
                deps.append(lw)
            for t in b.readers.values():
                if e == "pe" and t[0] == "e" and t[1] == e:
                    continue
                deps.append(t)
        for d in deps:
            self._wait(e, d)

    def _record(self, tok, reads, writes):
        for b in reads:
            b.readers[tok[1]] = tok
        for b in writes:
            b.last_w = tok
            b.readers = {}

    def op(self, e, fn, reads=(), writes=(), inc=True):
        self._deps(e, reads, writes)
        ins = fn(self.eng[e])
        if inc:
            self.cnt[e] += 1
            ins.then_inc(self.sem[e], 1)
            tok = ("e", e, self.cnt[e])
        else:
            tok = ("e", e, self.cnt[e] + 1)
        self._record(tok, reads, writes)
        return ins

    def dma(self, q, out, in_, reads=(), writes=(), **kw):
        lanes = self.lanes[q]
        i = self.lane_i[q]
        self.lane_i[q] = (i + 1) % len(lanes)
        lane = lanes[i]
        if lane[1] > 0:
            self._wait(q, ("d", lane[2], lane[1]))
        self._deps(q, reads, writes)
        ins = self.eng[q].dma_start(out=out, in_=in_, **kw)
        lane[1] += 16
        ins.then_inc(lane[0], 16)
        self._record(("d", lane[2], lane[1]), reads, writes)
        return ins

    def barrier(self):
        for e in self.eng:
            for k in self.sem:
                if k != e and self.cnt[k] > 0:
                    self._wait(e, ("e", k, self.cnt[k]))
            for q, lanes in self.lanes.items():
                for lane in lanes:
                    if lane[1] > 0:
                        self._wait(e, ("d", lane[2], lane[1]))

    def finish(self, bufs, e="sync"):
        for b in bufs:
            if b.last_w is not None:
                self._wait(e, b.last_w)


INPUT_SHAPES = {
    "ln1_g": [2, 1024], "w_in": [2, 1024, 1248], "attn_sink": [2, 6],
    "ssm_lam_re": [2, 2, 16, 64], "ssm_lam_im": [2, 2, 16, 64], "ssm_log_dt": [2, 2, 16],
    "ssm_b_re": [2, 2, 16, 64, 16], "ssm_b_im": [2, 2, 16, 64, 16],
    "ssm_c_re": [2, 2, 16, 16, 64], "ssm_c_im": [2, 2, 16, 16, 64],
    "ssm_d": [2, 256], "ssm_w_glu": [2, 256, 512], "ssm_b_glu": [2, 512],
    "mla_q_norm_g": [2, 192], "mla_w_uq": [2, 192, 576], "mla_kv_norm_g": [2, 128], "mla_w_ukv": [2, 128, 768],
    "out_g_attn": [2, 384], "out_g_ssm": [2, 256], "out_g_mla": [2, 384], "w_out": [2, 1024, 1024],
    "ln2_g": [2, 1024], "w_router_group": [2, 1024, 4], "b_router_group": [2, 4],
    "w_router_expert": [2, 1024, 32], "b_router_expert": [2, 32],
    "w_gate": [2, 4, 8, 1024, 256], "w_up": [2, 4, 8, 1024, 256], "w_down": [2, 4, 8, 256, 1024],
    "final_g": [1024],
}


class MK:
    def __init__(self, T=4096, depth=2, phases=None, debug=()):
        self.T = T
        self.NT = T // 128
        self.NCH = T // 512
        self.depth = depth
        self.debug = set(debug)
        self.phases = phases
        self.nc = nc = bass.Bass("TRN2", target_bir_lowering=False)
        self.es = ExitStack()
        self.trk = Trk(nc, self.es)
        self.din = {}
        self.din["x"] = nc.dram_tensor("x", [T, D], F32, kind="ExternalInput").ap()
        for k, shp in INPUT_SHAPES.items():
            self.din[k] = nc.dram_tensor(k, shp, F32, kind="ExternalInput").ap()
        self.out = nc.dram_tensor("out", [T, D], F32, kind="ExternalOutput").ap()
        self.dbg_out = []
        self.uid = 0
        self.HT = self.dram("HT", [D, T], BF16)
        self.XN2T = self.dram("XN2T", [D, T], BF16)
        self.XR = self.dram("XR", [T, D], F32)

    def sb(self, es, name, shape, dt):
        self.uid += 1
        return es.enter_context(self.nc.sbuf_tensor("%s_%d" % (name, self.uid), list(shape), dt))

    def ps(self, es, name, shape, dt):
        self.uid += 1
        return es.enter_context(self.nc.psum_tensor("%s_%d" % (name, self.uid), list(shape), dt))

    def dram(self, name, shape, dt):
        return self.nc.dram_tensor(name, list(shape), dt, kind="Internal").ap()

    def dump(self, name, ap, shape, dt, bufs):
        if name not in self.debug:
            return
        o = self.nc.dram_tensor("dbg_" + name, list(shape), dt, kind="ExternalOutput").ap()
        ob = Buf()
        self.trk.dma("sync", o, ap, reads=bufs, writes=[ob])
        self.dbg_out.append(ob)

    def dump_dram(self, name, ap, shape, dt):
        o = self.nc.dram_tensor("dbg_" + name, list(shape), dt, kind="ExternalOutput").ap()
        with ExitStack() as es:
            rows = shape[0]
            t = self.sb(es, "dd", [128, shape[1]], dt)
            for r0 in range(0, rows, 128):
                b1, ob = Buf(), Buf()
                n = min(128, rows - r0)
                self.trk.barrier()
                self.trk.dma("sync", t[0:n, :], ap[r0:r0 + n, :], writes=[b1])
                self.trk.dma("sync", o[r0:r0 + n, :], t[0:n, :], reads=[b1], writes=[ob])
                self.dbg_out.append(ob)
            self.trk.finish(self.dbg_out)
            self.trk.barrier()

    def range_reduce(self, es, ang, shape, bufs, eng="dve", scratch=None):
        Tk = self.trk
        if scratch is None:
            it = self.sb(es, "rr_i", shape, I32)
            kt = self.sb(es, "rr_k", shape, F32)
            bi, bk = Buf(), Buf()
        else:
            it, kt, bi, bk = scratch
        sl = tuple(slice(None) for _ in shape)
        Tk.op(eng, lambda e: e.tensor_scalar(out=it[sl], in0=ang, scalar1=float(1 / (2 * PI)), scalar2=None, op0=ALU.mult), reads=bufs, writes=[bi])
        Tk.op(eng, lambda e: e.tensor_copy(out=kt[sl], in_=it[sl]), reads=[bi], writes=[bk])
        Tk.op(eng, lambda e: e.scalar_tensor_tensor(out=ang, in0=kt[sl], scalar=float(-2 * PI), in1=ang, op0=ALU.mult, op1=ALU.add), reads=[bk] + bufs, writes=bufs)
        Tk.op(eng, lambda e: e.tensor_scalar(out=kt[sl], in0=ang, scalar1=float(PI), scalar2=float(-2 * PI), op0=ALU.is_gt, op1=ALU.mult), reads=bufs, writes=[bk])
        Tk.op(eng, lambda e: e.tensor_tensor(out=ang, in0=ang, in1=kt[sl], op=ALU.add), reads=[bk] + bufs, writes=bufs)
        Tk.op(eng, lambda e: e.tensor_scalar(out=kt[sl], in0=ang, scalar1=float(-PI), scalar2=float(2 * PI), op0=ALU.is_lt, op1=ALU.mult), reads=bufs, writes=[bk])
        Tk.op(eng, lambda e: e.tensor_tensor(out=ang, in0=ang, in1=kt[sl], op=ALU.add), reads=[bk] + bufs, writes=bufs)

    def consts(self):
        nc, Tk, es, T = self.nc, self.trk, self.es, self.T
        self.ident_bf = self.sb(es, "ident_bf", [128, 128], BF16)
        self.ident_f = self.sb(es, "ident_f", [128, 128], F32)
        self.ones_bf = self.sb(es, "ones_bf", [128, 128], BF16)
        self.b_const = Buf()
        bc = self.b_const
        for t in (self.ident_bf, self.ident_f):
            Tk.op("pool", lambda e, t=t: e.memset(t[:], 1.0), writes=[bc])
            Tk.op("pool", lambda e, t=t: e.affine_select(out=t[:], in_=t[:], pattern=[[-1, 128]], compare_op=ALU.is_equal, fill=0.0, base=0, channel_multiplier=1), reads=[bc], writes=[bc])
        Tk.op("pool", lambda e: e.memset(self.ones_bf[:], 1.0), writes=[bc])
        self.ROPE = self.dram("ROPE", [2, 32, T], F32)
        self.b_rope = Buf()
        with ExitStack() as tes:
            self.rope_cos = self.sb(tes, "rope_cos", [128, T], F32)
            self.rope_sin = self.sb(tes, "rope_sin", [128, T], F32)
            pi_ = self.sb(tes, "pi", [128, 1], I32)
            pf = self.sb(tes, "pf", [128, 1], F32)
            qi = self.sb(tes, "qi", [128, 1], I32)
            qf = self.sb(tes, "qf", [128, 1], F32)
            inv = self.sb(tes, "inv", [128, 1], F32)
            ti = self.sb(tes, "ti", [128, T], I32)
            tf = self.sb(tes, "tf", [128, T], F32)
            ang = self.sb(tes, "ang", [128, T], F32)
            b1, b2, b3 = Buf(), Buf(), Buf()
            Tk.op("pool", lambda e: e.iota(pi_[:], pattern=[[0, 1]], base=0, channel_multiplier=1), writes=[b1])
            Tk.op("dve", lambda e: e.tensor_copy(out=pf[:], in_=pi_[:]), reads=[b1], writes=[b1])
            Tk.op("dve", lambda e: e.tensor_scalar(out=qi[:], in0=pf[:], scalar1=-7.5, scalar2=1.0 / 16, op0=ALU.add, op1=ALU.mult), reads=[b1], writes=[b2])
            Tk.op("dve", lambda e: e.tensor_copy(out=qf[:], in_=qi[:]), reads=[b2], writes=[b2])
            Tk.op("dve", lambda e: e.scalar_tensor_tensor(out=pf[:], in0=qf[:], scalar=-16.0, in1=pf[:], op0=ALU.mult, op1=ALU.add), reads=[b1, b2], writes=[b1])
            Tk.op("act", lambda e: e.activation(out=inv[:], in_=pf[:], func=AF.Exp, scale=float(-math.log(10000.0) / 16)), reads=[b1], writes=[b3])
            Tk.op("pool", lambda e: e.iota(ti[:], pattern=[[1, T]], base=0, channel_multiplier=0), writes=[b2])
            Tk.op("dve", lambda e: e.tensor_copy(out=tf[:], in_=ti[:]), reads=[b2], writes=[b2])
            bang = Buf()
            rrs = (self.sb(tes, "rr_i", [128, T], I32), self.sb(tes, "rr_k", [128, T], F32), Buf(), Buf())
            for tab, shift in ((self.rope_sin, 0.0), (self.rope_cos, PI / 2)):
                Tk.op("dve", lambda e: e.tensor_scalar(out=ang[:], in0=tf[:], scalar1=inv[:, 0:1], scalar2=float(shift), op0=ALU.mult, op1=ALU.add), reads=[b2, b3], writes=[bang])
                self.range_reduce(tes, ang[:], [128, T], [bang], scratch=rrs)
                Tk.op("act", lambda e, tab=tab: e.activation(out=tab[:], in_=ang[:], func=AF.Sin), reads=[bang], writes=[self.b_rope])
            bt = self.b_rope
            self.b_rope = Buf()
            Tk.dma("sync", self.ROPE[0], self.rope_cos[64:96, :], reads=[bt], writes=[self.b_rope])
            Tk.dma("sync", self.ROPE[1], self.rope_sin[64:96, :], reads=[bt], writes=[self.b_rope])
            Tk.barrier()
        self.GATE = self.sb(es, "GATE", [128, self.NT, 32], F32)

    def p1(self, l, xsrc, xbuf_key):
        nc, Tk, T, NCH = self.nc, self.trk, self.T, self.NCH
        m = self.mix
        with ExitStack() as es:
            w_in_bf = self.sb(es, "w_in_bf", [128, 8, IN_COLS], BF16)
            w_qa = self.sb(es, "w_qa", [128, 8, 384], BF16)
            w_kr = self.sb(es, "w_kr", [128, 8, 96], BF16)
            w_sw = self.sb(es, "w_sw", [128, 8, 96], BF16)
            g1bc = self.sb(es, "g1bc", [128, D], F32)
            xt = [self.sb(es, "xt", [128, D], F32) for _ in range(2)]
            junk = self.sb(es, "junk", [128, D], BF16)
            xn = [self.sb(es, "xn", [128, D], BF16) for _ in range(2)]
            xnT = [self.sb(es, "xnT", [128, 8, 512], BF16) for _ in range(2)]
            ss = [self.sb(es, "ss", [128, 1], F32) for _ in range(2)]
            rstd = [self.sb(es, "rstd", [128, 1], F32) for _ in range(2)]
            kt1 = self.sb(es, "kt1", [128, 512], F32)
            kt2 = self.sb(es, "kt2", [128, 512], F32)
            rp = [self.sb(es, "rp", [128, 2, 512], F32) for _ in range(2)]
            brp = [Buf(), Buf()]
            pT = [self.ps(es, "pT", [128, 1024], BF16) for _ in range(2)]
            pm = [self.ps(es, "pm", [128, 512], F32) for _ in range(4)]
            pva = self.ps(es, "pva", [128, 512], F32)
            bw = Buf()
            bg = Buf()
            Tk.dma("pool", w_in_bf[:], self.din["w_in"][l].rearrange("(kc p) n -> p kc n", p=128), writes=[bw])
            Tk.dma("sync", g1bc[:], self.din["ln1_g"][l:l + 1, :].partition_broadcast(128), writes=[bg])
            bw2 = Buf()
            for j, h in enumerate([0, 3, 1, 4, 2, 5]):
                Tk.op("pool", lambda e, j=j, h=h: e.tensor_copy(out=w_qa[:, :, j * 64:(j + 1) * 64], in_=w_in_bf[:, :, h * 64:(h + 1) * 64]), reads=[bw], writes=[bw2])
            Tk.op("pool", lambda e: e.memset(w_kr[:, :, 0:64], 0.0), writes=[bw2])
            Tk.op("pool", lambda e: e.memset(w_sw[:, :, 0:64], 0.0), writes=[bw2])
            Tk.op("pool", lambda e: e.tensor_copy(out=w_kr[:, :, 64:96], in_=w_in_bf[:, :, 1216:1248]), reads=[bw], writes=[bw2])
            Tk.op("act", lambda e: e.mul(out=w_sw[:, :, 64:80], in_=w_in_bf[:, :, 1232:1248], mul=-1.0), reads=[bw], writes=[bw2])
            Tk.op("act", lambda e: e.copy(out=w_sw[:, :, 80:96], in_=w_in_bf[:, :, 1216:1232]), reads=[bw], writes=[bw2])
            Tk.op("pool", lambda e: e.memset(m["VA"][:, :, :, 64:65], 1.0), writes=[Tk.b("VAones", l)])
            bxt = [Buf(), Buf()]
            bxn = [Buf(), Buf()]
            bss = [Buf(), Buf()]
            bxnT = [Buf(), Buf()]
            bpT = [Buf(), Buf()]
            bpm = [Buf() for _ in range(4)]
            bpva = Buf()
            bjunk = Buf()
            bkt = Buf()
            ev = [0]

            def evac(out, in_, reads, writes):
                e = ("act", "dve")[ev[0] % 2]
                ev[0] += 1
                if e == "act":
                    Tk.op("act", lambda en: en.copy(out=out, in_=in_), reads=reads, writes=writes)
                else:
                    Tk.op("dve", lambda en: en.tensor_copy(out=out, in_=in_), reads=reads, writes=writes)

            pmi = [0]
            for c in range(NCH):
                cb = c % 2
                cols = slice(c * 512, (c + 1) * 512)
                for tt in range(4):
                    i = 4 * c + tt
                    p = i % 2
                    Tk.dma("sync", xt[p][:], xsrc[i * 128:(i + 1) * 128, :], reads=[Tk.b(xbuf_key, i)], writes=[bxt[p]])
                    Tk.op("dve", lambda e: e.memset(ss[p][:], 0.0), writes=[bss[p]])
                    Tk.op("act", lambda e: e.activation(out=junk[:], in_=xt[p][:], func=AF.Square, accum_out=ss[p][:]), reads=[bxt[p], bss[p]], writes=[bjunk, bss[p]])
                    Tk.op("dve", lambda e: e.tensor_scalar(out=ss[p][:], in0=ss[p][:], scalar1=1.0 / D, scalar2=EPS, op0=ALU.mult, op1=ALU.add), reads=[bss[p]], writes=[bss[p]])
                    Tk.op("act", lambda e: e.activation(out=ss[p][:], in_=ss[p][:], func=AF.Sqrt), reads=[bss[p]], writes=[bss[p]])
                    Tk.op("dve", lambda e: e.reciprocal(out=rstd[p][:], in_=ss[p][:]), reads=[bss[p]], writes=[bss[p]])
                    Tk.op("dve", lambda e: e.scalar_tensor_tensor(out=xn[p][:], in0=xt[p][:], scalar=rstd[p][:, 0:1], in1=g1bc[:], op0=ALU.mult, op1=ALU.mult), reads=[bxt[p], bss[p], bg], writes=[bxn[p]])
                    for kc in range(8):
                        Tk.op("pe", lambda e, kc=kc: e.transpose(pT[p][:, kc * 128:(kc + 1) * 128], xn[p][:, kc * 128:(kc + 1) * 128], self.ident_bf[:]),
                              reads=[bxn[p], self.b_const], writes=[bpT[p]], inc=(kc == 7))
                    evac(xnT[cb][:, :, tt * 128:(tt + 1) * 128], pT[p][:].rearrange("p (k t) -> p k t", k=8), [bpT[p]], [bxnT[cb]])
                for v in range(2):
                    Tk.dma("sync", rp[cb][64:96, v, :], self.ROPE[v, :, cols], reads=[self.b_rope], writes=[brp[cb]])
                groups = [
                    (w_qa, 0, 128, ("QA", 0)), (w_qa, 128, 128, ("QA", 1)), (w_qa, 256, 128, ("QA", 2)),
                    (w_in_bf, 384, 128, ("KA", None)),
                    (w_in_bf, 640, 128, ("U", 0)), (w_in_bf, 768, 128, ("U", 1)),
                    (w_in_bf, 896, 128, ("CQ", 0)), (w_in_bf, 1024, 64, ("CQ", 1)),
                    (w_in_bf, 1088, 128, ("CKV", None)),
                    (w_kr, 0, 96, ("KR", "main")), (w_sw, 0, 96, ("KR", "swap")),
                ]
                for (wt, c0, M, (name, sub)) in groups:
                    k = pmi[0] % 4
                    pmi[0] += 1
                    for kc in range(8):
                        Tk.op("pe", lambda e, kc=kc: e.matmul(pm[k][0:M, :], lhsT=wt[:, kc, c0:c0 + M], rhs=xnT[cb][:, kc, :], start=(kc == 0), stop=(kc == 7)),
                              reads=[bw, bw2, bxnT[cb]], writes=[bpm[k]], inc=(kc == 7))
                    if name == "KR":
                        rows = slice(64, 96)
                        if sub == "main":
                            Tk.op("dve", lambda e: e.tensor_tensor(out=kt1[rows, :], in0=pm[k][rows, :], in1=rp[cb][rows, 0, :], op=ALU.mult), reads=[bpm[k], brp[cb]], writes=[bkt])
                        else:
                            Tk.op("dve", lambda e: e.tensor_tensor(out=kt2[rows, :], in0=pm[k][rows, :], in1=rp[cb][rows, 1, :], op=ALU.mult), reads=[bpm[k], brp[cb]], writes=[bkt])
                            Tk.op("dve", lambda e: e.tensor_tensor(out=m["KR"][rows, cols], in0=kt1[rows, :], in1=kt2[rows, :], op=ALU.add), reads=[bkt], writes=[Tk.b("KR", l, c)])
                    else:
                        dst = m[name]
                        o = dst[0:M, cols] if sub is None else dst[0:M, sub, cols]
                        evac(o, pm[k][0:M, :], [bpm[k]], [Tk.b(name, l, c)])
                for tt in range(4):
                    i = 4 * c + tt
                    for kc in range(8):
                        Tk.op("pe", lambda e, kc=kc: e.matmul(pva[:, tt * 128:(tt + 1) * 128], lhsT=xnT[cb][:, kc, tt * 128:(tt + 1) * 128], rhs=w_in_bf[:, kc, 512:640], start=(kc == 0), stop=(kc == 7)),
                              reads=[bw, bxnT[cb]], writes=[bpva], inc=(kc == 7))
                    Tk.op("dve", lambda e: e.tensor_copy(out=m["VA"][:, i, :, 0:64], in_=pva[:, tt * 128:(tt + 1) * 128].rearrange("p (h d) -> p h d", h=2)), reads=[bpva], writes=[Tk.b("VA", l, i)])
            Tk.barrier()

    def alloc_mix(self, es_list):
        T, NT = self.T, self.NT
        m = self.mix = {}
        es_mla, es_u, es_attn = es_list
        m["CQ"] = self.sb(es_mla, "CQ", [128, 2, T], BF16)
        m["CKV"] = self.sb(es_mla, "CKV", [128, T], BF16)
        m["KR"] = self.sb(es_mla, "KR", [128, T], BF16)
        m["U"] = self.sb(es_u, "U", [128, 2, T], BF16)
        m["QA"] = self.sb(es_attn, "QA", [128, 3, T], BF16)
        m["KA"] = self.sb(es_attn, "KA", [128, T], BF16)
        m["VA"] = self.sb(es_attn, "VA", [128, NT, 2, 65], BF16)

    def build(self):
        Tk = self.trk
        self.consts()
        xsrc, xkey = self.din["x"], "xin"
        for l in range(self.depth):
            es_mla, es_u, es_attn = ExitStack(), ExitStack(), ExitStack()
            self.alloc_mix([es_mla, es_u, es_attn])
            self.p1(l, xsrc, xkey)
            if "p1" in self.debug:
                m, T, NT = self.mix, self.T, self.NT
                allb = list(Tk.bufs.values())
                self.debug |= {"QA", "KA", "VA", "U", "CQ", "CKV", "KR"}
                self.dump("QA", m["QA"][:], [128, 3, T], BF16, allb)
                self.dump("KA", m["KA"][:], [128, T], BF16, allb)
                self.dump("VA", m["VA"][:], [128, NT, 2, 65], BF16, allb)
                self.dump("U", m["U"][:], [128, 2, T], BF16, allb)
                self.dump("CQ", m["CQ"][:], [128, 2, T], BF16, allb)
                self.dump("CKV", m["CKV"][:], [128, T], BF16, allb)
                self.dump("KR", m["KR"][:], [128, T], BF16, allb)
            if self.phases == "p1":
                Tk.barrier()
                es_attn.close(); es_u.close(); es_mla.close()
                break
            self.p2(l)
            es_attn.close()
            if self.phases == "p2":
                self.dump_dram("HT", self.HT, [D, self.T], BF16)
                es_u.close(); es_mla.close()
                break
            self.p3(l)
            es_u.close()
            if self.phases == "p3":
                self.dump_dram("HT", self.HT, [D, self.T], BF16)
                es_mla.close()
                break
            self.p4(l)
            es_mla.close()
            if self.phases == "p4":
                self.dump_dram("HT", self.HT, [D, self.T], BF16)
                break
            self.p5(l, xsrc, xkey)
            if self.phases == "p5":
                self.dump("GATE", self.GATE[:], [128, self.NT, 32], F32, list(Tk.bufs.values()))
                self.dump_dram("XR", self.XR, [self.T, D], F32)
                break
            self.p6(l, last=(l == self.depth - 1))
            xsrc, xkey = self.XR, "XR"
        Tk.finish(self.dbg_out)
        Tk.finish([b for k, b in Tk.bufs.items() if k[0] == "OUT"])
        Tk.barrier()
        self.es.close()
        return self.nc


def _p2(self, l):
    nc, Tk, T, NT = self.nc, self.trk, self.T, self.NT
    m = self.mix
    QA, KA, VA = m["QA"], m["KA"], m["VA"]
    slopes = [2.0 ** (-8.0 * (h + 1) / 6) for h in range(6)]
    with ExitStack() as es:
        bias = self.sb(es, "bias", [128, 6, 384], F32)
        di = self.sb(es, "di", [128, 384], I32)
        df = self.sb(es, "df", [128, 384], F32)
        dn = self.sb(es, "dn", [128, 384], F32)
        pen = self.sb(es, "pen", [128, 384], F32)
        esk = self.sb(es, "esk", [128, 6], F32)
        gat = self.sb(es, "gat", [64, 6], F32)
        ones_f = self.sb(es, "ones_f", [128, 64], F32)
        NB = 3
        tmp = [self.sb(es, "tmp", [128, 384], F32) for _ in range(NB)]
        PT = [self.sb(es, "PT", [128, 384], BF16) for _ in range(NB)]
        Osb2 = [self.sb(es, "Osb", [65, 768], F32) for _ in range(2)]
        rd2 = [self.sb(es, "rd", [65, 768], F32) for _ in range(2)]
        o2 = [self.sb(es, "o", [64, 768], F32) for _ in range(2)]
        sq2 = [self.sb(es, "sq", [64, 768], BF16) for _ in range(2)]
        rs2 = [self.sb(es, "rs", [64, 128], F32) for _ in range(2)]
        hT = [self.sb(es, "hT", [64, 6, 128], BF16) for _ in range(2)]
        pS = [self.ps(es, "pS", [128, 512], F32) for _ in range(NB)]
        pO = [self.ps(es, "pO", [128, 512], F32) for _ in range(2)]
        pB = [self.ps(es, "pB", [128, 512], F32) for _ in range(2)]
        pSS = self.ps(es, "pSS", [128, 512], F32)
        bb = Buf()
        Tk.op("pool", lambda e: e.iota(di[:].rearrange("p (a q) -> p a q", a=3), pattern=[[128, 3], [-1, 128]], base=-128, channel_multiplier=1), writes=[bb])
        Tk.op("dve", lambda e: e.tensor_copy(out=df[:], in_=di[:]), reads=[bb], writes=[bb])
        Tk.op("dve", lambda e: e.tensor_scalar(out=dn[:], in0=df[:], scalar1=-1.0, scalar2=None, op0=ALU.mult), reads=[bb], writes=[bb])
        Tk.op("dve", lambda e: e.tensor_tensor(out=df[:], in0=df[:], in1=dn[:], op=ALU.max), reads=[bb], writes=[bb])
        Tk.op("dve", lambda e: e.tensor_scalar(out=pen[:], in0=df[:], scalar1=128.0, scalar2=-30000.0, op0=ALU.is_gt, op1=ALU.mult), reads=[bb], writes=[bb])
        for h in range(6):
            Tk.op("dve", lambda e: e.scalar_tensor_tensor(out=bias[:, h, :], in0=df[:], scalar=float(-slopes[h]), in1=pen[:], op0=ALU.mult, op1=ALU.add), reads=[bb], writes=[bb])
        Tk.op("pool", lambda e: e.memset(ones_f[:], 1.0), writes=[bb])
        Tk.dma("sync", esk[64:65, :], self.din["attn_sink"][l:l + 1, :], writes=[bb])
        Tk.op("act", lambda e: e.activation(out=esk[64:65, :], in_=esk[64:65, :], func=AF.Exp), reads=[bb], writes=[bb])
        Tk.dma("sync", gat[:], self.din["out_g_attn"][l].rearrange("(h d) -> d h", d=64), writes=[bb], allow_slow_non_contiguous=True)
        allin = [Tk.b(n, l, c) for n in ("QA", "KA") for c in range(self.NCH)] + [Tk.b("VA", l, i) for i in range(NT)] + [Tk.b("VAones", l)]
        btmp, bPT, bpS = [[Buf() for _ in range(NB)] for _ in range(3)]
        bpO, bpB, bpSS = Buf(), Buf(), Buf()
        bOsb2, brd2, bo2, bsq2, brs2 = [[Buf(), Buf()] for _ in range(5)]
        bhT = [Buf(), Buf()]
        steps = [(i, h) for i in range(NT) for h in range(6)]

        def geo(i):
            dds = [dd for dd in range(3) if 0 <= i + dd - 1 < NT]
            return dds, dds[0] * 128, (dds[-1] + 1) * 128

        def stS(n):
            i, h = steps[n]
            kv, j = h // 3, h % 3
            rows = slice(kv * 64, kv * 64 + 64)
            k = n % NB
            dds, c0, c1 = geo(i)
            qs = slice(i * 128, (i + 1) * 128)
            for dd in dds:
                ks = slice((i + dd - 1) * 128, (i + dd) * 128)
                Tk.op("pe", lambda e: e.matmul(pS[k][:, dd * 128:(dd + 1) * 128], lhsT=KA[rows, ks], rhs=QA[rows, j, qs], start=True, stop=True),
                      reads=allin, writes=[bpS[k]], inc=(dd == dds[-1]))

        def stP(n):
            i, h = steps[n]
            kv = h // 3
            k = n % NB
            dds, c0, c1 = geo(i)
            Tk.op("dve", lambda e: e.scalar_tensor_tensor(out=tmp[k][:, c0:c1], in0=pS[k][:, c0:c1], scalar=0.125, in1=bias[:, h, c0:c1], op0=ALU.mult, op1=ALU.add),
                  reads=[bpS[k], bb], writes=[btmp[k]])
            Tk.op("act", lambda e: e.activation(out=PT[k][:, c0:c1], in_=tmp[k][:, c0:c1], func=AF.Exp), reads=[btmp[k]], writes=[bPT[k]])
            po = pO[h // 4][0:65, (h % 4) * 128:(h % 4 + 1) * 128]
            for dd in dds:
                Tk.op("pe", lambda e: e.matmul(po, lhsT=VA[:, i + dd - 1, kv, :], rhs=PT[k][:, dd * 128:(dd + 1) * 128], start=(dd == dds[0]), stop=(dd == dds[-1])),
                      reads=allin + [bPT[k]], writes=[bpO], inc=(dd == dds[-1]))

        def epilogue(i):
            qs = slice(i * 128, (i + 1) * 128)
            hb = i % 2
            Osb, rd, o, sq, rs = Osb2[hb], rd2[hb], o2[hb], sq2[hb], rs2[hb]
            bOsb, brd, bo, bsq, brs = bOsb2[hb], brd2[hb], bo2[hb], bsq2[hb], brs2[hb]
            o3 = o[:].rearrange("p (h q) -> p h q", h=6)
            g = [[] for _ in range(6)]
            g[0].append(lambda: Tk.op("act", lambda e: e.copy(out=Osb[0:65, 0:512], in_=pO[0][0:65, :]), reads=[bpO], writes=[bOsb]))
            g[0].append(lambda: Tk.op("act", lambda e: e.copy(out=Osb[0:65, 512:768], in_=pO[1][0:65, 0:256]), reads=[bpO], writes=[bOsb]))
            g[1].append(lambda: Tk.op("dve", lambda e: e.tensor_tensor(out=rd[64:65, :].rearrange("p (h q) -> p h q", h=6), in0=Osb[64:65, :].rearrange("p (h q) -> p h q", h=6),
                                                                       in1=esk[64:65, :].unsqueeze(2).to_broadcast([1, 6, 128]), op=ALU.add), reads=[bOsb, bb], writes=[brd]))
            g[1].append(lambda: Tk.op("act", lambda e: e.activation(out=rd[64:65, :], in_=rd[64:65, :], func=AF.Ln), reads=[brd], writes=[brd]))
            g[1].append(lambda: Tk.op("act", lambda e: e.activation(out=rd[64:65, :], in_=rd[64:65, :], func=AF.Exp, scale=-1.0), reads=[brd], writes=[brd]))
            g[2].append(lambda: Tk.op("pe", lambda e: e.matmul(pB[0][0:64, :], lhsT=ones_f[64:65, 0:64], rhs=rd[64:65, 0:512], start=True, stop=True), reads=[brd, bb], writes=[bpB]))
            g[2].append(lambda: Tk.op("pe", lambda e: e.matmul(pB[1][0:64, 0:256], lhsT=ones_f[64:65, 0:64], rhs=rd[64:65, 512:768], start=True, stop=True), reads=[brd, bb], writes=[bpB]))
            g[2].append(lambda: Tk.op("dve", lambda e: e.tensor_tensor(out=o[:, 0:512], in0=Osb[0:64, 0:512], in1=pB[0][0:64, :], op=ALU.mult), reads=[bOsb, bpB], writes=[bo]))
            g[2].append(lambda: Tk.op("dve", lambda e: e.tensor_tensor(out=o[:, 512:768], in0=Osb[0:64, 512:768], in1=pB[1][0:64, 0:256], op=ALU.mult), reads=[bOsb, bpB], writes=[bo]))
            g[3].append(lambda: Tk.op("act", lambda e: e.activation(out=sq[:], in_=o[:], func=AF.Square), reads=[bo], writes=[bsq]))

            def ssq():
                for h in range(6):
                    Tk.op("pe", lambda e: e.matmul(pSS[0:64, 0:128], lhsT=self.ones_bf[0:64, 0:64], rhs=sq[:, h * 128:(h + 1) * 128], start=(h == 0), stop=(h == 5)),
                          reads=[bsq, self.b_const], writes=[bpSS], inc=(h == 5))
            g[4].append(ssq)
            g[4].append(lambda: Tk.op("dve", lambda e: e.tensor_scalar(out=rs[:], in0=pSS[0:64, 0:128], scalar1=1.0 / 384, scalar2=EPS, op0=ALU.mult, op1=ALU.add), reads=[bpSS], writes=[brs]))
            g[4].append(lambda: Tk.op("act", lambda e: e.activation(out=rs[:], in_=rs[:], func=AF.Ln), reads=[brs], writes=[brs]))
            g[5].append(lambda: Tk.op("act", lambda e: e.activation(out=rs[:], in_=rs[:], func=AF.Exp, scale=-0.5), reads=[brs], writes=[brs]))
            g[5].append(lambda: Tk.op("dve", lambda e: e.tensor_tensor(out=o3, in0=o3, in1=rs[:].unsqueeze(1).to_broadcast([64, 6, 128]), op=ALU.mult), reads=[bo, brs], writes=[bo]))
            g[5].append(lambda: Tk.op("dve", lambda e: e.tensor_tensor(out=hT[hb][:], in0=o3, in1=gat[:].unsqueeze(2).to_broadcast([64, 6, 128]), op=ALU.mult), reads=[bo, bb], writes=[bhT[hb]]))
            g[5].append(lambda: Tk.dma("sync", self.HT[0:384, qs].rearrange("(h d) q -> d h q", d=64), hT[hb][:], reads=[bhT[hb]], writes=[Tk.b("HT", l, "attn", i)]))
            return g

        NS_ = len(steps)
        pend = []
        for n in range(NS_ + 1):
            if n < NS_:
                stS(n)
            if n >= 1:
                stP(n - 1)
                i, h = steps[n - 1]
                if h == 5:
                    pend += [(n + 2 * gi_, grp) for gi_, grp in enumerate(epilogue(i))]
                    pend.sort(key=lambda t: t[0])
                while pend and pend[0][0] <= n:
                    for f in pend.pop(0)[1]:
                        f()
        for _, grp in pend:
            for f in grp:
                f()
        Tk.barrier()


MK.p2 = _p2


def rev_ap(a):
    ap = [list(d) for d in a.ap]
    step, n = ap[-1]
    ap[-1] = [-step, n]
    return bass.AP(a.tensor, a.offset + (n - 1) * step, ap)


def _p3(self, l):
    nc, Tk, T = self.nc, self.trk, self.T
    Lc = 128
    NC = T // Lc
    U = self.mix["U"]
    din = self.din
    allU = [Tk.b("U", l, c) for c in range(self.NCH)]
    with ExitStack() as es:
        CS = self.sb(es, "CS", [128, 32, 2 * Lc], F32)
        G1 = self.sb(es, "G1", [128, 32, 128], BF16)
        G2 = self.sb(es, "G2", [128, 32, 128], BF16)
        C1 = self.sb(es, "C1", [128, 32, 128], BF16)
        C2 = self.sb(es, "C2", [128, 32, 128], BF16)
        R = self.sb(es, "R", [128, 32, 128], F32)
        mag = self.sb(es, "mag", [128, 32], F32)
        diagD = self.sb(es, "diagD", [128, 2, 128], BF16)
        carry = self.sb(es, "carry", [128, 32], F32)
        bP = Buf()
        with ExitStack() as tes:
            xd = self.sb(tes, "xd", [32, 2, 128], F32)
            LR = self.sb(tes, "LR", [128, 32], F32)
            LI = self.sb(tes, "LI", [128, 32], F32)
            dt = self.sb(tes, "dt", [128, 32], F32)
            th = self.sb(tes, "th", [128, 32], F32)
            cth = self.sb(tes, "cth", [128, 32], F32)
            sth = self.sb(tes, "sth", [128, 32], F32)
            ar = self.sb(tes, "ar", [128, 32], F32)
            ai = self.sb(tes, "ai", [128, 32], F32)
            den = self.sb(tes, "den", [128, 32], F32)
            t1 = self.sb(tes, "t1", [128, 32], F32)
            t2 = self.sb(tes, "t2", [128, 32], F32)
            fr = self.sb(tes, "fr", [128, 32], F32)
            fi = self.sb(tes, "fi", [128, 32], F32)
            frs = self.sb(tes, "frs", [128, 32], F32)
            fis = self.sb(tes, "fis", [128, 32], F32)
            sgn = self.sb(tes, "sgn", [128, 1], F32)
            Pm = self.sb(tes, "Pm", [128, 32, 16], F32)
            Qm = self.sb(tes, "Qm", [128, 32, 16], F32)
            GT1 = self.sb(tes, "GT1", [128, 32, 16], F32)
            GT2 = self.sb(tes, "GT2", [128, 32, 16], F32)
            gtmp = self.sb(tes, "gtmp", [128, 32, 16], F32)
            TB = self.sb(tes, "TB", [128, 128], F32)
            rowmask = self.sb(tes, "rowmask", [128, 8], F32)
            CT = self.sb(tes, "CT", [128, 2, 128], F32)
            crl = self.sb(tes, "crl", [128, 64], F32)
            cil = self.sb(tes, "cil", [128, 64], F32)
            shift = self.sb(tes, "shift", [128, 128], F32)
            dcol = self.sb(tes, "dcol", [128, 2], F32)
            iot = self.sb(tes, "iot", [128, Lc], I32)
            iof = self.sb(tes, "iof", [128, Lc], F32)
            ang = self.sb(tes, "ang", [128, 8, Lc], F32)
            pp_ = self.ps(tes, "pprep", [128, 512], F32)
            b = Buf()
            bps = Buf()
            for name, dst in (("ssm_lam_re", LR), ("ssm_lam_im", LI)):
                src = din[name][l].rearrange("d g n -> (d g) n")
                Tk.dma("sync", xd[:, 0, 0:64], src, writes=[b])
                Tk.dma("sync", xd[:, 0, 64:128], src, writes=[b])
                Tk.op("pe", lambda e: e.matmul(pp_[:, 0:32], lhsT=xd[:, 0, :], rhs=self.ident_f[0:32, 0:32], start=True, stop=True), reads=[b, self.b_const], writes=[bps])
                Tk.op("dve", lambda e: e.tensor_copy(out=dst[:], in_=pp_[:, 0:32]), reads=[bps], writes=[b])
            Tk.dma("sync", dt[:], din["ssm_log_dt"][l:l + 1].rearrange("o d g -> o (d g)").partition_broadcast(128), writes=[b])
            Tk.op("act", lambda e: e.activation(out=dt[:], in_=dt[:], func=AF.Exp), reads=[b], writes=[b])
            Tk.op("dve", lambda e: e.tensor_tensor(out=t1[:], in0=LR[:], in1=dt[:], op=ALU.mult), reads=[b], writes=[b])
            Tk.op("act", lambda e: e.activation(out=mag[:], in_=t1[:], func=AF.Exp), reads=[b], writes=[bP])
            Tk.op("dve", lambda e: e.tensor_tensor(out=th[:], in0=LI[:], in1=dt[:], op=ALU.mult), reads=[b], writes=[b])
            rrs_s = (self.sb(tes, "rr_i", [128, 32], I32), self.sb(tes, "rr_k", [128, 32], F32), Buf(), Buf())
            for dst, shf in ((sth, 0.0), (cth, PI / 2)):
                Tk.op("dve", lambda e: e.tensor_scalar(out=t2[:], in0=th[:], scalar1=float(shf), scalar2=None, op0=ALU.add), reads=[b], writes=[b])
                self.range_reduce(tes, t2[:], [128, 32], [b], scratch=rrs_s)
                Tk.op("act", lambda e: e.activation(out=dst[:], in_=t2[:], func=AF.Sin), reads=[b], writes=[b])
            Tk.op("dve", lambda e: e.tensor_tensor(out=ar[:], in0=mag[:], in1=cth[:], op=ALU.mult), reads=[b, bP], writes=[b])
            Tk.op("dve", lambda e: e.tensor_tensor(out=ai[:], in0=mag[:], in1=sth[:], op=ALU.mult), reads=[b, bP], writes=[b])
            Tk.op("dve", lambda e: e.tensor_scalar(out=ar[:], in0=ar[:], scalar1=-1.0, scalar2=None, op0=ALU.add), reads=[b], writes=[b])
            Tk.op("dve", lambda e: e.tensor_tensor(out=den[:], in0=LR[:], in1=LR[:], op=ALU.mult), reads=[b], writes=[b])
            Tk.op("dve", lambda e: e.tensor_tensor(out=t1[:], in0=LI[:], in1=LI[:], op=ALU.mult), reads=[b], writes=[b])
            Tk.op("dve", lambda e: e.tensor_tensor(out=den[:], in0=den[:], in1=t1[:], op=ALU.add), reads=[b], writes=[b])
            Tk.op("dve", lambda e: e.reciprocal(out=den[:], in_=den[:]), reads=[b], writes=[b])
            Tk.op("dve", lambda e: e.tensor_tensor(out=t1[:], in0=ar[:], in1=LR[:], op=ALU.mult), reads=[b], writes=[b])
            Tk.op("dve", lambda e: e.tensor_tensor(out=t2[:], in0=ai[:], in1=LI[:], op=ALU.mult), reads=[b], writes=[b])
            Tk.op("dve", lambda e: e.tensor_tensor(out=t1[:], in0=t1[:], in1=t2[:], op=ALU.add), reads=[b], writes=[b])
            Tk.op("dve", lambda e: e.tensor_tensor(out=fr[:], in0=t1[:], in1=den[:], op=ALU.mult), reads=[b], writes=[b])
            Tk.op("dve", lambda e: e.tensor_tensor(out=t1[:], in0=ai[:], in1=LR[:], op=ALU.mult), reads=[b], writes=[b])
            Tk.op("dve", lambda e: e.tensor_tensor(out=t2[:], in0=ar[:], in1=LI[:], op=ALU.mult), reads=[b], writes=[b])
            Tk.op("dve", lambda e: e.tensor_tensor(out=t1[:], in0=t1[:], in1=t2[:], op=ALU.subtract), reads=[b], writes=[b])
            Tk.op("dve", lambda e: e.tensor_tensor(out=fi[:], in0=t1[:], in1=den[:], op=ALU.mult), reads=[b], writes=[b])
            Tk.op("pool", lambda e: e.memset(sgn[0:64, :], 1.0), writes=[b])
            Tk.op("pool", lambda e: e.memset(sgn[64:128, :], -1.0), writes=[b])
            Tk.op("dve", lambda e: e.tensor_scalar(out=frs[:], in0=fr[:], scalar1=sgn[:, 0:1], scalar2=None, op0=ALU.mult), reads=[b], writes=[b])
            Tk.op("dve", lambda e: e.tensor_scalar(out=fis[:], in0=fi[:], scalar1=sgn[:, 0:1], scalar2=-1.0, op0=ALU.mult, op1=ALU.mult), reads=[b], writes=[b])
            bre = din["ssm_b_re"][l].rearrange("d g n c -> n (d g) c")
            bim = din["ssm_b_im"][l].rearrange("d g n c -> n (d g) c")
            Tk.dma("sync", Pm[0:64], bre, writes=[b])
            Tk.dma("sync", Pm[64:128], bim, writes=[b])
            Tk.dma("sync", Qm[0:64], bim, writes=[b])
            Tk.dma("sync", Qm[64:128], bre, writes=[b])

            def bc3(t):
                return t[:].unsqueeze(2).to_broadcast([128, 32, 16])
            Tk.op("dve", lambda e: e.tensor_tensor(out=GT1[:], in0=Pm[:], in1=bc3(fr), op=ALU.mult), reads=[b], writes=[b])
            Tk.op("dve", lambda e: e.tensor_tensor(out=gtmp[:], in0=Qm[:], in1=bc3(fis), op=ALU.mult), reads=[b], writes=[b])
            Tk.op("dve", lambda e: e.tensor_tensor(out=GT1[:], in0=GT1[:], in1=gtmp[:], op=ALU.add), reads=[b], writes=[b])
            Tk.op("dve", lambda e: e.tensor_tensor(out=GT2[:], in0=Qm[:], in1=bc3(frs), op=ALU.mult), reads=[b], writes=[b])
            Tk.op("dve", lambda e: e.tensor_tensor(out=gtmp[:], in0=Pm[:], in1=bc3(fi), op=ALU.mult), reads=[b], writes=[b])
            Tk.op("dve", lambda e: e.tensor_tensor(out=GT2[:], in0=GT2[:], in1=gtmp[:], op=ALU.add), reads=[b], writes=[b])
            Tk.op("pool", lambda e: e.memset(rowmask[:], 1.0), writes=[b])
            Tk.op("pool", lambda e: e.affine_select(out=rowmask[:], in_=rowmask[:], pattern=[[-16, 8]], compare_op=ALU.is_ge, fill=0.0, base=0, channel_multiplier=1), reads=[b], writes=[b])
            Tk.op("pool", lambda e: e.affine_select(out=rowmask[:], in_=rowmask[:], pattern=[[16, 8]], compare_op=ALU.is_ge, fill=0.0, base=15, channel_multiplier=-1), reads=[b], writes=[b])
            for GT, Gd in ((GT1, G1), (GT2, G2)):
                for d in range(2):
                    for ct in range(2):
                        dg0 = d * 16 + ct * 8
                        Tk.op("pe", lambda e: e.transpose(pp_[:, 0:128], GT[:, dg0:dg0 + 8, :].rearrange("p g c -> p (g c)"), self.ident_f[:]), reads=[b, self.b_const], writes=[bps])
                        Tk.op("act", lambda e: e.copy(out=TB[:], in_=pp_[:, 0:128]), reads=[bps], writes=[b])
                        for gg in range(8):
                            Tk.op("dve", lambda e: e.tensor_scalar(out=Gd[:, dg0 + gg, :], in0=TB[:], scalar1=rowmask[:, gg:gg + 1], scalar2=None, op0=ALU.mult), reads=[b], writes=[bP])
            Tk.op("pool", lambda e: e.memset(C1[:], 0.0), writes=[bP])
            Tk.op("pool", lambda e: e.memset(C2[:], 0.0), writes=[bP])
            cre = din["ssm_c_re"][l].rearrange("d g c n -> (d g c) n")
            cim = din["ssm_c_im"][l].rearrange("d g c n -> (d g c) n")
            for d in range(2):
                for ct in range(2):
                    dg0 = d * 16 + ct * 8
                    r0 = (d * 2 + ct) * 128
                    Tk.dma("sync", crl[:], cre[r0:r0 + 128, :], writes=[b])
                    Tk.dma("sync", cil[:], cim[r0:r0 + 128, :], writes=[b])
                    Tk.op("act", lambda e: e.copy(out=CT[:, 0, 0:64], in_=crl[:]), reads=[b], writes=[b])
                    Tk.op("act", lambda e: e.mul(out=CT[:, 0, 64:128], in_=cil[:], mul=-1.0), reads=[b], writes=[b])
                    Tk.op("act", lambda e: e.mul(out=CT[:, 1, 0:64], in_=cil[:], mul=-1.0), reads=[b], writes=[b])
                    Tk.op("act", lambda e: e.mul(out=CT[:, 1, 64:128], in_=crl[:], mul=-1.0), reads=[b], writes=[b])
                    for v, Cd in ((0, C1), (1, C2)):
                        Tk.op("pe", lambda e: e.transpose(pp_[:, 0:128], CT[:, v, :], self.ident_f[:]), reads=[b, self.b_const], writes=[bps])
                        for gg in range(8):
                            Tk.op("dve", lambda e: e.tensor_copy(out=Cd[:, dg0 + gg, gg * 16:(gg + 1) * 16], in_=pp_[:, gg * 16:(gg + 1) * 16]), reads=[bps], writes=[bP])
            Tk.op("pool", lambda e: e.iota(iot[:], pattern=[[1, Lc]], base=1, channel_multiplier=0), writes=[b])
            Tk.op("dve", lambda e: e.tensor_copy(out=iof[:], in_=iot[:]), reads=[b], writes=[b])
            bang = Buf()
            rrs_b = (self.sb(tes, "rr_i", [128, 8, Lc], I32), self.sb(tes, "rr_k", [128, 8, Lc], F32), Buf(), Buf())
            for q8 in range(4):
                for off, shf in ((Lc, 0.0), (0, PI / 2)):
                    Tk.op("dve", lambda e: e.tensor_tensor(out=ang[:], in0=iof[:].unsqueeze(1).to_broadcast([128, 8, Lc]), in1=th[:, q8 * 8:(q8 + 1) * 8].unsqueeze(2).to_broadcast([128, 8, Lc]), op=ALU.mult), reads=[b], writes=[bang])
                    if shf:
                        Tk.op("dve", lambda e: e.tensor_scalar(out=ang[:], in0=ang[:], scalar1=float(shf), scalar2=None, op0=ALU.add), reads=[bang], writes=[bang])
                    self.range_reduce(tes, ang[:], [128, 8, Lc], [bang], scratch=rrs_b)
                    Tk.op("act", lambda e: e.activation(out=CS[:, q8 * 8:(q8 + 1) * 8, off:off + Lc], in_=ang[:], func=AF.Sin), reads=[bang], writes=[bP])
            Tk.op("dve", lambda e: e.tensor_copy(out=shift[:, 0:64], in_=self.ident_f[:, 64:128]), reads=[self.b_const], writes=[b])
            Tk.op("dve", lambda e: e.tensor_copy(out=shift[:, 64:128], in_=self.ident_f[:, 0:64]), reads=[self.b_const], writes=[b])
            Tk.op("dve", lambda e: e.tensor_scalar(out=t1[:], in0=CS[:, :, 2 * Lc - 1], scalar1=sgn[:, 0:1], scalar2=None, op0=ALU.mult), reads=[b, bP], writes=[b])
            for dg in range(32):
                Tk.op("dve", lambda e: e.tensor_scalar(out=R[:, dg, :], in0=self.ident_f[:], scalar1=CS[:, dg, Lc - 1:Lc], scalar2=None, op0=ALU.mult), reads=[b, bP, self.b_const], writes=[bP])
                Tk.op("dve", lambda e: e.scalar_tensor_tensor(out=R[:, dg, :], in0=shift[:], scalar=t1[:, dg:dg + 1], in1=R[:, dg, :], op0=ALU.mult, op1=ALU.add), reads=[b, bP], writes=[bP])
            Tk.dma("sync", dcol[:], din["ssm_d"][l].rearrange("(ct p) -> p ct", p=128), writes=[b], allow_slow_non_contiguous=True)
            for ct in range(2):
                Tk.op("dve", lambda e: e.tensor_scalar(out=diagD[:, ct, :], in0=self.ident_f[:], scalar1=dcol[:, ct:ct + 1], scalar2=None, op0=ALU.mult), reads=[b, self.b_const], writes=[bP])
            self.dump("ssm_G1", G1[:], [128, 32, 128], BF16, [bP])
            self.dump("ssm_C1", C1[:], [128, 32, 128], BF16, [bP])
            self.dump("ssm_CS", CS[:], [128, 32, 2 * Lc], F32, [bP])
            self.dump("ssm_mag", mag[:], [128, 32], F32, [bP])
            Tk.barrier()
        Y = self.sb(es, "Y", [128, 2, T], F32)
        with ExitStack() as tes:
            NB = 4
            zz = [self.sb(tes, "zz", [128, 2 * Lc], F32) for _ in range(NB)]
            z = [self.sb(tes, "z", [128, Lc], F32) for _ in range(NB)]
            w = [self.sb(tes, "w", [128, Lc], F32) for _ in range(NB)]
            pp = [self.sb(tes, "pp", [128, 2, Lc], BF16) for _ in range(NB)]
            pGb = [self.ps(tes, "pG", [128, 512], F32) for _ in range(NB)]
            pC = self.ps(tes, "pC", [128, 512], F32)
            pY = [self.ps(tes, "pY", [128, 512], F32) for _ in range(2)]
            bzz, bz, bpp = [[Buf() for _ in range(NB)] for _ in range(3)]
            bw = [Buf() for _ in range(NB)]
            bpG = [Buf() for _ in range(NB)]
            bpC = [Buf()] * 32
            bcar = [Buf() for _ in range(32)]
            bpY = [Buf(), Buf()]
            bY = [[Buf() for _ in range(NC)] for _ in range(2)]
            its = []
            yi = 0
            for j in range(NC):
                for d in range(2):
                    for ct in range(2):
                        if d == 0:
                            blk = j
                            usl = U[:, ct, j * Lc:(j + 1) * Lc]
                        else:
                            blk = NC - 1 - j
                            usl = rev_ap(U[:, ct, blk * Lc:(blk + 1) * Lc])
                        for gg in range(8):
                            its.append(dict(j=j, d=d, ct=ct, gg=gg, dg=d * 16 + ct * 8 + gg, blk=blk, usl=usl, yk=yi % 2, n=len(its)))
                        yi += 1

            def stA(q):
                kb, dg, usl = q["n"] % NB, q["dg"], q["usl"]
                pG = pGb[kb][:, 0:2 * Lc]
                Tk.op("pe", lambda e: e.matmul(pG[:, 0:Lc], lhsT=G1[:, dg, :], rhs=usl, start=True, stop=True), reads=allU + [bP], writes=[bpG[kb]], inc=False)
                Tk.op("pe", lambda e: e.matmul(pG[:, Lc:2 * Lc], lhsT=G2[:, dg, :], rhs=usl, start=True, stop=True), reads=allU + [bP], writes=[bpG[kb]])

            def stB(qs):
                for q in qs:
                    kb, dg = q["n"] % NB, q["dg"]
                    pG = pGb[kb][:, 0:2 * Lc]
                    Tk.op("dve", lambda e: e.tensor_tensor(out=zz[kb][:], in0=pG, in1=CS[:, dg, :], op=ALU.mult), reads=[bpG[kb], bP], writes=[bzz[kb]])
                for q in qs:
                    kb = q["n"] % NB
                    Tk.op("dve", lambda e: e.tensor_tensor(out=z[kb][:], in0=zz[kb][:, 0:Lc], in1=zz[kb][:, Lc:2 * Lc], op=ALU.add), reads=[bzz[kb]], writes=[bz[kb]])
                for q in qs:
                    kb, dg, j = q["n"] % NB, q["dg"], q["j"]
                    init = 0.0 if j == 0 else carry[:, dg:dg + 1]
                    Tk.op("dve", lambda e: e.tensor_tensor_scan(out=w[kb][:], data0=mag[:, dg:dg + 1].to_broadcast([128, Lc]), data1=z[kb][:], initial=init, op0=ALU.mult, op1=ALU.add),
                          reads=[bz[kb], bP, bcar[dg]], writes=[bw[kb]])

            def stC(q):
                kb, dg, j = q["n"] % NB, q["dg"], q["j"]
                if j < NC - 1:
                    Tk.op("pe", lambda e: e.matmul(pC[:, dg:dg + 1], lhsT=R[:, dg, :], rhs=w[kb][:, Lc - 1:Lc], start=True, stop=True), reads=[bw[kb], bP], writes=[bpC[dg]])
                    Tk.op("act", lambda e: e.copy(out=carry[:, dg:dg + 1], in_=pC[:, dg:dg + 1]), reads=[bpC[dg]], writes=[bcar[dg]])
                Tk.op("pool", lambda e: e.tensor_tensor(out=pp[kb][:], in0=CS[:, dg, :].rearrange("p (a q) -> p a q", a=2), in1=w[kb][:].unsqueeze(1).to_broadcast([128, 2, Lc]), op=ALU.mult),
                      reads=[bw[kb], bP], writes=[bpp[kb]])

            def stE(q):
                kb, dg, j, d, ct, gg, blk, usl = q["n"] % NB, q["dg"], q["j"], q["d"], q["ct"], q["gg"], q["blk"], q["usl"]
                py, bpy = pY[q["yk"]], bpY[q["yk"]]
                Tk.op("pe", lambda e: e.matmul(py[:, 0:Lc], lhsT=C1[:, dg, :], rhs=pp[kb][:, 0, :], start=(gg == 0), stop=False), reads=[bpp[kb], bP], writes=[bpy], inc=False)
                first = (j < NC / 2)
                last = (gg == 7 and d == 1 and first)
                Tk.op("pe", lambda e: e.matmul(py[:, 0:Lc], lhsT=C2[:, dg, :], rhs=pp[kb][:, 1, :], start=False, stop=last), reads=[bpp[kb], bP], writes=[bpy])
                if gg != 7:
                    return
                ysl = Y[:, ct, blk * Lc:(blk + 1) * Lc]
                osl = ysl if d == 0 else rev_ap(ysl)
                if d == 0:
                    Tk.op("pe", lambda e: e.matmul(py[:, 0:Lc], lhsT=diagD[:, ct, :], rhs=usl, start=False, stop=first), reads=allU + [bP], writes=[bpy])
                if not first:
                    Tk.op("pe", lambda e: e.matmul(py[:, 0:Lc], lhsT=self.ident_f[:], rhs=osl, start=False, stop=True), reads=[bY[ct][blk], self.b_const], writes=[bpy])
                Tk.op("act", lambda e: e.copy(out=osl, in_=py[:, 0:Lc]), reads=[bpy], writes=[bY[ct][blk]])

            NI = len(its)
            NP = NI // 2
            prs = [its[2 * m:2 * m + 2] for m in range(NP)]
            for sidx in range(NP + 2):
                if sidx < NP:
                    for q in prs[sidx]:
                        stA(q)
                if 1 <= sidx <= NP:
                    stB(prs[sidx - 1])
                    for q in prs[sidx - 1]:
                        stC(q)
                if sidx >= 2:
                    for q in prs[sidx - 2]:
                        stE(q)
            self.dump("ssm_Y", Y[:], [128, 2, T], F32, [bY[ct][k] for ct in range(2) for k in range(NC)])
            Tk.barrier()
        with ExitStack() as tes:
            wglu = self.sb(tes, "wglu", [128, 2, 512], BF16)
            bglu = self.sb(tes, "bglu", [128, 4], F32)
            gss = self.sb(tes, "gss", [128, 2], F32)
            gY = [self.sb(tes, "gY", [128, 2, 512], BF16) for _ in range(2)]
            sg = self.sb(tes, "sg", [128, 2, 512], F32)
            og = self.sb(tes, "og", [128, 2, 512], F32)
            sq = self.sb(tes, "sq3", [128, 2, 512], BF16)
            rs = self.sb(tes, "rs3", [128, 512], F32)
            ho = [self.sb(tes, "ho", [128, 2, 512], BF16) for _ in range(2)]
            pz = [self.ps(tes, "pz", [128, 512], F32) for _ in range(4)]
            pss = self.ps(tes, "pss3", [128, 512], F32)
            bw_ = Buf()
            Tk.dma("pool", wglu[:], din["ssm_w_glu"][l].rearrange("(ct p) n -> p ct n", p=128), writes=[bw_])
            Tk.dma("sync", bglu[:], din["ssm_b_glu"][l].rearrange("(j p) -> p j", p=128), writes=[bw_], allow_slow_non_contiguous=True)
            Tk.dma("sync", gss[:], din["out_g_ssm"][l].rearrange("(j p) -> p j", p=128), writes=[bw_], allow_slow_non_contiguous=True)
            bgY, bho = [Buf(), Buf()], [Buf(), Buf()]
            bpz = [Buf() for _ in range(4)]
            bsg, bog, bsq, bpss, brs = [Buf() for _ in range(5)]
            allY = [bY[ct][k] for ct in range(2) for k in range(NC)]
            for c in range(self.NCH):
                k = c % 2
                cols = slice(c * 512, (c + 1) * 512)
                Tk.op("act", lambda e: e.activation(out=gY[k][:], in_=Y[:, :, cols], func=AF.Gelu_apprx_tanh), reads=allY, writes=[bgY[k]])
                for jt in range(4):
                    for ct in range(2):
                        Tk.op("pe", lambda e: e.matmul(pz[jt][:], lhsT=wglu[:, ct, jt * 128:(jt + 1) * 128], rhs=gY[k][:, ct, :], start=(ct == 0), stop=(ct == 1)), reads=[bgY[k], bw_], writes=[bpz[jt]], inc=(ct == 1))
                for a in range(2):
                    Tk.op("act", lambda e: e.activation(out=sg[:, a, :], in_=pz[2 + a][:], func=AF.Sigmoid, bias=bglu[:, 2 + a:3 + a]), reads=[bpz[2 + a], bw_], writes=[bsg])
                    Tk.op("dve", lambda e: e.scalar_tensor_tensor(out=og[:, a, :], in0=pz[a][:], scalar=bglu[:, a:a + 1], in1=sg[:, a, :], op0=ALU.add, op1=ALU.mult), reads=[bpz[a], bsg, bw_], writes=[bog])
                Tk.op("act", lambda e: e.activation(out=sq[:], in_=og[:], func=AF.Square), reads=[bog], writes=[bsq])
                for a in range(2):
                    Tk.op("pe", lambda e: e.matmul(pss[:], lhsT=self.ones_bf[:], rhs=sq[:, a, :], start=(a == 0), stop=(a == 1)), reads=[bsq, self.b_const], writes=[bpss], inc=(a == 1))
                Tk.op("dve", lambda e: e.tensor_scalar(out=rs[:], in0=pss[:], scalar1=1.0 / 256, scalar2=EPS, op0=ALU.mult, op1=ALU.add), reads=[bpss], writes=[brs])
                Tk.op("act", lambda e: e.activation(out=rs[:], in_=rs[:], func=AF.Sqrt), reads=[brs], writes=[brs])
                Tk.op("dve", lambda e: e.reciprocal(out=rs[:], in_=rs[:]), reads=[brs], writes=[brs])
                for a in range(2):
                    Tk.op("dve", lambda e: e.scalar_tensor_tensor(out=ho[k][:, a, :], in0=og[:, a, :], scalar=gss[:, a:a + 1], in1=rs[:], op0=ALU.mult, op1=ALU.mult), reads=[bog, brs, bw_], writes=[bho[k]])
                Tk.dma("sync", self.HT[384:640, cols].rearrange("(a p) q -> p a q", p=128), ho[k][:], reads=[bho[k]], writes=[Tk.b("HT", l, "ssm", c)])
            Tk.barrier()


MK.p3 = _p3


def _p4(self, l):
    nc, Tk, T, NT, NCH = self.nc, self.trk, self.T, self.NT, self.NCH
    m = self.mix
    CQ, CKV, KR = m["CQ"], m["CKV"], m["KR"]
    din = self.din
    SCALE = 96.0 ** -0.5
    allin = [Tk.b(n, l, c) for n in ("CQ", "CKV", "KR") for c in range(NCH)]
    with ExitStack() as es:
        wuq = self.sb(es, "wuq", [128, 2, 576], BF16)
        wsw = self.sb(es, "wsw", [128, 2, 6, 96], BF16)
        wk = self.sb(es, "wk", [128, 6, 64], BF16)
        wv = self.sb(es, "wv", [128, 6, 64], BF16)
        gml = self.sb(es, "gml", [64, 6], F32)
        ones_f = self.sb(es, "ones_f4", [128, 64], F32)
        K = self.sb(es, "K", [128, 6, T], BF16)
        V = self.sb(es, "V", [128, NT, 6, 65], BF16)
        bW = Buf()
        bK = [Buf() for _ in range(NCH)]
        bKr = Buf()
        bV = [Buf() for _ in range(NT)]
        with ExitStack() as tes:
            sq_ = self.sb(tes, "squ", [128, 2, 576], F32)
            skv = self.sb(tes, "skv", [128, 768], F32)
            gq = self.sb(tes, "gq", [128, 2], F32)
            gkv = self.sb(tes, "gkv", [128, 1], F32)
            wukv = self.sb(tes, "wukv", [128, 6, 128], BF16)
            b = Buf()
            Tk.dma("sync", sq_[:, 0, :], din["mla_w_uq"][l, 0:128, :], writes=[b])
            Tk.dma("sync", sq_[0:64, 1, :], din["mla_w_uq"][l, 128:192, :], writes=[b])
            Tk.dma("sync", skv[:], din["mla_w_ukv"][l], writes=[b])
            gqs = din["mla_q_norm_g"][l]
            Tk.dma("sync", gq[:, 0:1], gqs[0:128].rearrange("(p o) -> p o", o=1), writes=[b])
            Tk.dma("sync", gq[0:64, 1:2], gqs[128:192].rearrange("(p o) -> p o", o=1), writes=[b])
            Tk.dma("sync", gkv[:], din["mla_kv_norm_g"][l].rearrange("(p o) -> p o", o=1), writes=[b])
            Tk.dma("sync", gml[:], din["out_g_mla"][l].rearrange("(h d) -> d h", d=64), writes=[bW], allow_slow_non_contiguous=True)
            Tk.op("dve", lambda e: e.tensor_scalar(out=wuq[:, 0, :], in0=sq_[:, 0, :], scalar1=gq[:, 0:1], scalar2=None, op0=ALU.mult), reads=[b], writes=[bW])
            Tk.op("dve", lambda e: e.tensor_scalar(out=wuq[0:64, 1, :], in0=sq_[0:64, 1, :], scalar1=gq[0:64, 1:2], scalar2=None, op0=ALU.mult), reads=[b], writes=[bW])
            Tk.op("dve", lambda e: e.tensor_scalar(out=wukv[:].rearrange("p h f -> p (h f)"), in0=skv[:], scalar1=gkv[:, 0:1], scalar2=None, op0=ALU.mult), reads=[b], writes=[b])
            Tk.op("pool", lambda e: e.memset(wsw[:], 0.0), writes=[bW])
            for kc, pr in ((0, 128), (1, 64)):
                src = wuq[0:pr, kc, :].rearrange("p (h f) -> p h f", h=6)
                Tk.op("act", lambda e: e.mul(out=wsw[0:pr, kc, :, 64:80], in_=src[:, :, 80:96], mul=-1.0), reads=[bW], writes=[bW])
                Tk.op("act", lambda e: e.copy(out=wsw[0:pr, kc, :, 80:96], in_=src[:, :, 64:80]), reads=[bW], writes=[bW])
            Tk.op("dve", lambda e: e.tensor_copy(out=wk[:], in_=wukv[:, :, 0:64]), reads=[b], writes=[bW])
            Tk.op("dve", lambda e: e.tensor_copy(out=wv[:], in_=wukv[:, :, 64:128]), reads=[b], writes=[bW])
            Tk.op("pool", lambda e: e.memset(ones_f[:], 1.0), writes=[bW])
            Tk.op("pool", lambda e: e.memset(V[:, :, :, 64:65], 1.0), writes=[bW])
            sqk = [self.sb(tes, "sqk", [128, 512], BF16) for _ in range(2)]
            rkv = [self.sb(tes, "rkv", [128, 512], F32) for _ in range(2)]
            rcol = [self.sb(tes, "rcol", [128, 1], F32) for _ in range(2)]
            pss = self.ps(tes, "pss4", [128, 512], F32)
            pk = [self.ps(tes, "pk", [128, 512], F32) for _ in range(2)]
            pv = [self.ps(tes, "pv", [128, 512], F32) for _ in range(2)]
            pc = self.ps(tes, "pc", [128, 512], F32)
            bsqk, brkv, brcol = [Buf(), Buf()], [Buf(), Buf()], [Buf(), Buf()]
            bpss, bpc = Buf(), Buf()
            bpk, bpv = [Buf(), Buf()], [Buf(), Buf()]
            n = 0
            for h in range(6):
                Tk.op(("act", "pool")[h % 2], lambda e: (e.copy(out=K[64:96, h, :], in_=KR[64:96, :]) if h % 2 == 0 else e.tensor_copy(out=K[64:96, h, :], in_=KR[64:96, :])), reads=allin, writes=[bKr])
            for c in range(NCH):
                k = c % 2
                cols = slice(c * 512, (c + 1) * 512)
                Tk.op("act", lambda e: e.activation(out=sqk[k][:], in_=CKV[:, cols], func=AF.Square), reads=allin, writes=[bsqk[k]])
                Tk.op("pe", lambda e: e.matmul(pss[:], lhsT=self.ones_bf[:], rhs=sqk[k][:], start=True, stop=True), reads=[bsqk[k], self.b_const], writes=[bpss])
                Tk.op("dve", lambda e: e.tensor_scalar(out=rkv[k][:], in0=pss[:], scalar1=1.0 / 128, scalar2=EPS, op0=ALU.mult, op1=ALU.add), reads=[bpss], writes=[brkv[k]])
                Tk.op("act", lambda e: e.activation(out=rkv[k][:], in_=rkv[k][:], func=AF.Sqrt), reads=[brkv[k]], writes=[brkv[k]])
                Tk.op("dve", lambda e: e.reciprocal(out=rkv[k][:], in_=rkv[k][:]), reads=[brkv[k]], writes=[brkv[k]])
                for h in range(6):
                    kk = n % 2
                    n += 1
                    Tk.op("pe", lambda e: e.matmul(pk[kk][0:64, :], lhsT=wk[:, h, :], rhs=CKV[:, cols], start=True, stop=True), reads=allin + [bW], writes=[bpk[kk]])
                    Tk.op("dve", lambda e: e.tensor_tensor(out=K[0:64, h, cols], in0=pk[kk][0:64, :], in1=rkv[k][0:64, :], op=ALU.mult), reads=[bpk[kk], brkv[k]], writes=[bK[c]])
                for tt in range(4):
                    i = 4 * c + tt
                    kk = i % 2
                    ts_ = slice(i * 128, (i + 1) * 128)
                    Tk.op("pe", lambda e: e.matmul(pc[:, i % 512:i % 512 + 1], lhsT=sqk[k][:, tt * 128:(tt + 1) * 128], rhs=self.ones_bf[:, 0:1], start=True, stop=True), reads=[bsqk[k], self.b_const], writes=[bpc])
                    Tk.op("dve", lambda e: e.tensor_scalar(out=rcol[kk][:], in0=pc[:, i % 512:i % 512 + 1], scalar1=1.0 / 128, scalar2=EPS, op0=ALU.mult, op1=ALU.add), reads=[bpc], writes=[brcol[kk]])
                    Tk.op("act", lambda e: e.activation(out=rcol[kk][:], in_=rcol[kk][:], func=AF.Sqrt), reads=[brcol[kk]], writes=[brcol[kk]])
                    Tk.op("dve", lambda e: e.reciprocal(out=rcol[kk][:], in_=rcol[kk][:]), reads=[brcol[kk]], writes=[brcol[kk]])
                    Tk.op("pe", lambda e: e.matmul(pv[kk][:, 0:384], lhsT=CKV[:, ts_], rhs=wv[:].rearrange("p h f -> p (h f)"), start=True, stop=True), reads=allin + [bW], writes=[bpv[kk]])
                    Tk.op("dve", lambda e: e.tensor_scalar(out=V[:, i, :, 0:64], in0=pv[kk][:, 0:384].rearrange("p (h f) -> p h f", h=6), scalar1=rcol[kk][:, 0:1], scalar2=None, op0=ALU.mult), reads=[bpv[kk], brcol[kk]], writes=[bV[i]])
            self.dump("mla_K", K[:], [128, 6, T], BF16, bK + [bKr])
            self.dump("mla_V", V[:], [128, NT, 6, 65], BF16, bV + [bW])
            Tk.barrier()
        with ExitStack() as tes:
            sqq = self.sb(tes, "sqq", [128, 2, 512], BF16)
            rq = self.sb(tes, "rq", [128, 512], F32)
            CR = self.sb(tes, "CR", [128, 512], F32)
            SR = self.sb(tes, "SR", [128, 512], F32)
            qt1 = self.sb(tes, "qt1", [128, 512], F32)
            qt2 = self.sb(tes, "qt2", [128, 512], F32)
            Q = [self.sb(tes, "Q", [128, 6, 512], BF16) for _ in range(2)]
            rp = self.sb(tes, "rp4", [128, 2, 512], F32)
            brp = Buf()
            PT = [self.sb(tes, "PT4", [128, 512], BF16) for _ in range(3)]
            Osb = self.sb(tes, "Osb4", [65, 512], F32)
            rd = self.sb(tes, "rd4", [65, 512], F32)
            oall = self.sb(tes, "oall", [64, 6, 512], F32)
            sqo = self.sb(tes, "sqo", [64, 6, 512], BF16)
            rs = self.sb(tes, "rs4", [64, 512], F32)
            hT = self.sb(tes, "hT4", [64, 6, 512], BF16)
            pS = [self.ps(tes, "pS4", [128, 512], F32) for _ in range(4)]
            pO = [self.ps(tes, "pO4", [128, 512], F32) for _ in range(2)]
            pM = [self.ps(tes, "pM4", [128, 512], F32) for _ in range(2)]
            bsqq, brq, bCR, bqt = Buf(), Buf(), Buf(), Buf()
            bQ, bhT = [Buf(), Buf()], Buf()
            bPT, bpS = [Buf() for _ in range(3)], [Buf() for _ in range(4)]
            bpO, bpM = [Buf(), Buf()], [Buf(), Buf()]
            bOsb, brd, boall, bsqo, brs = [Buf() for _ in range(5)]
            allK = bK + [bKr]
            allV = bV + [bW]
            r2 = slice(64, 96)

            def prepA1(c):
                cols = slice(c * 512, (c + 1) * 512)
                Tk.op("act", lambda e: e.activation(out=sqq[:, 0, :], in_=CQ[:, 0, cols], func=AF.Square), reads=allin, writes=[bsqq])
                Tk.op("act", lambda e: e.activation(out=sqq[0:64, 1, :], in_=CQ[0:64, 1, cols], func=AF.Square), reads=allin, writes=[bsqq])
                for v in range(2):
                    Tk.dma("sync", rp[64:96, v, :], self.ROPE[v, :, cols], reads=[self.b_rope], writes=[brp])

            def prepA2(c):
                Tk.op("pe", lambda e: e.matmul(pM[0][:], lhsT=self.ones_bf[:], rhs=sqq[:, 0, :], start=True, stop=False), reads=[bsqq, self.b_const], writes=[bpM[0]], inc=False)
                Tk.op("pe", lambda e: e.matmul(pM[0][:], lhsT=self.ones_bf[0:64, :], rhs=sqq[0:64, 1, :], start=False, stop=True), reads=[bsqq, self.b_const], writes=[bpM[0]])
                Tk.op("dve", lambda e: e.tensor_scalar(out=rq[:], in0=pM[0][:], scalar1=1.0 / 192, scalar2=EPS, op0=ALU.mult, op1=ALU.add), reads=[bpM[0]], writes=[brq])
                Tk.op("act", lambda e: e.activation(out=rq[:], in_=rq[:], func=AF.Sqrt), reads=[brq], writes=[brq])
                Tk.op("dve", lambda e: e.reciprocal(out=rq[:], in_=rq[:]), reads=[brq], writes=[brq])
                Tk.op("dve", lambda e: e.tensor_tensor(out=CR[r2, :], in0=rp[r2, 0, :], in1=rq[r2, :], op=ALU.mult), reads=[brq, brp], writes=[bCR])
                Tk.op("dve", lambda e: e.tensor_tensor(out=SR[r2, :], in0=rp[r2, 1, :], in1=rq[r2, :], op=ALU.mult), reads=[brq, brp], writes=[bCR])

            def prepQ(c, h):
                cols = slice(c * 512, (c + 1) * 512)
                qb = c % 2
                for v in range(2):
                    for kc, pr in ((0, 128), (1, 64)):
                        lhs = wuq[0:pr, kc, h * 96:(h + 1) * 96] if v == 0 else wsw[0:pr, kc, h, :]
                        Tk.op("pe", lambda e: e.matmul(pM[v][0:96, :], lhsT=lhs, rhs=CQ[0:pr, kc, cols], start=(kc == 0), stop=(kc == 1)), reads=allin + [bW], writes=[bpM[v]], inc=(kc == 1))
                Tk.op("dve", lambda e: e.tensor_tensor(out=Q[qb][0:64, h, :], in0=pM[0][0:64, :], in1=rq[0:64, :], op=ALU.mult), reads=[bpM[0], brq], writes=[bQ[qb]])
                Tk.op("dve", lambda e: e.tensor_tensor(out=qt1[r2, :], in0=pM[0][r2, :], in1=CR[r2, :], op=ALU.mult), reads=[bpM[0], bCR], writes=[bqt])
                Tk.op("dve", lambda e: e.tensor_tensor(out=qt2[r2, :], in0=pM[1][r2, :], in1=SR[r2, :], op=ALU.mult), reads=[bpM[1], bCR], writes=[bqt])
                Tk.op("dve", lambda e: e.tensor_tensor(out=Q[qb][r2, h, :], in0=qt1[r2, :], in1=qt2[r2, :], op=ALU.add), reads=[bqt], writes=[bQ[qb]])

            def epi(c):
                cols = slice(c * 512, (c + 1) * 512)
                Tk.op("act", lambda e: e.activation(out=sqo[:], in_=oall[:], func=AF.Square), reads=[boall], writes=[bsqo])
                for h in range(6):
                    Tk.op("pe", lambda e: e.matmul(pM[1][0:64, :], lhsT=self.ones_bf[0:64, 0:64], rhs=sqo[:, h, :], start=(h == 0), stop=(h == 5)), reads=[bsqo, self.b_const], writes=[bpM[1]], inc=(h == 5))
                Tk.op("dve", lambda e: e.tensor_scalar(out=rs[:], in0=pM[1][0:64, :], scalar1=1.0 / 384, scalar2=EPS, op0=ALU.mult, op1=ALU.add), reads=[bpM[1]], writes=[brs])
                Tk.op("act", lambda e: e.activation(out=rs[:], in_=rs[:], func=AF.Sqrt), reads=[brs], writes=[brs])
                Tk.op("dve", lambda e: e.reciprocal(out=rs[:], in_=rs[:]), reads=[brs], writes=[brs])
                Tk.op("dve", lambda e: e.tensor_tensor(out=oall[:], in0=oall[:], in1=rs[:].unsqueeze(1).to_broadcast([64, 6, 512]), op=ALU.mult), reads=[boall, brs], writes=[boall])
                Tk.op("dve", lambda e: e.tensor_tensor(out=hT[:], in0=oall[:], in1=gml[:].unsqueeze(2).to_broadcast([64, 6, 512]), op=ALU.mult), reads=[boall, bW], writes=[bhT])
                Tk.dma("sync", self.HT[640:1024, cols].rearrange("(h d) q -> d h q", d=64), hT[:], reads=[bhT], writes=[Tk.b("HT", l, "mla", c)])

            steps = [(c, h, kt) for c in range(NCH) for h in range(6) for kt in range(NT)]
            SPC = 6 * NT
            NS_ = len(steps)

            def stS(n):
                c, h, kt = steps[n]
                k4 = n % 4
                Tk.op("pe", lambda e: e.matmul(pS[k4][:], lhsT=K[0:96, h, kt * 128:(kt + 1) * 128], rhs=Q[c % 2][0:96, h, :], start=True, stop=True), reads=allK + [bQ[c % 2]], writes=[bpS[k4]])

            def stPV(n):
                c, h, kt = steps[n]
                k3, k4 = n % 3, n % 4
                po, bpo = pO[h % 2], bpO[h % 2]
                Tk.op("act", lambda e: e.activation(out=PT[k3][:], in_=pS[k4][:], func=AF.Exp, scale=SCALE), reads=[bpS[k4]], writes=[bPT[k3]])
                Tk.op("pe", lambda e: e.matmul(po[0:65, :], lhsT=V[:, kt, h, :], rhs=PT[k3][:], start=(kt == 0), stop=(kt == NT - 1)), reads=allV + [bPT[k3]], writes=[bpo], inc=(kt == NT - 1))
                if kt == NT - 1:
                    Tk.op("act", lambda e: e.copy(out=Osb[:], in_=po[0:65, :]), reads=[bpo], writes=[bOsb])
                    Tk.op("dve", lambda e: e.reciprocal(out=rd[64:65, :], in_=Osb[64:65, :]), reads=[bOsb], writes=[brd])

            def stEp(h):
                Tk.op("pe", lambda e: e.matmul(pM[0][0:64, :], lhsT=ones_f[64:65, 0:64], rhs=rd[64:65, :], start=True, stop=True), reads=[brd, bW], writes=[bpM[0]])
                Tk.op("dve", lambda e: e.tensor_tensor(out=oall[:, h, :], in0=Osb[0:64, :], in1=pM[0][0:64, :], op=ALU.mult), reads=[bOsb, bpM[0]], writes=[boall])

            hooks = {}

            def hook(n, f):
                hooks.setdefault(min(n, NS_), []).append(f)

            dly = min(4, NT - 2)
            for c in range(NCH):
                base = c * SPC
                if c + 1 < NCH:
                    hook(base + SPC // 8, lambda c=c: prepA1(c + 1))
                    hook(base + SPC // 8 + 3, lambda c=c: prepA2(c + 1))
                    for h in range(6):
                        hook(base + SPC // 4 + (h * SPC) // 10, lambda c=c, h=h: prepQ(c + 1, h))
                for h in range(6):
                    hook(base + (h + 1) * NT + dly, lambda h=h: stEp(h))
                hook(base + SPC + dly + 4, lambda c=c: epi(c))
            prepA1(0)
            prepA2(0)
            for h in range(6):
                prepQ(0, h)
            if True:
                self.dump("mla_Q", Q[0][:], [128, 6, 512], BF16, [bQ[0]])
            for n in range(NS_ + 2):
                if n < NS_:
                    stS(n)
                if n >= 2:
                    stPV(n - 2)
                for f in hooks.get(n - 1, ()):
                    f()
            Tk.barrier()


MK.p4 = _p4


def _p5(self, l, xsrc, xkey):
    nc, Tk, T, NT, NCH = self.nc, self.trk, self.T, self.NT, self.NCH
    din = self.din
    GATE = self.GATE
    with ExitStack() as es:
        wout = self.sb(es, "wout", [128, 8, D], BF16)
        wr = self.sb(es, "wr", [128, 8, 36], F32)
        brbc = self.sb(es, "brbc", [128, 36], F32)
        g2bc = self.sb(es, "g2bc", [128, D], F32)
        hTs = [self.sb(es, "hTs", [128, 8, 512], BF16) for _ in range(2)]
        xt = [self.sb(es, "xt5", [128, D], F32) for _ in range(2)]
        x1 = [self.sb(es, "x1", [128, D], F32) for _ in range(2)]
        junk = self.sb(es, "junk5", [128, D], BF16)
        xn2 = [self.sb(es, "xn2", [128, D], F32) for _ in range(2)]
        xTf = [self.sb(es, "xTf", [128, 8, 128], F32) for _ in range(2)]
        xTb = [self.sb(es, "xTb", [128, 8, 512], BF16) for _ in range(2)]
        ss = [self.sb(es, "ss5", [128, 1], F32) for _ in range(2)]
        rstd = [self.sb(es, "rstd5", [128, 1], F32) for _ in range(2)]
        sm = [self.sb(es, "sm", [128, 128], F32) for _ in range(2)]
        px = [self.ps(es, "px", [128, 512], F32) for _ in range(2)]
        pT = [self.ps(es, "pT5", [128, 1024], F32)]
        pr = self.ps(es, "pr", [128, 512], F32)
        bw = Buf()
        Tk.dma("pool", wout[:], din["w_out"][l].rearrange("(kc p) n -> p kc n", p=128), writes=[bw])
        Tk.dma("sync", wr[:, :, 0:4], din["w_router_group"][l].rearrange("(kc p) n -> p kc n", p=128), writes=[bw])
        Tk.dma("sync", wr[:, :, 4:36], din["w_router_expert"][l].rearrange("(kc p) n -> p kc n", p=128), writes=[bw])
        Tk.dma("sync", brbc[:, 0:4], din["b_router_group"][l:l + 1, :].partition_broadcast(128), writes=[bw])
        Tk.dma("sync", brbc[:, 4:36], din["b_router_expert"][l:l + 1, :].partition_broadcast(128), writes=[bw])
        Tk.dma("sync", g2bc[:], din["ln2_g"][l:l + 1, :].partition_broadcast(128), writes=[bw])
        bhTs, bxt, bx1, bxn2, bxTf, bxTb, bss, bsm = [[Buf(), Buf()] for _ in range(8)]
        bpx = [Buf(), Buf()]
        bpT = Buf()
        bpr, bjunk = Buf(), Buf()
        pT1 = pT[0]

        def s0(i):
            c, tt = divmod(i, 4)
            cb, p = c % 2, i % 2
            cols = slice(c * 512, (c + 1) * 512)
            if tt == 0:
                Tk.dma("sync", hTs[cb][:], self.HT[:, cols].rearrange("(kc p) q -> p kc q", p=128),
                       reads=[Tk.b("HT", l, "attn", j) for j in range(4 * c, 4 * c + 4)] + [Tk.b("HT", l, "ssm", c), Tk.b("HT", l, "mla", c)], writes=[bhTs[cb]])
            Tk.dma("sync", xt[p][:], xsrc[i * 128:(i + 1) * 128, :], reads=[Tk.b(xkey, i)], writes=[bxt[p]])
            for half in range(2):
                for kc in range(8):
                    Tk.op("pe", lambda e: e.matmul(px[half][:], lhsT=hTs[cb][:, kc, tt * 128:(tt + 1) * 128], rhs=wout[:, kc, half * 512:(half + 1) * 512], start=(kc == 0), stop=(kc == 7)),
                          reads=[bhTs[cb], bw], writes=[bpx[half]], inc=(kc == 7))

        def s1(i):
            p = i % 2
            rows = slice(i * 128, (i + 1) * 128)
            for half in range(2):
                Tk.op("dve", lambda e: e.tensor_tensor(out=x1[p][:, half * 512:(half + 1) * 512], in0=xt[p][:, half * 512:(half + 1) * 512], in1=px[half][:], op=ALU.add), reads=[bxt[p], bpx[half]], writes=[bx1[p]])
                yield
            Tk.dma("sync", self.XR[rows, :], x1[p][:], reads=[bx1[p], Tk.b(xkey, i)], writes=[Tk.b("XR", i)])
            yield
            Tk.op("dve", lambda e: e.memset(ss[p][:], 0.0), writes=[bss[p]])
            yield
            Tk.op("act", lambda e: e.activation(out=junk[:], in_=x1[p][:], func=AF.Square, accum_out=ss[p][:]), reads=[bx1[p], bss[p]], writes=[bjunk, bss[p]])
            yield
            Tk.op("dve", lambda e: e.tensor_scalar(out=ss[p][:], in0=ss[p][:], scalar1=1.0 / D, scalar2=EPS, op0=ALU.mult, op1=ALU.add), reads=[bss[p]], writes=[bss[p]])
            yield
            Tk.op("act", lambda e: e.activation(out=ss[p][:], in_=ss[p][:], func=AF.Ln), reads=[bss[p]], writes=[bss[p]])
            yield
            Tk.op("act", lambda e: e.activation(out=rstd[p][:], in_=ss[p][:], func=AF.Exp, scale=-0.5), reads=[bss[p]], writes=[bss[p]])
            yield
            Tk.op("dve", lambda e: e.scalar_tensor_tensor(out=xn2[p][:], in0=x1[p][:], scalar=rstd[p][:, 0:1], in1=g2bc[:], op0=ALU.mult, op1=ALU.mult), reads=[bx1[p], bss[p], bw], writes=[bxn2[p]])
            yield

        def s2(i):
            c, tt = divmod(i, 4)
            cb, p = c % 2, i % 2
            cols = slice(c * 512, (c + 1) * 512)
            for kc in range(8):
                Tk.op("pe", lambda e: e.transpose(pT1[:, kc * 128:(kc + 1) * 128], xn2[p][:, kc * 128:(kc + 1) * 128], self.ident_f[:]), reads=[bxn2[p], self.b_const], writes=[bpT], inc=(kc == 7))
            for hb_ in range(2):
                pT3 = pT1[:, hb_ * 512:(hb_ + 1) * 512].rearrange("p (k t) -> p k t", k=4)
                Tk.op("act", lambda e: e.copy(out=xTf[p][:, hb_ * 4:(hb_ + 1) * 4, :], in_=pT3), reads=[bpT], writes=[bxTf[p]])
                Tk.op("dve", lambda e: e.tensor_copy(out=xTb[cb][:, hb_ * 4:(hb_ + 1) * 4, tt * 128:(tt + 1) * 128], in_=xTf[p][:, hb_ * 4:(hb_ + 1) * 4, :]), reads=[bxTf[p]], writes=[bxTb[cb]])
            if tt == 3:
                Tk.dma("sync", self.XN2T[:, cols].rearrange("(kc p) q -> p kc q", p=128), xTb[cb][:], reads=[bxTb[cb]], writes=[Tk.b("XN2T", l, c)])

        def s3(i):
            p = i % 2
            for kc in range(8):
                Tk.op("pe", lambda e: e.matmul(pr[:, 0:36], lhsT=xTf[p][:, kc, :], rhs=wr[:, kc, :], start=(kc == 0), stop=(kc == 7)), reads=[bxTf[p], bw], writes=[bpr], inc=(kc == 7))

        def s4(i):
            p = i % 2
            s = sm[p]
            bs = bsm[p]
            lg = s[:, 0:36]
            gmax, ngmax, gsum, pg = s[:, 36:37], s[:, 37:38], s[:, 38:39], s[:, 39:40]
            ghot, m1, gex = s[:, 40:44], s[:, 44:48], s[:, 48:52]
            top8 = s[:, 52:60]
            d21, e21, w1, w2 = s[:, 60:61], s[:, 61:62], s[:, 62:63], s[:, 63:64]
            msk = s[:, 64:96]
            g1 = s[:, 96:128]
            msk3 = msk.rearrange("p (g e) -> p g e", g=4)

            ops = []

            def dv(fn, extra=()):
                ops.append(lambda: Tk.op("dve", fn, reads=[bs] + list(extra), writes=[bs]))

            dv(lambda e: e.tensor_tensor(out=lg, in0=pr[:, 0:36], in1=brbc[:], op=ALU.add), [bpr, bw])
            dv(lambda e: e.tensor_reduce(out=gmax, in_=lg[:, 0:4], axis=AX.X, op=ALU.max))
            dv(lambda e: e.tensor_scalar(out=ghot, in0=lg[:, 0:4], scalar1=gmax, scalar2=None, op0=ALU.is_ge))
            dv(lambda e: e.tensor_scalar(out=ngmax, in0=gmax, scalar1=-1.0, scalar2=None, op0=ALU.mult))
            dv(lambda e: e.memset(gsum, 0.0))
            ops.append(lambda: Tk.op("act", lambda e: e.activation(out=gex, in_=lg[:, 0:4], func=AF.Exp, bias=ngmax, accum_out=gsum), reads=[bs], writes=[bs]))
            dv(lambda e: e.reciprocal(out=pg, in_=gsum))
            dv(lambda e: e.tensor_scalar(out=m1, in0=ghot, scalar1=-1.0, scalar2=1.0e4, op0=ALU.add, op1=ALU.mult))
            dv(lambda e: e.tensor_tensor(out=msk3, in0=lg[:, 4:36].rearrange("p (g e) -> p g e", g=4), in1=ghot.unsqueeze(2).to_broadcast([128, 4, 8]), op=ALU.mult))
            dv(lambda e: e.tensor_tensor(out=msk3, in0=msk3, in1=m1.unsqueeze(2).to_broadcast([128, 4, 8]), op=ALU.add))
            dv(lambda e: e.max(out=top8, in_=msk))
            dv(lambda e: e.tensor_tensor(out=d21, in0=top8[:, 1:2], in1=top8[:, 0:1], op=ALU.subtract))
            ops.append(lambda: Tk.op("act", lambda e: e.activation(out=e21, in_=d21, func=AF.Exp), reads=[bs], writes=[bs]))
            dv(lambda e: e.tensor_scalar(out=w1, in0=e21, scalar1=1.0, scalar2=None, op0=ALU.add))
            dv(lambda e: e.reciprocal(out=w1, in_=w1))
            dv(lambda e: e.tensor_tensor(out=w1, in0=w1, in1=pg, op=ALU.mult))
            dv(lambda e: e.tensor_tensor(out=w2, in0=w1, in1=e21, op=ALU.mult))
            dv(lambda e: e.tensor_scalar(out=g1, in0=msk, scalar1=top8[:, 0:1], scalar2=w1, op0=ALU.is_equal, op1=ALU.mult))
            dv(lambda e: e.tensor_scalar(out=msk, in0=msk, scalar1=top8[:, 1:2], scalar2=w2, op0=ALU.is_equal, op1=ALU.mult))
            ops.append(lambda: Tk.op("dve", lambda e: e.tensor_tensor(out=GATE[:, i, :], in0=g1, in1=msk, op=ALU.add), reads=[bs], writes=[Tk.b("GATE", l, i)]))
            for f in ops:
                f()
                yield

        def interleave(gens):
            gens = list(gens)
            while gens:
                for g_ in list(gens):
                    try:
                        next(g_)
                    except StopIteration:
                        gens.remove(g_)

        for k in range(NT + 4):
            gens = []
            if 0 <= k - 4 < NT:
                gens.append(s4(k - 4))
            if 0 <= k - 1 < NT:
                gens.append(s1(k - 1))
            interleave(gens)
            if k < NT:
                s0(k)
            if 0 <= k - 2 < NT:
                s2(k - 2)
            if 0 <= k - 3 < NT:
                s3(k - 3)
        Tk.barrier()


def _p6(self, l, last):
    nc, Tk, T, NT = self.nc, self.trk, self.T, self.NT
    din = self.din
    GATE = self.GATE
    SC = min(getattr(self, "moe_sc", 2048), T)
    NS = T // SC
    NTS = SC // 128
    NC4 = SC // 512
    NW = 3
    with ExitStack() as es:
        XN = self.sb(es, "XN", [128, 8, SC], BF16)
        yacc = self.sb(es, "yacc", [128, NTS, D], F32)
        wg = [self.sb(es, "wg", [128, 8, 256], BF16) for _ in range(NW)]
        wu = [self.sb(es, "wu", [128, 8, 256], BF16) for _ in range(NW)]
        wd = [self.sb(es, "wd", [128, 2, D], BF16) for _ in range(NW)]
        sgl = [self.sb(es, "sgl", [128, 2, 512], F32) for _ in range(2)]
        hT = [self.sb(es, "hT6", [128, 2, 512], BF16) for _ in range(2)]
        xt = [self.sb(es, "xt6", [128, D], F32) for _ in range(4)]
        fgbc = self.sb(es, "fgbc", [128, D], F32)
        junk = self.sb(es, "junk6", [128, D], BF16)
        ss = [self.sb(es, "ss6", [128, 1], F32) for _ in range(2)]
        pgu = [self.ps(es, "pgu", [128, 512], F32) for _ in range(4)]
        py = [self.ps(es, "py", [128, 512], F32) for _ in range(4)]
        bfg = Buf()
        if last:
            Tk.dma("sync", fgbc[:], din["final_g"].rearrange("(o n) -> o n", o=1).partition_broadcast(128), writes=[bfg])
        bXN = [Buf() for _ in range(NC4)]
        bw = [Buf() for _ in range(NW)]
        bsgl, bhT = [[[Buf(), Buf()] for _ in range(2)] for _ in range(2)]
        bxt = [Buf() for _ in range(4)]
        bss = [Buf(), Buf()]
        bpgu, bpy = [Buf() for _ in range(4)], [Buf() for _ in range(4)]
        byacc = [Buf() for _ in range(NTS)]
        bjunk = Buf()
        allgate = [Tk.b("GATE", l, i) for i in range(NT)]
        allxn = [Tk.b("XN2T", l, c) for c in range(self.NCH)]
        cnt = {"py": 0}

        def load_xn(s, c4):
            t0 = s * SC + c4 * 512
            Tk.dma("sync", XN[:, :, c4 * 512:(c4 + 1) * 512], self.XN2T[:, t0:t0 + 512].rearrange("(kc p) q -> p kc q", p=128), reads=allxn, writes=[bXN[c4]])

        def load_w(gex):
            ex = gex % 32
            g, ee = divmod(ex, 8)
            wb = gex % NW
            Tk.dma("pool", wg[wb][:], din["w_gate"][l, g, ee].rearrange("(kc p) f -> p kc f", p=128), writes=[bw[wb]])
            Tk.dma("pool", wu[wb][:], din["w_up"][l, g, ee].rearrange("(kc p) f -> p kc f", p=128), writes=[bw[wb]])
            Tk.dma("pool", wd[wb][:], din["w_down"][l, g, ee].rearrange("(fc p) n -> p fc n", p=128), writes=[bw[wb]])

        blocks = [(s, ex, c4) for s in range(NS) for ex in range(32) for c4 in range(NC4)]
        NBk = len(blocks)

        def stA(n, ft):
            s, ex, c4 = blocks[n]
            wb = (s * 32 + ex) % NW
            hb = n % 2
            cols = slice(c4 * 512, (c4 + 1) * 512)
            for v, wt in ((0, wg[wb]), (1, wu[wb])):
                pp = pgu[v * 2 + ft]
                for kc in range(8):
                    Tk.op("pe", lambda e: e.matmul(pp[:], lhsT=wt[:, kc, ft * 128:(ft + 1) * 128], rhs=XN[:, kc, cols], start=(kc == 0), stop=(kc == 7)),
                          reads=[bw[wb], bXN[c4]], writes=[bpgu[v * 2 + ft]], inc=(kc == 7))
            Tk.op("act", lambda e: e.activation(out=sgl[hb][:, ft, :], in_=pgu[ft][:], func=AF.Silu), reads=[bpgu[ft]], writes=[bsgl[hb][ft]])
            Tk.op("dve", lambda e: e.tensor_tensor(out=hT[hb][:, ft, :], in0=sgl[hb][:, ft, :], in1=pgu[2 + ft][:], op=ALU.mult), reads=[bsgl[hb][ft], bpgu[2 + ft]], writes=[bhT[hb][ft]])
            if ft == 1 and ex == 31 and s + 1 < NS:
                load_xn(s + 1, c4)

        def stD(n):
            s, ex, c4 = blocks[n]
            wb = (s * 32 + ex) % NW
            hb = n % 2
            for tt in range(4):
                ti = c4 * 4 + tt
                gi = s * NTS + ti
                if ex == 0:
                    xb = gi % 4
                    Tk.dma("sync", xt[xb][:], self.XR[gi * 128:(gi + 1) * 128, :], reads=[Tk.b("XR", gi)], writes=[bxt[xb]])
                for half in range(2):
                    k4 = cnt["py"] % 4
                    cnt["py"] += 1
                    for ft in range(2):
                        Tk.op("pe", lambda e: e.matmul(py[k4][:], lhsT=hT[hb][:, ft, tt * 128:(tt + 1) * 128], rhs=wd[wb][:, ft, half * 512:(half + 1) * 512], start=(ft == 0), stop=(ft == 1)),
                              reads=[bhT[hb][ft], bw[wb]], writes=[bpy[k4]], inc=(ft == 1))
                    hs = slice(half * 512, (half + 1) * 512)
                    ya = yacc[:, ti, hs]
                    gcol = GATE[:, gi, ex:ex + 1]
                    if ex == 0:
                        Tk.op("dve", lambda e: e.scalar_tensor_tensor(out=ya, in0=py[k4][:], scalar=gcol, in1=xt[gi % 4][:, hs], op0=ALU.mult, op1=ALU.add), reads=[bpy[k4], bxt[gi % 4]] + allgate, writes=[byacc[ti]])
                    else:
                        Tk.op("dve", lambda e: e.scalar_tensor_tensor(out=ya, in0=py[k4][:], scalar=gcol, in1=ya, op0=ALU.mult, op1=ALU.add), reads=[bpy[k4], byacc[ti]] + allgate, writes=[byacc[ti]])

        def finish(s):
            for ti in range(NTS):
                gi = s * NTS + ti
                p = gi % 2
                rows = slice(gi * 128, (gi + 1) * 128)
                if not last:
                    Tk.dma("sync", self.XR[rows, :], yacc[:, ti, :], reads=[byacc[ti]], writes=[Tk.b("XR", gi)])
                else:
                    Tk.op("dve", lambda e: e.memset(ss[p][:], 0.0), writes=[bss[p]])
                    Tk.op("act", lambda e: e.activation(out=junk[:], in_=yacc[:, ti, :], func=AF.Square, accum_out=ss[p][:]), reads=[byacc[ti], bss[p]], writes=[bjunk, bss[p]])
                    Tk.op("dve", lambda e: e.tensor_scalar(out=ss[p][:], in0=ss[p][:], scalar1=1.0 / D, scalar2=EPS, op0=ALU.mult, op1=ALU.add), reads=[bss[p]], writes=[bss[p]])
                    Tk.op("act", lambda e: e.activation(out=ss[p][:], in_=ss[p][:], func=AF.Sqrt), reads=[bss[p]], writes=[bss[p]])
                    Tk.op("dve", lambda e: e.reciprocal(out=ss[p][:], in_=ss[p][:]), reads=[bss[p]], writes=[bss[p]])
                    xb = gi % 4
                    Tk.op("dve", lambda e: e.scalar_tensor_tensor(out=xt[xb][:], in0=yacc[:, ti, :], scalar=ss[p][:, 0:1], in1=fgbc[:], op0=ALU.mult, op1=ALU.mult), reads=[byacc[ti], bss[p], bfg], writes=[bxt[xb]])
                    Tk.dma("sync", self.out[rows, :], xt[xb][:], reads=[bxt[xb]], writes=[Tk.b("OUT", gi)])

        for c4 in range(NC4):
            load_xn(0, c4)
        for gex in range(min(NW, NS * 32)):
            load_w(gex)
        stA(0, 0)
        stA(0, 1)
        for n in range(NBk):
            s, ex, c4 = blocks[n]
            if n + 1 < NBk:
                stA(n + 1, 0)
            stD(n)
            if c4 == NC4 - 1 and s * 32 + ex + NW < NS * 32:
                load_w(s * 32 + ex + NW)
            if ex == 31 and c4 == NC4 - 1:
                finish(s)
            if n + 1 < NBk:
                stA(n + 1, 1)
        Tk.barrier()


MK.p5 = _p5
MK.p6 = _p6


def kernel(**inputs):
    x = np.asarray(inputs["x"], dtype=np.float32)
    B, T, _ = x.shape
    mk = MK(T=T, depth=2)
    nc = mk.build()
    shared = {k: np.ascontiguousarray(np.asarray(inputs[k], dtype=np.float32)) for k in INPUT_SHAPES}
    in_maps = []
    for b in range(B):
        d = dict(shared)
        d["x"] = np.ascontiguousarray(x[b])
        in_maps.append(d)
    res = run_bass_kernel_spmd(nc, in_maps, core_ids=list(range(B)))
    return np.stack([np.asarray(r["out"], dtype=np.float32) for r in res.results], axis=0)
```
